# Optimizing a Trainium2 kernel written in Bass

```python
import math
import jax, jax.numpy as jnp
from jax import lax
import numpy as np

D_MODEL = 2048
BATCH = 8
SEQ = 2048
DEPTH = 2

HEAD_DIM = 64
D_MIX = D_MODEL
N_MIXERS = 4
GROUP_WIDTH = D_MIX // N_MIXERS
N_HEADS_GROUP = GROUP_WIDTH // HEAD_DIM

DILATED_PATTERNS = ((128, 1), (512, 4), (2048, 16))
DIL_BLOCK = 128

SGU_CHUNK = 128
SGU_LN_EPS = 1e-5

MOBA_BLOCK = 256
MOBA_TOPK = 3
MOBA_Q_CHUNK = 16

RWKV_W_LORA = 96
RWKV_A_LORA = 96
RWKV_G_LORA = 256
RWKV_LN_EPS = 64e-5
RWKV_WIDTH = 3 * GROUP_WIDTH + RWKV_W_LORA + RWKV_A_LORA + RWKV_G_LORA
RWKV_SPLITS = (GROUP_WIDTH, GROUP_WIDTH + RWKV_W_LORA, 2 * GROUP_WIDTH + RWKV_W_LORA,
               3 * GROUP_WIDTH + RWKV_W_LORA, 3 * GROUP_WIDTH + RWKV_W_LORA + RWKV_A_LORA)

NUM_BUCKETS = 32
MAX_DISTANCE = 2048

D_FF = -(-8 * D_MODEL // (3 * 256)) * 256

QKV_WIDTH = 3 * GROUP_WIDTH
SGU_WIDTH = 2 * GROUP_WIDTH
IN_SPLITS = (QKV_WIDTH, QKV_WIDTH + SGU_WIDTH, 2 * QKV_WIDTH + SGU_WIDTH)
D_IN_PROJ = 2 * QKV_WIDTH + SGU_WIDTH + RWKV_WIDTH

NORM_EPS = 1e-6
NEG_INF = -1e30

kernel_name = 'hymba_style_dilated_sgu_moba_rwkv7_hybrid'


def rmsnorm(x, g):
    xf = x.astype(jnp.float32)
    y = xf * lax.rsqrt(jnp.mean(xf * xf, axis=-1, keepdims=True) + NORM_EPS)
    return (y * g.astype(jnp.float32)).astype(x.dtype)


def t5_bucket(dist):
    dist = jnp.maximum(dist, 0)
    max_exact = NUM_BUCKETS // 2
    d = jnp.maximum(dist, 1).astype(jnp.float32)
    large = max_exact + (jnp.log(d / max_exact) / math.log(MAX_DISTANCE / max_exact)
                         * (NUM_BUCKETS - max_exact)).astype(jnp.int32)
    large = jnp.minimum(large, NUM_BUCKETS - 1)
    return jnp.where(dist < max_exact, dist, large)


def split_qkv_heads(p):
    B, S, _ = p.shape
    t = p.reshape(B, S, 3, N_HEADS_GROUP, HEAD_DIM).transpose(2, 0, 3, 1, 4)
    return t[0], t[1], t[2]


def merge_heads(y):
    B, H, S, Dh = y.shape
    return y.transpose(0, 2, 1, 3).reshape(B, S, H * Dh)


def dilated_window_attention(q, k, v, bias_hb, window, dilation):
    B, H, S, Dh = q.shape
    steps = window // dilation
    C = DIL_BLOCK
    L = S // dilation
    nb = -(-L // C)
    Lp = nb * C

    def to_sub(t):
        return t.reshape(B, H, L, dilation, Dh).transpose(0, 1, 3, 2, 4)

    qb = jnp.pad(to_sub(q), ((0, 0), (0, 0), (0, 0), (0, Lp - L), (0, 0))).reshape(B, H, dilation, nb, C, Dh)

    def band(t):
        tp = jnp.pad(to_sub(t), ((0, 0), (0, 0), (0, 0), (C, Lp - L), (0, 0))).reshape(B, H, dilation, nb + 1, C, Dh)
        return jnp.concatenate([tp[:, :, :, :-1], tp[:, :, :, 1:]], axis=4)

    kb, vb = band(k), band(v)
    logits = jnp.einsum('bhrnqd,bhrnkd->bhrnqk', qb, kb).astype(jnp.float32) * (Dh ** -0.5)
    qa = jnp.arange(C)[:, None]
    kbi = jnp.arange(2 * C)[None, :]
    delta = qa + C - kbi
    blk = jnp.arange(nb)[:, None, None]
    valid = (delta >= 0) & (delta <= steps) & ((blk > 0) | (kbi >= C))
    bias = bias_hb[:, t5_bucket(delta * dilation)].astype(jnp.float32)
    logits = logits + bias[None, :, None, None]
    logits = jnp.where(valid[None, None, None], logits, NEG_INF)
    m = jnp.max(logits, axis=-1, keepdims=True)
    p = jnp.exp(logits - m)
    den = jnp.sum(p, axis=-1, keepdims=True)
    out = jnp.einsum('bhrnqk,bhrnkd->bhrnqd', (p / den).astype(v.dtype), vb)
    lse = (m + jnp.log(den))[..., 0]

    def from_sub(t):
        t = t[:, :, :, :L]
        return jnp.moveaxis(t, 2, 3).reshape((B, H, S) + t.shape[4:])

    return from_sub(out.reshape(B, H, dilation, Lp, Dh)), from_sub(lse.reshape(B, H, dilation, Lp))


def mixer_dilated(q, k, v, bias_hb):
    results = [dilated_window_attention(q, k, v, bias_hb, w, d) for (w, d) in DILATED_PATTERNS]
    outs = jnp.stack([r[0] for r in results], axis=0)
    lses = jnp.stack([r[1] for r in results], axis=0)
    wts = jax.nn.softmax(lses, axis=0)
    return jnp.einsum('pbhs,pbhsd->bhsd', wts.astype(outs.dtype), outs)


def mixer_sgu(z, ln_g, w_s, b_s):
    B, S, _ = z.shape
    z = jax.nn.gelu(z)
    u, vv = jnp.split(z, 2, axis=-1)
    vf = vv.astype(jnp.float32)
    mu = jnp.mean(vf, axis=-1, keepdims=True)
    var = jnp.mean(jnp.square(vf - mu), axis=-1, keepdims=True)
    vv = ((vf - mu) * lax.rsqrt(var + SGU_LN_EPS) * ln_g.astype(jnp.float32)).astype(z.dtype)
    nc = S // SGU_CHUNK
    vv = vv.reshape(B, nc, SGU_CHUNK, N_HEADS_GROUP, HEAD_DIM)
    w = jnp.tril(w_s)
    mixed = jnp.einsum('gts,bnsgc->bntgc', w, vv) + b_s.T[None, None, :, :, None]
    return u * mixed.reshape(B, S, GROUP_WIDTH)


def mixer_moba(q, k, v, bias_hb):
    B, H, S, Dh = q.shape
    BS = MOBA_BLOCK
    nblk = -(-S // BS)
    Sp = nblk * BS
    pad = ((0, 0), (0, 0), (0, Sp - S), (0, 0))
    qp, kp, vp = jnp.pad(q, pad), jnp.pad(k, pad), jnp.pad(v, pad)
    kb = kp.reshape(B, H, nblk, BS, Dh)
    vb = vp.reshape(B, H, nblk, BS, Dh)
    kbar = jnp.mean(kb.astype(jnp.float32), axis=3)
    topk = min(MOBA_TOPK, nblk)
    QC = MOBA_Q_CHUNK
    n_chunks = Sp // QC
    scale = Dh ** -0.5
    gather_blocks = jax.vmap(jax.vmap(lambda blocks, ids: blocks[ids]))
    head_bias = jax.vmap(lambda tbl, bk: tbl[bk], in_axes=(0, 1), out_axes=1)
    key_off = jnp.arange(BS)

    def chunk(c):
        s0 = c * QC
        ob = s0 // BS
        qc = lax.dynamic_slice_in_dim(qp, s0, QC, axis=2)
        qpos = s0 + jnp.arange(QC)
        gate = jnp.einsum('bhqd,bhnd->bhqn', qc.astype(jnp.float32), kbar)
        gate = jnp.where(jnp.arange(nblk) < ob, gate, NEG_INF)
        _, idx = lax.top_k(gate, topk)
        sel_valid = idx < ob
        ksel = gather_blocks(kb, idx)
        vsel = gather_blocks(vb, idx)
        kpos_sel = idx[..., None] * BS + key_off
        dist_sel = qpos[None, None, :, None, None] - kpos_sel
        l_sel = jnp.einsum('bhqd,bhqjkd->bhqjk', qc, ksel).astype(jnp.float32) * scale
        l_sel = l_sel + head_bias(bias_hb, t5_bucket(dist_sel)).astype(jnp.float32)
        l_sel = jnp.where(sel_valid[..., None], l_sel, NEG_INF)
        kown = lax.dynamic_index_in_dim(kb, ob, axis=2, keepdims=False)
        vown = lax.dynamic_index_in_dim(vb, ob, axis=2, keepdims=False)
        dist_own = qpos[:, None] - (ob * BS + key_off)[None, :]
        l_own = jnp.einsum('bhqd,bhkd->bhqk', qc, kown).astype(jnp.float32) * scale
        l_own = l_own + bias_hb[:, t5_bucket(dist_own)].astype(jnp.float32)[None]
        l_own = jnp.where((dist_own >= 0)[None, None], l_own, NEG_INF)
        logits = jnp.concatenate([l_sel.reshape(B, H, QC, topk * BS), l_own], axis=-1)
        p = jax.nn.softmax(logits, axis=-1).astype(v.dtype)
        p_sel = p[..., :topk * BS].reshape(B, H, QC, topk, BS)
        p_own = p[..., topk * BS:]
        return (jnp.einsum('bhqjk,bhqjkd->bhqd', p_sel, vsel)
                + jnp.einsum('bhqk,bhkd->bhqd', p_own, vown))

    outs = lax.map(chunk, jnp.arange(n_chunks))
    return outs.transpose(1, 2, 0, 3, 4).reshape(B, H, Sp, Dh)[:, :, :S]


def token_shift(y, mu):
    y_prev = jnp.pad(y, ((0, 0), (1, 0), (0, 0)))[:, :-1]
    return y + (y_prev - y) * mu


def rwkv7_scan(r, w, k, v, a, b):
    def step(state, inp):
        r_t, w_t, k_t, v_t, a_t, b_t = inp
        sa = jnp.einsum('bhvk,bhk->bhv', state, a_t)
        state = (state * w_t[:, :, None, :] + sa[..., None] * b_t[:, :, None, :]
                 + v_t[..., None] * k_t[:, :, None, :])
        return state, jnp.einsum('bhvk,bhk->bhv', state, r_t)

    B, S, H, N = r.shape
    xs = tuple(jnp.moveaxis(t, 1, 0) for t in (r, w, k, v, a, b))
    _, ys = lax.scan(step, jnp.zeros((B, H, N, N), jnp.float32), xs)
    return jnp.moveaxis(ys, 0, 1)


def mixer_rwkv7(p, mu, w0, w2, a0, a2, g2, k_k, k_a, r_k, lnx_g, lnx_b):
    B, S, _ = p.shape
    H, N = N_HEADS_GROUP, HEAD_DIM
    p = token_shift(p, mu)
    r, wd, k, v, ad, gd = jnp.split(p, RWKV_SPLITS, axis=-1)
    w_log = -jax.nn.softplus(-(w0 + jnp.tanh(wd) @ w2).astype(jnp.float32)) - 0.5
    decay = jnp.exp(-jnp.exp(w_log))
    a = jax.nn.sigmoid(a0 + ad @ a2)
    g = jax.nn.sigmoid(gd) @ g2
    heads = lambda t: t.reshape(B, S, H, N).astype(jnp.float32)
    kk = heads(k * k_k)
    kk = kk / jnp.maximum(jnp.sqrt(jnp.sum(kk * kk, axis=-1, keepdims=True)), 1e-12)
    k = k * (1 + (a - 1) * k_a)
    rh, kh, vh, ah = heads(r), heads(k), heads(v), heads(a)
    y = rwkv7_scan(rh, heads(decay), kh, vh, -kk, kk * ah)
    ym = jnp.mean(y, axis=-1, keepdims=True)
    yv = jnp.mean(jnp.square(y - ym), axis=-1, keepdims=True)
    y = ((y - ym) * lax.rsqrt(yv + RWKV_LN_EPS)).reshape(B, S, GROUP_WIDTH)
    y = y * lnx_g.astype(jnp.float32) + lnx_b.astype(jnp.float32)
    bonus = jnp.sum(rh * kh * r_k.astype(jnp.float32), axis=-1, keepdims=True) * vh
    y = y + bonus.reshape(B, S, GROUP_WIDTH)
    return (y * g.astype(jnp.float32)).astype(p.dtype)


def swiglu(h, w_gate, w_up, w_down):
    return (jax.nn.silu(h @ w_gate) * (h @ w_up)) @ w_down


def setup_inputs(seed: int = 0) -> dict:
    key = jax.random.key(seed)
    ks = iter(jax.random.split(key, 32))
    nrm = lambda shape, scale: jax.random.normal(next(ks), shape, jnp.float32) * scale
    uni = lambda shape, lo, hi: jax.random.uniform(next(ks), shape, jnp.float32, lo, hi)
    H, N, GW, T = N_HEADS_GROUP, HEAD_DIM, GROUP_WIDTH, SGU_CHUNK
    return {
        'x': nrm((BATCH, SEQ, D_MODEL), 1.0),
        'norm_mix_g': 1.0 + nrm((DEPTH, D_MODEL), 0.02),
        'w_in': nrm((DEPTH, D_MODEL, D_IN_PROJ), D_MODEL ** -0.5),
        'pos_bias': nrm((NUM_BUCKETS, 2 * H), 0.3),
        'sgu_ln_g': 1.0 + nrm((DEPTH, GW), 0.02),
        'sgu_w': nrm((DEPTH, H, T, T), T ** -0.5),
        'sgu_b': 1.0 + nrm((DEPTH, H, T), 0.1),
        'rwkv_mu': uni((DEPTH, RWKV_WIDTH), 0.0, 1.0),
        'rwkv_w0': uni((DEPTH, GW), -6.0, -1.0),
        'rwkv_w2': nrm((DEPTH, RWKV_W_LORA, GW), 0.1),
        'rwkv_a0': nrm((DEPTH, GW), 0.1),
        'rwkv_a2': nrm((DEPTH, RWKV_A_LORA, GW), 0.1),
        'rwkv_g2': nrm((DEPTH, RWKV_G_LORA, GW), RWKV_G_LORA ** -0.5),
        'rwkv_k_k': 0.85 + nrm((DEPTH, GW), 0.05),
        'rwkv_k_a': 1.0 + nrm((DEPTH, GW), 0.05),
        'rwkv_r_k': nrm((DEPTH, H, N), 0.1),
        'rwkv_lnx_g': 1.0 + nrm((DEPTH, GW), 0.02),
        'rwkv_lnx_b': nrm((DEPTH, GW), 0.02),
        'branch_norm_g': 1.0 + nrm((DEPTH, D_MIX), 0.02),
        'w_out': nrm((DEPTH, D_MIX, D_MODEL), D_MIX ** -0.5),
        'norm_ffn_g': 1.0 + nrm((DEPTH, D_MODEL), 0.02),
        'w_gate': nrm((DEPTH, D_MODEL, D_FF), D_MODEL ** -0.5),
        'w_up': nrm((DEPTH, D_MODEL, D_FF), D_MODEL ** -0.5),
        'w_down': nrm((DEPTH, D_FF, D_MODEL), D_FF ** -0.5),
        'norm_final_g': 1.0 + nrm((D_MODEL,), 0.02),
    }


def reference(x, norm_mix_g, w_in, pos_bias, sgu_ln_g, sgu_w, sgu_b, rwkv_mu, rwkv_w0,
              rwkv_w2, rwkv_a0, rwkv_a2, rwkv_g2, rwkv_k_k, rwkv_k_a, rwkv_r_k, rwkv_lnx_g,
              rwkv_lnx_b, branch_norm_g, w_out, norm_ffn_g, w_gate, w_up, w_down, norm_final_g):
    B, S, _ = x.shape
    bias_a = pos_bias[:, :N_HEADS_GROUP].T
    bias_c = pos_bias[:, N_HEADS_GROUP:].T
    for l in range(DEPTH):
        h = rmsnorm(x, norm_mix_g[l])
        proj = h @ w_in[l]
        pa, pb, pc, pd = jnp.split(proj, IN_SPLITS, axis=-1)
        ya = merge_heads(mixer_dilated(*split_qkv_heads(pa), bias_a))
        yb = mixer_sgu(pb, sgu_ln_g[l], sgu_w[l], sgu_b[l])
        yc = merge_heads(mixer_moba(*split_qkv_heads(pc), bias_c))
        yd = mixer_rwkv7(pd, rwkv_mu[l], rwkv_w0[l], rwkv_w2[l], rwkv_a0[l], rwkv_a2[l],
                         rwkv_g2[l], rwkv_k_k[l], rwkv_k_a[l], rwkv_r_k[l],
                         rwkv_lnx_g[l], rwkv_lnx_b[l])
        ycat = jnp.stack([ya, yb, yc, yd], axis=2)
        ycat = rmsnorm(ycat, branch_norm_g[l].reshape(N_MIXERS, GROUP_WIDTH))
        x = x + ycat.reshape(B, S, D_MIX) @ w_out[l]
        h = rmsnorm(x, norm_ffn_g[l])
        x = x + swiglu(h, w_gate[l], w_up[l], w_down[l])
    return rmsnorm(x, norm_final_g)
```

```python
import math
import numpy as np
import concourse.bass as bass
import concourse.mybir as mybir
from concourse.bass_utils import run_bass_kernel_spmd

F32 = mybir.dt.float32
BF16 = mybir.dt.bfloat16
AF = mybir.ActivationFunctionType
ALU = mybir.AluOpType
AX = mybir.AxisListType

S_ = 2048
D_ = 2048
DEPTH = 2
NCORES = 8
GW = 512
DIN = 6080
DFF = 5632
NKC = 16
LU = 2560
TW = 2432
BIG = 30000.0


class Sched:
    def __init__(self, nc):
        self.nc = nc
        self.eng = {"pe": nc.tensor, "dve": nc.vector, "act": nc.scalar, "pool": nc.gpsimd, "sp": nc.sync}
        self.sem = {k: nc.alloc_semaphore(f"se_{k}") for k in self.eng}
        self.cnt = {k: 0 for k in self.eng}
        self.dsem = {}
        self.dtot = {}
        self.waited = {k: {} for k in self.eng}
        self.last_w = {}
        self.readers = {}
        self.nbuf = 0
        self.rot = {}

    def sb(self, name, shape, dt):
        return self.nc.sbuf_tensor(name, list(shape), dt).__enter__()

    def _deps(self, r, w):
        evs = []
        for t in r:
            if t in self.last_w:
                evs.append(self.last_w[t])
        for t in w:
            if t in self.last_w:
                evs.append(self.last_w[t])
            evs.extend(self.readers.get(t, ()))
        return evs

    def _wait(self, e, evs):
        need = {}
        for (key, val) in evs:
            if key == e and e == "pe":
                continue
            if val > need.get(key, 0):
                need[key] = val
        for key, val in need.items():
            if self.waited[e].get(key, 0) >= val:
                continue
            if key in self.sem:
                sem = self.sem[key]
            else:
                sem = self.dsem[key]
                val = max(val, self.dtot[key])
            self.eng[e].wait_ge(sem, val)
            self.waited[e][key] = val

    def _commit(self, ev, r, w):
        for t in w:
            self.last_w[t] = ev
            self.readers[t] = []
        for t in r:
            self.readers.setdefault(t, []).append(ev)

    def op(self, e, fn, r=(), w=()):
        self._wait(e, self._deps(r, w))
        inst = fn(self.eng[e])
        inst.then_inc(self.sem[e], 1)
        self.cnt[e] += 1
        self._commit((e, self.cnt[e]), r, w)

    def dma(self, q, key, out, in_, r=(), w=(), **kw):
        if key not in self.dsem:
            self.dsem[key] = self.nc.alloc_semaphore(f"sd_{key}")
            self.dtot[key] = 0
        self._wait(q, self._deps(r, w))
        self.eng[q].dma_start(out=out, in_=in_, **kw).then_inc(self.dsem[key], 16)
        self.dtot[key] += 16
        self._commit((key, self.dtot[key]), r, w)

    def barrier(self):
        evs = [(k, v) for k, v in self.cnt.items() if v > 0]
        evs += [(k, v) for k, v in self.dtot.items() if v > 0]
        for e in self.eng:
            self._wait(e, [ev for ev in evs if ev[0] != e])
        self.last_w = {}
        self.readers = {}

    def rr(self, name, n):
        i = self.rot.get(name, 0)
        self.rot[name] = (i + 1) % n
        return i


def t5_bucket_np(dist):
    dist = np.maximum(dist, 0)
    d = np.maximum(dist, 1).astype(np.float32)
    large = 16 + (np.log(d / np.float32(16)) / np.float32(math.log(2048 / 16)) * np.float32(16)).astype(np.int32)
    large = np.minimum(large, 31)
    return np.where(dist < 16, dist, large)


def host_constants():
    c = {}
    c["c_ident"] = np.eye(128, dtype=np.float32)
    d = np.arange(LU) - 511
    bk = t5_bucket_np(d)
    cntA = np.zeros(LU, np.float32)
    for (wdw, dil) in ((128, 1), (512, 4), (2048, 16)):
        cntA += ((d >= 0) & (d % dil == 0) & (d <= wdw)).astype(np.float32)
    ohA = np.zeros((32, LU), np.float32)
    ohC = np.zeros((32, LU), np.float32)
    ohA[bk, np.arange(LU)] = cntA
    ohC[bk, np.arange(LU)] = (d >= 0).astype(np.float32)
    c["c_ohA"] = ohA
    c["c_ohC"] = ohC
    s = np.arange(128)
    c["c_tril"] = (s[:, None] <= s[None, :]).astype(np.float32)
    ohb = np.zeros((8, 16, 128), np.float32)
    for kb in range(16):
        ohb[kb // 2, kb, :] = 1.0
    c["c_ohb"] = ohb.reshape(8, 16 * 128)
    i = np.arange(64)
    m = np.zeros((64, 128), np.float32)
    m[:, :64] = (i[:, None] <= i[None, :])
    m[:, 64:] = (i[:, None] < i[None, :])
    c["c_rmask"] = m
    c["c_rmaskT"] = (i[:, None] > i[None, :]).astype(np.float32)
    p_ = np.arange(128)
    par, ii = p_ // 64, p_ % 64
    c["c_mi2"] = (ii[:, None] <= i[None, :]).astype(np.float32)
    c["c_msbd"] = ((par[:, None] == par[None, :]) & (ii[:, None] < ii[None, :])).astype(np.float32)
    c["c_msbdT"] = ((par[:, None] == par[None, :]) & (ii[:, None] > ii[None, :])).astype(np.float32)
    rs = np.ones((1, S_), np.float32)
    rs[0, ::64] = 0.0
    c["c_reset"] = rs
    return c


CONST_SHAPES = {"c_ident": [128, 128], "c_ohA": [32, LU], "c_ohC": [32, LU], "c_tril": [128, 128],
                "c_ohb": [8, 2048], "c_rmask": [64, 128], "c_rmaskT": [64, 64], "c_reset": [1, S_],
                "c_mi2": [128, 64], "c_msbd": [128, 128], "c_msbdT": [128, 128]}

IN_SHAPES = {
    "x": [S_, D_], "norm_mix_g": [DEPTH, D_], "w_in": [DEPTH, D_, DIN], "pos_bias": [32, 16],
    "sgu_ln_g": [DEPTH, GW], "sgu_wT": [DEPTH, 8, 128, 128], "sgu_bT": [DEPTH, 128, 8],
    "rwkv_mu": [DEPTH, 1984], "rwkv_w0": [DEPTH, GW], "rwkv_w2": [DEPTH, 96, GW], "rwkv_a0": [DEPTH, GW],
    "rwkv_a2": [DEPTH, 96, GW], "rwkv_g2": [DEPTH, 256, GW], "rwkv_k_k": [DEPTH, GW], "rwkv_k_a": [DEPTH, GW],
    "rwkv_r_k": [DEPTH, GW], "rwkv_lnx_g": [DEPTH, GW], "rwkv_lnx_b": [DEPTH, GW],
    "branch_norm_g": [DEPTH, D_], "w_out": [DEPTH, D_, D_], "norm_ffn_g": [DEPTH, D_],
    "w_gate": [DEPTH, D_, DFF], "w_up": [DEPTH, D_, DFF], "w_down": [DEPTH, DFF, D_], "norm_final_g": [1, D_],
}


_UID = [0]
_PROG = {}


def sbt(es, nc, name, shape, dt):
    _UID[0] += 1
    return es.enter_context(nc.sbuf_tensor(f"{name}_u{_UID[0]}", list(shape), dt))


class Ctx:
    pass


def build(debug=False, stages=("pre", "in", "A", "B", "C", "D", "out", "ffn", "fin"), depth=DEPTH):
    from contextlib import ExitStack
    nc = bass.Bass("TRN2", target_bir_lowering=False)
    I = {}
    for k, shp in list(IN_SHAPES.items()) + list(CONST_SHAPES.items()):
        I[k] = nc.dram_tensor(k, list(shp), F32, kind="ExternalInput").ap()
    out = nc.dram_tensor("out", [S_, D_], F32, kind="ExternalOutput").ap()
    skind = "ExternalOutput" if debug else "Internal"

    def scr(name, shape, dt):
        return nc.dram_tensor(name, list(shape), dt, kind=skind).ap()

    G = Ctx()
    G.nc = nc
    G.I = I
    G.out = out
    G.xres = scr("xres", [S_, D_], F32)
    G.qkA = scr("qkA", [1024, S_], BF16)
    G.vA = scr("vA", [S_, GW], BF16)
    G.qkC = scr("qkC", [1024, S_], BF16)
    G.vC = scr("vC", [S_, GW], BF16)
    G.pb = scr("pb", [S_, 1024], F32)
    G.pdT = scr("pdT", [1984, S_], F32)
    G.ycat = scr("ycat", [S_, D_], F32)
    G.actT = scr("actT", [4, 128, (DFF // 128) * 512], BF16)
    G.u2 = scr("u2", [2, 8 * LU], BF16)
    G.uA = scr("uA", [8, 128, LU], BF16)
    G.uC = scr("uC", [8, 128, LU], BF16)
    G.ydr = scr("ydr", [S_, GW], F32)
    G.vtk = scr("vtk", [S_, GW], F32)
    S = Sched(nc)
    G.S = S
    G.ps = [nc.psum_tensor(f"ps{i}", [128, 512], F32).__enter__() for i in range(8)]

    with ExitStack() as es0:
        G.identb = sbt(es0, nc, "identb", [128, 128], BF16)
        G.identf = sbt(es0, nc, "identf", [128, 128], F32)
        S.dma("pool", "c0", G.identb[:], I["c_ident"], w=["identb"])
        S.dma("sp", "c1", G.identf[:], I["c_ident"], w=["identf"])
        if "pre" in stages:
            stage_pre(G)
        S.barrier()
        for l in range(depth):
            xsrc = I["x"] if l == 0 else G.xres
            if "in" in stages:
                stage_inproj(G, l, xsrc)
                S.barrier()
            if "A" in stages:
                stage_attn(G, l, moba=False)
                S.barrier()
            if "B" in stages:
                stage_sgu(G, l)
                S.barrier()
            if "C" in stages:
                stage_attn(G, l, moba=True)
                S.barrier()
            if "D" in stages:
                stage_rwkv(G, l)
                S.barrier()
            if "out" in stages:
                stage_outproj(G, l, xsrc)
                S.barrier()
                with ExitStack() as esl:
                    h2T = sbt(esl, nc, "h2T", [128, NKC, S_], BF16)
                    if "ffn" in stages:
                        stage_ffn_up(G, l, h2T)
                S.barrier()
                if "ffn" in stages:
                    stage_ffn_down(G, l)
                    S.barrier()
        if "fin" in stages:
            stage_final(G)
        S.barrier()
    _PROG['G'] = G
    return nc


def stage_pre(G):
    from contextlib import ExitStack
    nc, S, I = G.nc, G.S, G.I
    with ExitStack() as es:
        pbias = sbt(es, nc, "pbias", [32, 16], F32)
        eb = sbt(es, nc, "eb", [32, 16], F32)
        oh = sbt(es, nc, "oh", [32, 2, LU], F32)
        ub = sbt(es, nc, "ub", [8, 2, LU], BF16)
        S.dma("sp", "c1", pbias[:], I["pos_bias"], w=["pbias"])
        S.dma("sp", "c1", oh[:, 0, :], I["c_ohA"], w=["oh0"])
        S.dma("sp", "c1", oh[:, 1, :], I["c_ohC"], w=["oh1"])
        S.op("act", lambda e: e.activation(out=eb[:], in_=pbias[:], func=AF.Exp), r=["pbias"], w=["eb"])
        for m in range(2):
            for ch in range(LU // 512):
                b = 4 + S.rr("pp", 4)
                S.op("pe", lambda e: e.matmul(G.ps[b][0:8, :], lhsT=eb[:, m * 8:(m + 1) * 8],
                                              rhs=oh[:, m, ch * 512:(ch + 1) * 512], start=True, stop=True),
                     r=["eb", f"oh{m}"], w=[f"ps{b}"])
                S.op("dve", lambda e: e.tensor_copy(out=ub[:, m, ch * 512:(ch + 1) * 512], in_=G.ps[b][0:8, :]),
                     r=[f"ps{b}"], w=[f"ub{m}"])
        big = sbt(es, nc, "ubig", [128, 8 * LU], BF16)
        for m, dst in ((0, G.uA), (1, G.uC)):
            S.dma("sp", "c1", G.u2[m:m + 1, :].rearrange("o (h i) -> (o h) i", h=8), ub[:, m, :], r=[f"ub{m}"], w=["u2"])
            S.dma("sp", "c1", big[:], G.u2[m:m + 1, :].partition_broadcast(128), r=["u2"], w=["ubig"])
            S.dma("sp", "c1", dst.rearrange("h r i -> r h i"), big[:].rearrange("p (h i) -> p h i", h=8), r=["ubig"], w=["uAC"])


def rms_to_T(G, es, src, g_ap, hT, hname, tag, after_chunk=None):
    nc, S = G.nc, G.S
    gb = sbt(es, nc, tag + "gb", [128, D_], F32)
    xt = [sbt(es, nc, tag + f"xt{i}", [128, D_], F32) for i in range(4)]
    hb2 = [sbt(es, nc, tag + f"hb{i}", [128, D_], BF16) for i in range(2)]
    st = sbt(es, nc, tag + "st", [128, 64], F32)
    S.dma("sp", "c1", gb[:], g_ap.partition_broadcast(128), w=[tag + "gb"])

    def load_x(t_):
        S.dma("sp", f"x{t_ % 4}", xt[t_ % 4][:], src[t_ * 128:(t_ + 1) * 128, :], w=[tag + f"xt{t_ % 4}"])

    for t_ in range(4):
        load_x(t_)
    for tt in range(16):
        i = tt % 4
        hb = {i: hb2[tt % 2]}
        S.op("act", lambda e: e.activation(out=hb[i][:], in_=xt[i][:], func=AF.Square,
                                           accum_out=st[:, 4 * tt:4 * tt + 1]),
             r=[tag + f"xt{i}"], w=[tag + f"hb{tt % 2}", tag + f"st{tt}"])
        S.op("dve", lambda e: e.tensor_scalar(out=st[:, 4 * tt + 1:4 * tt + 2], in0=st[:, 4 * tt:4 * tt + 1],
                                              scalar1=1.0 / D_, scalar2=1e-6, op0=ALU.mult, op1=ALU.add),
             r=[tag + f"st{tt}"], w=[tag + f"st{tt}"])
        S.op("act", lambda e: e.activation(out=st[:, 4 * tt + 2:4 * tt + 3], in_=st[:, 4 * tt + 1:4 * tt + 2],
                                           func=AF.Sqrt), r=[tag + f"st{tt}"], w=[tag + f"st{tt}"])
        S.op("dve", lambda e: e.reciprocal(out=st[:, 4 * tt + 3:4 * tt + 4], in_=st[:, 4 * tt + 2:4 * tt + 3]),
             r=[tag + f"st{tt}"], w=[tag + f"st{tt}"])
        S.op("act", lambda e: e.activation(out=xt[i][:], in_=xt[i][:], func=AF.Identity, scale=st[:, 4 * tt + 3:4 * tt + 4]),
             r=[tag + f"xt{i}", tag + f"st{tt}"], w=[tag + f"xt{i}"])
        S.op("dve" if tt % 2 == 0 else "pool",
             lambda e: e.tensor_tensor(out=hb[i][:], in0=xt[i][:], in1=gb[:], op=ALU.mult),
             r=[tag + f"xt{i}", tag + "gb"], w=[tag + f"hb{tt % 2}"])
        transpose_rows(G, hb[i], tag + f"hb{tt % 2}", hT, f"{hname}{tt // 4}", tt)
        if tt + 4 < 16:
            load_x(tt + 4)
        if after_chunk is not None and tt % 4 == 3:
            after_chunk(tt // 4)


def transpose_rows(G, hb, hbname, hT, hname, tt):
    S = G.S
    for half in range(2):
        b = S.rr("tp", 4)
        pv = G.ps[b][:].bitcast(BF16)
        for k8 in range(8):
            kc = half * 8 + k8
            S.op("pe", lambda e: e.transpose(out=pv[:, k8 * 128:(k8 + 1) * 128], in_=hb[:, kc * 128:(kc + 1) * 128],
                                             identity=G.identb[:]),
                 r=[hbname, "identb"], w=[f"ps{b}"])
        eng = "act" if half == 0 else "dve"
        dst = hT[:, half * 8:(half + 1) * 8, tt * 128:(tt + 1) * 128]
        srcv = pv.rearrange("p (a b) -> p a b", a=8)
        if eng == "act":
            S.op("act", lambda e: e.copy(out=dst, in_=srcv), r=[f"ps{b}"], w=[hname])
        else:
            S.op("dve", lambda e: e.tensor_copy(out=dst, in_=srcv), r=[f"ps{b}"], w=[hname])


def load_w(G, wt, wname, src3, ncols, nk=NKC):
    G.S.dma("pool", wname, wt[:, 0:nk, 0:ncols], src3, w=[wname])


def stage_inproj(G, l, xsrc):
    from contextlib import ExitStack
    nc, S, I = G.nc, G.S, G.I
    groups = [
        ("F", 0, 512, G.qkA, 0, BF16), ("F", 512, 512, G.qkA, 512, BF16), ("T", 1024, 512, G.vA, 0, BF16),
        ("T", 1536, 512, G.pb, 0, F32), ("T", 2048, 512, G.pb, 512, F32),
        ("F", 2560, 512, G.qkC, 0, BF16), ("F", 3072, 512, G.qkC, 512, BF16), ("T", 3584, 512, G.vC, 0, BF16),
        ("F", 4096, 512, G.pdT, 0, F32), ("F", 4608, 96, G.pdT, 512, F32), ("F", 4704, 512, G.pdT, 608, F32),
        ("F", 5216, 512, G.pdT, 1120, F32), ("F", 5728, 96, G.pdT, 1632, F32), ("F", 5824, 256, G.pdT, 1728, F32),
    ]
    with ExitStack() as es:
        hT = sbt(es, nc, "hT", [128, NKC, S_], BF16)
        wt = [sbt(es, nc, f"wt{i}", [128, NKC, 512], BF16) for i in range(2)]
        sg32 = [sbt(es, nc, f"sg32_{i}", [128, 512], F32) for i in range(3)]
        sg16 = [sbt(es, nc, f"sg16_{i}", [128, 512], BF16) for i in range(3)]
        w3 = I["w_in"][l].rearrange("(kc p) c -> p kc c", p=128)

        def issue(gi):
            mode, c0, ncol, dst, d0, dt = groups[gi]
            load_w(G, wt[gi % 2], f"wt{gi % 2}", w3[:, :, c0:c0 + ncol], ncol)

        evc = [0]

        def emit_tile(gi, a, m, tc):
            mode, c0, ncol, dst, d0, dt = groups[gi]
            w = wt[gi % 2]
            wn = f"wt{gi % 2}"
            b = 4 + S.rr("pp", 4)
            for kc in range(NKC):
                if mode == "F":
                    S.op("pe", lambda e: e.matmul(G.ps[b][0:m, :], lhsT=w[:, kc, a * 128:a * 128 + m],
                                                  rhs=hT[:, kc, tc * 512:(tc + 1) * 512],
                                                  start=(kc == 0), stop=(kc == NKC - 1)),
                         r=[wn, f"hT{tc}"], w=[f"ps{b}"])
                else:
                    S.op("pe", lambda e: e.matmul(G.ps[b][:, 0:ncol], lhsT=hT[:, kc, a * 128:(a + 1) * 128],
                                                  rhs=w[:, kc, 0:ncol], start=(kc == 0), stop=(kc == NKC - 1)),
                         r=[wn, f"hT{a // 4}"], w=[f"ps{b}"])
            si = S.rr("sg" + ("32" if dt == F32 else "16"), 3)
            sg = sg32[si] if dt == F32 else sg16[si]
            sgn = ("sg32_" if dt == F32 else "sg16_") + str(si)
            ncl = 512 if mode == "F" else ncol
            evc[0] += 1
            if evc[0] % 2 == 0:
                S.op("act", lambda e: e.copy(out=sg[0:m, 0:ncl], in_=G.ps[b][0:m, 0:ncl]), r=[f"ps{b}"], w=[sgn])
            else:
                S.op("dve", lambda e: e.tensor_copy(out=sg[0:m, 0:ncl], in_=G.ps[b][0:m, 0:ncl]), r=[f"ps{b}"], w=[sgn])
            if mode == "F":
                dap = dst[d0 + a * 128:d0 + a * 128 + m, tc * 512:(tc + 1) * 512]
            else:
                dap = dst[a * 128:(a + 1) * 128, d0:d0 + ncol]
            S.dma("sp", sgn, dap, sg[0:m, 0:ncl], r=[sgn])

        def group_tiles(gi):
            mode, c0, ncol, dst, d0, dt = groups[gi]
            tiles = []
            if mode == "F":
                for mt in range((ncol + 127) // 128):
                    for tc in range(4):
                        tiles.append((mt, min(128, ncol - mt * 128), tc))
            else:
                for tt in range(16):
                    tiles.append((tt, 128, 0))
            return tiles

        issue(0)
        issue(1)

        def after_chunk(tc):
            for gi in (0, 1):
                for (a, m, tcc) in group_tiles(gi):
                    if tcc == tc:
                        emit_tile(gi, a, m, tc)

        rms_to_T(G, es, xsrc, I["norm_mix_g"][l:l + 1, :], hT, "hT", "n", after_chunk=after_chunk)
        issue(2)
        for gi in range(2, len(groups)):
            if gi + 1 < len(groups):
                issue(gi + 1)
            for (a, m, tc) in group_tiles(gi):
                emit_tile(gi, a, m, tc)


def stage_attn(G, l, moba):
    from contextlib import ExitStack
    nc, S, I = G.nc, G.S, G.I
    qk = G.qkC if moba else G.qkA
    vsrc = G.vC if moba else G.vA
    usrc = G.uC if moba else G.uA
    ycol0 = 1024 if moba else 0
    tg = "C" if moba else "A"
    with ExitStack() as es:
        qT = [sbt(es, nc, f"qT{i}", [64, S_], BF16) for i in range(2)]
        kT = [sbt(es, nc, f"kT{i}", [64, S_], BF16) for i in range(2)]
        va = [sbt(es, nc, f"va{i}", [128, 16, 65], BF16) for i in range(2)]
        Tm = [sbt(es, nc, f"Tm{i}", [128, TW], BF16) for i in range(2)]
        Pe = [sbt(es, nc, f"Pe{i}", [128, 512], BF16) for i in range(3)]
        Pb = [sbt(es, nc, f"Pb{i}", [128, 16, 512], BF16) for i in range(2)]
        YH = [sbt(es, nc, f"YH{i}", [128, 16, 64], F32) for i in range(2)]
        rd = sbt(es, nc, "rd", [128, 16], F32)
        if moba:
            ohb = sbt(es, nc, "ohb", [8, 16, 128], BF16)
            kb32 = sbt(es, nc, "kb32", [64, 8], F32)
            kbb = sbt(es, nc, "kbb", [64, 8], BF16)
            gm = sbt(es, nc, "gm", [128, 16, 8], F32)
            mx = sbt(es, nc, "mx", [128, 16, 8], F32)
            nm = sbt(es, nc, "nm", [128, 16, 8], BF16)
            nmT = sbt(es, nc, "nmT", [8, 1024], BF16)
            S.dma("pool", "c0", ohb[:], I["c_ohb"].rearrange("n (a b) -> n a b", a=16), w=["ohb"])
        for i in range(2):
            S.op("dve", lambda e: e.memset(va[i][:, :, 64:65], 1.0), w=[f"va{i}"])

        def load_head(h):
            i = h % 2
            S.dma("sp", f"q{i}", qT[i][:], qk[h * 64:(h + 1) * 64, :], w=[f"qT{i}"])
            S.dma("sp", f"k{i}", kT[i][:], qk[512 + h * 64:512 + (h + 1) * 64, :], w=[f"kT{i}"])
            S.dma("sp", f"v{i}", va[i][:, :, 0:64],
                  vsrc.rearrange("(kb p) c -> p kb c", p=128)[:, :, h * 64:(h + 1) * 64], w=[f"va{i}"])
            tsrc = bass.AP(tensor=usrc.tensor, offset=h * 128 * LU + 127, ap=[[LU - 1, 128], [1, TW]])
            S.dma("sp", f"t{i}", Tm[i][:], tsrc, w=[f"Tm{i}"])

        ust = {}

        def s_phase(h, c):
            i = h % 2
            qn, kn, vn, tn, yn = f"qT{i}", f"kT{i}", f"va{i}", f"Tm{i}", f"YH{i}"
            if c == 0 and moba:
                S.op("dve", lambda e: e.tensor_reduce(out=kb32[:], in_=kT[i][:].rearrange("p (n s) -> p n s", n=8),
                                                      axis=AX.X, op=ALU.add), r=[kn], w=["kb32"])
                S.op("dve", lambda e: e.tensor_scalar(out=kbb[:], in0=kb32[:], scalar1=1.0 / 256, scalar2=None,
                                                      op0=ALU.mult), r=["kb32"], w=["kbb"])
                b = S.rr("st", 4)
                for tt in range(16):
                    S.op("pe", lambda e: e.matmul(G.ps[b][:, tt * 8:(tt + 1) * 8], lhsT=qT[i][:, tt * 128:(tt + 1) * 128],
                                                  rhs=kbb[:], start=True, stop=True), r=[qn, "kbb"], w=[f"ps{b}"])
                S.op("dve", lambda e: e.tensor_copy(out=gm[:].rearrange("p a b -> p (a b)"), in_=G.ps[b][:, 0:128]),
                     r=[f"ps{b}"], w=["gm"] + [f"gm{t_}" for t_ in range(8, 16)] + [f"mx{t_}" for t_ in range(8, 16)])
                S.op("dve", lambda e: e.memset(nm[:], 0.0), w=["nm"] + [f"nm{t_}" for t_ in range(8, 16)])
                for tt in range(8, 16):
                    ob = tt // 2
                    S.op("dve", lambda e: e.memset(gm[:, tt, ob:8], -1e30), r=[], w=[f"gm{tt}"])
                for tt in range(8, 16):
                    S.op("dve", lambda e: e.max(out=mx[:, tt, :], in_=gm[:, tt, :]), r=["gm", f"gm{tt}"], w=[f"mx{tt}"])
                for tt in range(8, 16):
                    ob = tt // 2
                    S.op("dve", lambda e: e.tensor_scalar(out=nm[:, tt, 0:ob], in0=gm[:, tt, 0:ob],
                                                          scalar1=mx[:, tt, 2:3], scalar2=-BIG,
                                                          op0=ALU.is_lt, op1=ALU.mult), r=["gm", f"gm{tt}", f"mx{tt}"], w=[f"nm{tt}"])
                b = S.rr("st", 4)
                pv = G.ps[b][:].bitcast(BF16)
                for tt in range(8, 16):
                    S.op("pe", lambda e: e.transpose(out=pv[0:8, (tt - 8) * 128:(tt - 7) * 128], in_=nm[:, tt, :],
                                                     identity=G.identb[:]), r=["nm", f"nm{tt}", "identb"], w=[f"ps{b}"])
                S.op("dve", lambda e: e.tensor_copy(out=nmT[:], in_=pv[0:8, 0:1024]), r=[f"ps{b}"], w=["nmT"])
            pbi = S.rr("pb", 2)
            pbn = f"Pb{pbi}"
            q0s = {}
            for kb in range(4 * c + 4):
                q0 = max(512 * c, 128 * kb)
                ncol = 512 * c + 512 - q0
                q0s[kb] = q0
                b = S.rr("st", 4)
                mm2 = moba and c >= 2
                S.op("pe", lambda e: e.matmul(G.ps[b][:, 0:ncol], lhsT=kT[i][:, kb * 128:(kb + 1) * 128],
                                              rhs=qT[i][:, q0:q0 + ncol], start=True, stop=not mm2),
                     r=[qn, kn], w=[f"ps{b}"])
                if mm2:
                    S.op("pe", lambda e: e.matmul(G.ps[b][:, 0:ncol], lhsT=ohb[:, kb, :],
                                                  rhs=nmT[:, q0 - 1024:q0 - 1024 + ncol], start=False, stop=True),
                         r=["ohb", "nmT"], w=[f"ps{b}"])
                pi = S.rr("pe_", 3)
                S.op("act", lambda e: e.activation(out=Pe[pi][:, 0:ncol], in_=G.ps[b][:, 0:ncol], func=AF.Exp,
                                                   scale=0.125), r=[f"ps{b}"], w=[f"Pe{pi}"])
                j0 = q0 - 128 * kb + 384
                S.op("dve" if kb % 2 == 0 else "pool",
                     lambda e: e.tensor_tensor(out=Pb[pbi][:, kb, 0:ncol], in0=Pe[pi][:, 0:ncol],
                                               in1=Tm[i][:, j0:j0 + ncol], op=ALU.mult),
                     r=[f"Pe{pi}", tn], w=[pbn])
            ust[(h, c)] = (pbi, q0s)

        def pv_phase(h, c):
            i = h % 2
            vn, yn = f"va{i}", f"YH{i}"
            pbi, q0s = ust.pop((h, c))
            pbn = f"Pb{pbi}"
            for qb in range(4 * c, 4 * c + 4):
                b = 4 + S.rr("pvb", 4)
                for kb in range(qb + 1):
                    off = qb * 128 - q0s[kb]
                    S.op("pe", lambda e: e.matmul(G.ps[b][:, 0:65], lhsT=Pb[pbi][:, kb, off:off + 128],
                                                  rhs=va[i][:, kb, :], start=(kb == 0), stop=(kb == qb)),
                         r=[pbn, vn], w=[f"ps{b}"])
                S.op("dve", lambda e: e.reciprocal(out=rd[:, qb:qb + 1], in_=G.ps[b][:, 64:65]),
                     r=[f"ps{b}"], w=[f"rd{qb}"])
                S.op("dve", lambda e: e.tensor_scalar(out=YH[i][:, qb, :], in0=G.ps[b][:, 0:64],
                                                      scalar1=rd[:, qb:qb + 1], scalar2=None, op0=ALU.mult),
                     r=[f"ps{b}", f"rd{qb}"], w=[yn])
            if c == 3:
                S.dma("sp", f"y{i}", G.ycat.rearrange("(qb p) c -> p qb c", p=128)[:, :, ycol0 + h * 64:ycol0 + (h + 1) * 64],
                      YH[i][:], r=[yn])
                if h + 2 < 8:
                    load_head(h + 2)

        load_head(0)
        load_head(1)
        units = [(h, c) for h in range(8) for c in range(4)]
        for u, (h, c) in enumerate(units):
            s_phase(h, c)
            if u > 0:
                pv_phase(*units[u - 1])
        pv_phase(*units[-1])


def lockstep(lists):
    n = max(len(x) for x in lists)
    for si in range(n):
        for x in lists:
            if si < len(x):
                x[si]()


def stage_sgu(G, l):
    from contextlib import ExitStack
    nc, S, I = G.nc, G.S, G.I
    NS = 4
    with ExitStack() as es:
        wsT = sbt(es, nc, "wsT", [128, 8, 128], BF16)
        wsf = sbt(es, nc, "wsf", [128, 8, 128], F32)
        tril = sbt(es, nc, "tril", [128, 128], F32)
        bT = sbt(es, nc, "bT", [128, 8], F32)
        lg = sbt(es, nc, "lg", [128, GW], F32)
        zt = [sbt(es, nc, f"zt{i}", [128, 1024], F32) for i in range(NS)]
        t1s = [sbt(es, nc, f"t1_{i}", [128, 1024], F32) for i in range(NS)]
        t2s = [sbt(es, nc, f"t2_{i}", [128, 1024], F32) for i in range(NS)]
        vns = [sbt(es, nc, f"vn{i}", [128, GW], BF16) for i in range(NS)]
        yos = [sbt(es, nc, f"yo{i}", [128, GW], F32) for i in range(NS)]
        bss = [sbt(es, nc, f"bs{i}", [128, 6], F32) for i in range(NS)]
        mvs = [sbt(es, nc, f"mv{i}", [128, 4], F32) for i in range(NS)]
        S.dma("sp", "c1", wsf[:], I["sgu_wT"][l].rearrange("g s t -> s g t"), w=["wsf"])
        S.dma("sp", "c1", tril[:], I["c_tril"], w=["tril"])
        S.dma("sp", "c1", bT[:], I["sgu_bT"][l], w=["bT"])
        S.dma("sp", "c1", lg[:], I["sgu_ln_g"][l:l + 1, :].partition_broadcast(128), w=["lg"])
        for g in range(8):
            S.op("dve", lambda e: e.tensor_tensor(out=wsT[:, g, :], in0=wsf[:, g, :], in1=tril[:], op=ALU.mult),
                 r=["wsf", "tril"], w=["wsT"])

        def tile_steps(tt, i):
            z, t1, t2, vn, yo, bs, mv = zt[i], t1s[i], t2s[i], vns[i], yos[i], bss[i], mvs[i]
            zn, t1n, t2n, vnn, yon, bsn, mvn = f"zt{i}", f"t1_{i}", f"t2_{i}", f"vn{i}", f"yo{i}", f"bs{i}", f"mv{i}"
            st = []
            st.append(lambda: S.dma("sp", f"x{i}", z[:], G.pb[tt * 128:(tt + 1) * 128, :], w=[zn]))
            st.append(lambda: S.op("act", lambda e: e.activation(out=t1[:], in_=z[:], func=AF.Square), r=[zn], w=[t1n]))
            st.append(lambda: S.op("dve", lambda e: e.tensor_scalar(out=t1[:], in0=t1[:], scalar1=0.044715, scalar2=1.0, op0=ALU.mult,
                                                                    op1=ALU.add), r=[t1n], w=[t1n]))
            st.append(lambda: S.op("pool", lambda e: e.tensor_tensor(out=t1[:], in0=t1[:], in1=z[:], op=ALU.mult), r=[t1n, zn], w=[t1n]))
            st.append(lambda: S.op("act", lambda e: e.activation(out=t2[:], in_=t1[:], func=AF.Sigmoid, scale=1.5957691216057308),
                                   r=[t1n], w=[t2n]))
            st.append(lambda: S.op("dve", lambda e: e.tensor_tensor(out=t2[:], in0=t2[:], in1=z[:], op=ALU.mult), r=[t2n, zn], w=[t2n]))
            st.append(lambda: S.op("dve", lambda e: e.bn_stats(out=bs[:], in_=t2[:, 512:1024]), r=[t2n], w=[bsn]))
            st.append(lambda: S.op("dve", lambda e: e.bn_aggr(out=mv[:, 0:2], in_=bs[:]), r=[bsn], w=[mvn]))
            st.append(lambda: S.op("dve", lambda e: e.tensor_scalar(out=mv[:, 2:3], in0=mv[:, 1:2], scalar1=1e-5, scalar2=None,
                                                                    op0=ALU.add), r=[mvn], w=[mvn]))
            st.append(lambda: S.op("act", lambda e: e.activation(out=mv[:, 2:3], in_=mv[:, 2:3], func=AF.Sqrt), r=[mvn], w=[mvn]))
            st.append(lambda: S.op("dve", lambda e: e.reciprocal(out=mv[:, 3:4], in_=mv[:, 2:3]), r=[mvn], w=[mvn]))
            st.append(lambda: S.op("dve", lambda e: e.tensor_scalar(out=t1[:, 0:512], in0=t2[:, 512:1024], scalar1=mv[:, 0:1],
                                                                    scalar2=mv[:, 3:4], op0=ALU.subtract, op1=ALU.mult),
                                   r=[t2n, mvn], w=[t1n]))
            st.append(lambda: S.op("pool", lambda e: e.tensor_tensor(out=vn[:], in0=t1[:, 0:512], in1=lg[:], op=ALU.mult),
                                   r=[t1n, "lg"], w=[vnn]))

            def mm():
                b = S.rr("st", 4)
                for g in range(8):
                    S.op("pe", lambda e: e.matmul(G.ps[b][:, g * 64:(g + 1) * 64], lhsT=wsT[:, g, :],
                                                  rhs=vn[:, g * 64:(g + 1) * 64], start=True, stop=True),
                         r=["wsT", vnn], w=[f"ps{b}"])
                S.op("dve", lambda e: e.tensor_tensor(out=t1[:, 512:1024].rearrange("p (g c) -> p g c", g=8),
                                                      in0=G.ps[b][:].rearrange("p (g c) -> p g c", g=8),
                                                      in1=bT[:].unsqueeze(2).to_broadcast([128, 8, 64]), op=ALU.add),
                     r=[f"ps{b}", "bT"], w=[t1n])
            st.append(mm)
            st.append(lambda: S.op("pool", lambda e: e.tensor_tensor(out=yo[:], in0=t1[:, 512:1024], in1=t2[:, 0:512], op=ALU.mult),
                                   r=[t1n, t2n], w=[yon]))
            st.append(lambda: S.dma("sp", f"y{i}", G.ycat[tt * 128:(tt + 1) * 128, 512:1024], yo[:], r=[yon]))
            return st

        for t0 in range(0, 16, NS):
            lockstep([tile_steps(t0 + i, i) for i in range(NS)])


def stage_outproj(G, l, xsrc):
    from contextlib import ExitStack
    nc, S, I = G.nc, G.S, G.I
    with ExitStack() as es:
        yT = sbt(es, nc, "yT", [128, NKC, S_], BF16)
        wt = [sbt(es, nc, f"wo{i}", [128, NKC, 512], BF16) for i in range(2)]
        w3 = I["w_out"][l].rearrange("(kc p) c -> p kc c", p=128)
        load_w(G, wt[0], "wt0", w3[:, :, 0:512], 512)
        load_w(G, wt[1], "wt1", w3[:, :, 512:1024], 512)
        gb = sbt(es, nc, "bgb", [128, D_], F32)
        yt = [sbt(es, nc, f"byt{i}", [128, D_], F32) for i in range(4)]
        yb = [sbt(es, nc, f"byb{i}", [128, D_], BF16) for i in range(2)]
        st = sbt(es, nc, "bst", [128, 16, 16], F32)
        xo = [sbt(es, nc, f"xo{i}", [128, 512], F32) for i in range(3)]
        xn = [sbt(es, nc, f"xn{i}", [128, 512], F32) for i in range(3)]
        order = [(cg, tc * 4 + t4) for tc in range(4) for cg in (0, 1) for t4 in range(4)]
        order += [(cg, tt) for cg in (2, 3) for tt in range(16)]
        pos = [0]

        def load_xo(k):
            cg, tt = order[k]
            S.dma("sp", f"xo{k % 3}", xo[k % 3][:], xsrc[tt * 128:(tt + 1) * 128, cg * 512:(cg + 1) * 512], w=[f"xo{k % 3}"])

        def emit_tile():
            k = pos[0]
            pos[0] += 1
            cg, tt = order[k]
            w = wt[cg % 2]
            wn = f"wt{cg % 2}"
            si = k % 3
            if k + 2 < len(order):
                load_xo(k + 2)
            b = 4 + S.rr("pp", 4)
            for kc in range(NKC):
                S.op("pe", lambda e: e.matmul(G.ps[b][:, :], lhsT=yT[:, kc, tt * 128:(tt + 1) * 128], rhs=w[:, kc, :],
                                              start=(kc == 0), stop=(kc == NKC - 1)), r=[wn, f"yT{tt // 4}"], w=[f"ps{b}"])
            S.op("dve", lambda e: e.tensor_tensor(out=xn[si][:], in0=G.ps[b][:, :], in1=xo[si][:], op=ALU.add),
                 r=[f"ps{b}", f"xo{si}"], w=[f"xn{si}"])
            S.dma("sp", f"xn{si}", G.xres[tt * 128:(tt + 1) * 128, cg * 512:(cg + 1) * 512], xn[si][:], r=[f"xn{si}"])

        S.dma("sp", "c1", gb[:], I["branch_norm_g"][l:l + 1, :].partition_broadcast(128), w=["bgb"])
        load_xo(0)
        load_xo(1)

        def load_y(t_):
            S.dma("sp", f"x{t_ % 4}", yt[t_ % 4][:], G.ycat[t_ * 128:(t_ + 1) * 128, :], w=[f"byt{t_ % 4}"])

        for t_ in range(4):
            load_y(t_)
        for tt in range(16):
            i = tt % 4
            i2 = tt % 2
            for br in range(4):
                S.op("act", lambda e: e.activation(out=yb[i2][:, br * 512:(br + 1) * 512],
                                                   in_=yt[i][:, br * 512:(br + 1) * 512], func=AF.Square,
                                                   accum_out=st[:, tt, br:br + 1]),
                     r=[f"byt{i}"], w=[f"byb{i2}", f"bst{tt}"])
            S.op("dve", lambda e: e.tensor_scalar(out=st[:, tt, 4:8], in0=st[:, tt, 0:4], scalar1=1.0 / GW,
                                                  scalar2=1e-6, op0=ALU.mult, op1=ALU.add),
                 r=[f"bst{tt}"], w=[f"bst{tt}"])
            S.op("act", lambda e: e.activation(out=st[:, tt, 8:12], in_=st[:, tt, 4:8], func=AF.Sqrt),
                 r=[f"bst{tt}"], w=[f"bst{tt}"])
            S.op("dve", lambda e: e.reciprocal(out=st[:, tt, 12:16], in_=st[:, tt, 8:12]),
                 r=[f"bst{tt}"], w=[f"bst{tt}"])
            for br in range(4):
                S.op("dve", lambda e: e.scalar_tensor_tensor(out=yb[i2][:, br * 512:(br + 1) * 512],
                                                             in0=yt[i][:, br * 512:(br + 1) * 512],
                                                             scalar=st[:, tt, 12 + br:13 + br],
                                                             in1=gb[:, br * 512:(br + 1) * 512],
                                                             op0=ALU.mult, op1=ALU.mult),
                     r=[f"byt{i}", f"bst{tt}", "bgb"], w=[f"byb{i2}"])
            transpose_rows(G, yb[i2], f"byb{i2}", yT, f"yT{tt // 4}", tt)
            if tt + 4 < 16:
                load_y(tt + 4)
            if tt % 4 == 3:
                for _ in range(8):
                    emit_tile()
        load_w(G, wt[0], "wt0", w3[:, :, 1024:1536], 512)
        load_w(G, wt[1], "wt1", w3[:, :, 1536:2048], 512)
        while pos[0] < len(order):
            emit_tile()


def stage_ffn_up(G, l, h2T):
    from contextlib import ExitStack
    nc, S, I = G.nc, G.S, G.I
    with ExitStack() as es:
        wg = [sbt(es, nc, f"wg{i}", [128, NKC, 512], BF16) for i in range(2)]
        wu = [sbt(es, nc, f"wu{i}", [128, NKC, 512], BF16) for i in range(2)]
        sg = [sbt(es, nc, f"fs{i}", [128, 512], F32) for i in range(3)]
        ao = [sbt(es, nc, f"ao{i}", [128, 512], BF16) for i in range(3)]
        g3 = I["w_gate"][l].rearrange("(kc p) c -> p kc c", p=128)
        u3 = I["w_up"][l].rearrange("(kc p) c -> p kc c", p=128)

        def issue(gi):
            load_w(G, wg[gi % 2], f"wg{gi % 2}", g3[:, :, gi * 512:(gi + 1) * 512], 512)
            load_w(G, wu[gi % 2], f"wu{gi % 2}", u3[:, :, gi * 512:(gi + 1) * 512], 512)

        def emit_tile(gi, mt, tc):
            j = gi % 2
            bg = S.rr("st", 4)
            bu = 4 + S.rr("pp", 4)
            for kc in range(NKC):
                S.op("pe", lambda e: e.matmul(G.ps[bg][:, :], lhsT=wg[j][:, kc, mt * 128:(mt + 1) * 128],
                                              rhs=h2T[:, kc, tc * 512:(tc + 1) * 512], start=(kc == 0),
                                              stop=(kc == NKC - 1)), r=[f"wg{j}", f"h2T{tc}"], w=[f"ps{bg}"])
            for kc in range(NKC):
                S.op("pe", lambda e: e.matmul(G.ps[bu][:, :], lhsT=wu[j][:, kc, mt * 128:(mt + 1) * 128],
                                              rhs=h2T[:, kc, tc * 512:(tc + 1) * 512], start=(kc == 0),
                                              stop=(kc == NKC - 1)), r=[f"wu{j}", f"h2T{tc}"], w=[f"ps{bu}"])
            si = S.rr("fs", 3)
            S.op("act", lambda e: e.activation(out=sg[si][:], in_=G.ps[bg][:, :], func=AF.Silu),
                 r=[f"ps{bg}"], w=[f"fs{si}"])
            S.op("dve", lambda e: e.tensor_tensor(out=ao[si][:], in0=G.ps[bu][:, :], in1=sg[si][:], op=ALU.mult),
                 r=[f"ps{bu}", f"fs{si}"], w=[f"ao{si}"])
            jj = gi * 4 + mt
            S.dma("sp", f"ao{si}", G.actT[tc, :, jj * 512:(jj + 1) * 512], ao[si][:], r=[f"ao{si}"])

        issue(0)
        issue(1)

        def after_chunk(tc):
            for mt in range(4):
                emit_tile(0, mt, tc)

        rms_to_T(G, es, G.xres, I["norm_ffn_g"][l:l + 1, :], h2T, "h2T", "f", after_chunk=after_chunk)
        for gi in range(1, DFF // 512):
            if gi + 1 < DFF // 512:
                issue(gi + 1)
            for mt in range(4):
                for tc in range(4):
                    emit_tile(gi, mt, tc)


def stage_ffn_down(G, l):
    from contextlib import ExitStack
    nc, S, I = G.nc, G.S, G.I
    NJ = DFF // 128
    with ExitStack() as es:
        wd = [sbt(es, nc, f"wd{i}", [128, NJ, 512], BF16) for i in range(2)]
        at = [sbt(es, nc, f"at{i}", [128, NJ, 512], BF16) for i in range(2)]
        xo = [sbt(es, nc, f"dxo{i}", [128, 512], F32) for i in range(3)]
        xn = [sbt(es, nc, f"dxn{i}", [128, 512], F32) for i in range(3)]
        d3 = I["w_down"][l].rearrange("(j p) c -> p j c", p=128)

        def issue(cg):
            for hf in range(2):
                G.S.dma("pool", f"wd{cg % 2}", wd[cg % 2][:, hf * (NJ // 2):(hf + 1) * (NJ // 2), :],
                        d3[:, hf * (NJ // 2):(hf + 1) * (NJ // 2), cg * 512:(cg + 1) * 512], w=[f"wd{cg % 2}"])

        issue(0)
        units = [(cg, tc) for cg in range(4) for tc in range(4)]

        def load_at(u):
            cg, tc = units[u]
            S.dma("sp", f"at{u % 2}", at[u % 2][:].rearrange("p j t -> p (j t)"), G.actT[tc], w=[f"at{u % 2}"])

        def load_xo(k):
            u, t4 = divmod(k, 4)
            cg, tc = units[u]
            tt = tc * 4 + t4
            S.dma("sp", f"xo{k % 3}", xo[k % 3][:], G.xres[tt * 128:(tt + 1) * 128, cg * 512:(cg + 1) * 512],
                  w=[f"dxo{k % 3}"])

        load_at(0)
        load_xo(0)
        load_xo(1)
        for u, (cg, tc) in enumerate(units):
            if tc == 0 and cg + 1 < 4:
                issue(cg + 1)
            if u + 1 < len(units):
                load_at(u + 1)
            w = wd[cg % 2]
            wn = f"wd{cg % 2}"
            ai = u % 2
            for t4 in range(4):
                k = u * 4 + t4
                tt = tc * 4 + t4
                si = k % 3
                if k + 2 < 4 * len(units):
                    load_xo(k + 2)
                b = 4 + S.rr("pp", 4)
                for j in range(NJ):
                    S.op("pe", lambda e: e.matmul(G.ps[b][:, :], lhsT=at[ai][:, j, t4 * 128:(t4 + 1) * 128],
                                                  rhs=w[:, j, :], start=(j == 0), stop=(j == NJ - 1)),
                         r=[wn, f"at{ai}"], w=[f"ps{b}"])
                S.op("dve", lambda e: e.tensor_tensor(out=xn[si][:], in0=G.ps[b][:, :], in1=xo[si][:], op=ALU.add),
                     r=[f"ps{b}", f"dxo{si}"], w=[f"dxn{si}"])
                S.dma("sp", f"xn{si}", G.xres[tt * 128:(tt + 1) * 128, cg * 512:(cg + 1) * 512], xn[si][:],
                      r=[f"dxn{si}"])


def stage_final(G):
    from contextlib import ExitStack
    nc, S, I = G.nc, G.S, G.I
    with ExitStack() as es:
        gb = sbt(es, nc, "fgb", [128, D_], F32)
        xt = [sbt(es, nc, f"fxt{i}", [128, D_], F32) for i in range(2)]
        ot = [sbt(es, nc, f"fot{i}", [128, D_], F32) for i in range(2)]
        st = sbt(es, nc, "fst", [128, 64], F32)
        S.dma("sp", "c1", gb[:], I["norm_final_g"][0:1, :].partition_broadcast(128), w=["fgb"])
        for tt in range(16):
            i = tt % 2
            S.dma("sp", f"x{i}", xt[i][:], G.xres[tt * 128:(tt + 1) * 128, :], w=[f"fxt{i}"])
            S.op("act", lambda e: e.activation(out=ot[i][:], in_=xt[i][:], func=AF.Square, accum_out=st[:, 4 * tt:4 * tt + 1]),
                 r=[f"fxt{i}"], w=[f"fot{i}", f"fst{tt}"])
            S.op("dve", lambda e: e.tensor_scalar(out=st[:, 4 * tt + 1:4 * tt + 2], in0=st[:, 4 * tt:4 * tt + 1],
                                                  scalar1=1.0 / D_, scalar2=1e-6, op0=ALU.mult, op1=ALU.add),
                 r=[f"fst{tt}"], w=[f"fst{tt}"])
            S.op("act", lambda e: e.activation(out=st[:, 4 * tt + 2:4 * tt + 3], in_=st[:, 4 * tt + 1:4 * tt + 2],
                                               func=AF.Sqrt), r=[f"fst{tt}"], w=[f"fst{tt}"])
            S.op("dve", lambda e: e.reciprocal(out=st[:, 4 * tt + 3:4 * tt + 4], in_=st[:, 4 * tt + 2:4 * tt + 3]),
                 r=[f"fst{tt}"], w=[f"fst{tt}"])
            S.op("dve", lambda e: e.scalar_tensor_tensor(out=ot[i][:], in0=xt[i][:], scalar=st[:, 4 * tt + 3:4 * tt + 4],
                                                         in1=gb[:], op0=ALU.mult, op1=ALU.mult),
                 r=[f"fxt{i}", f"fst{tt}", "fgb"], w=[f"fot{i}"])
            S.dma("sp", f"y{i}", G.out[tt * 128:(tt + 1) * 128, :], ot[i][:], r=[f"fot{i}"])


IN_SHAPES["rwkv_pk"] = [DEPTH, 128, 4, 8]
IN_SHAPES["rwkv_mu2"] = [DEPTH, 128, 4]
for _k in ("rwkv_mu", "rwkv_w0", "rwkv_a0", "rwkv_k_k", "rwkv_k_a", "rwkv_r_k"):
    IN_SHAPES.pop(_k)
CONST_SHAPES["c_rmask"] = [64, 128]


def stage_rwkv(G, l):
    from contextlib import ExitStack
    nc, S, I = G.nc, G.S, G.I
    NCH = 32
    with ExitStack() as es:
        tw = sbt(es, nc, "tw", [96, S_], BF16)
        adb = sbt(es, nc, "adb", [96, S_], BF16)
        sgb = sbt(es, nc, "sgb", [128, 2, S_], BF16)
        w2b = sbt(es, nc, "w2b", [96, GW], BF16)
        a2b = sbt(es, nc, "a2b", [96, GW], BF16)
        g2b = sbt(es, nc, "g2b", [128, 2, GW], BF16)
        pk = sbt(es, nc, "pk", [128, 4, 8], F32)
        omk = sbt(es, nc, "omk", [128, 4], F32)
        ones2 = sbt(es, nc, "ones2", [128, 128], BF16)
        bones = sbt(es, nc, "bones", [128, 2], BF16)
        E2 = sbt(es, nc, "E2", [128, 64], F32)
        mu2 = sbt(es, nc, "mu2", [128, 4], F32)
        rst = sbt(es, nc, "rst", [128, S_], F32)
        mi2 = sbt(es, nc, "mi2", [128, 64], F32)
        msbd = sbt(es, nc, "msbd", [128, 128], F32)
        msbdT = sbt(es, nc, "msbdT", [128, 128], F32)
        ones = sbt(es, nc, "ones", [64, 64], BF16)
        bon = sbt(es, nc, "bon", [128, 16, 8], F32)
        S.dma("pool", "c0", w2b[:], I["rwkv_w2"][l], w=["w2b"])
        S.dma("pool", "c0", a2b[:], I["rwkv_a2"][l], w=["a2b"])
        S.dma("pool", "c0", g2b[:], I["rwkv_g2"][l].rearrange("(j p) c -> p j c", p=128), w=["g2b"])
        S.dma("sp", "c1", pk[:], I["rwkv_pk"][l], w=["pk"])
        S.dma("sp", "c1", mu2[:], I["rwkv_mu2"][l], w=["mu2"])
        S.dma("sp", "c1", rst[:], I["c_reset"].partition_broadcast(128), w=["rst"])
        S.dma("sp", "c1", mi2[:], I["c_mi2"], w=["mi2"])
        S.dma("sp", "c1", msbd[:], I["c_msbd"], w=["msbd"])
        S.dma("sp", "c1", msbdT[:], I["c_msbdT"], w=["msbdT"])
        S.op("dve", lambda e: e.memset(ones[:], 1.0), w=["ones"])
        S.op("dve", lambda e: e.memset(ones2[:], 0.0), w=["ones2"])
        S.op("dve", lambda e: e.memset(bones[:], 0.0), w=["bones"])
        for hh in range(2):
            S.op("dve", lambda e: e.memset(ones2[hh * 64:(hh + 1) * 64, hh * 64:(hh + 1) * 64], 1.0), w=["ones2"])
            S.op("dve", lambda e: e.memset(bones[hh * 64:(hh + 1) * 64, hh:hh + 1], 1.0), w=["bones"])
        S.op("dve", lambda e: e.tensor_tensor(out=E2[:], in0=G.identf[:, 0:64], in1=G.identf[:, 64:128], op=ALU.add),
             r=["identf"], w=["E2"])
        S.op("dve", lambda e: e.tensor_scalar(out=omk[:], in0=pk[:, :, 6], scalar1=-1.0, scalar2=1.0, op0=ALU.mult,
                                              op1=ALU.add), r=["pk"], w=["omk"])
        with ExitStack() as e1:
            raw = sbt(e1, nc, "raw", [128, S_ + 1], F32)
            dd = sbt(e1, nc, "dd", [128, S_], F32)
            for (r0, nr, mcol, kind) in ((512, 96, 0, "w"), (1632, 96, 1, "a"), (1728, 128, 2, "g0"), (1856, 128, 3, "g1")):
                S.op("dve", lambda e: e.memset(raw[0:nr, 0:1], 0.0), w=["raw"])
                S.dma("sp", "x0", raw[0:nr, 1:S_ + 1], G.pdT[r0:r0 + nr, :], w=["raw"])
                S.op("dve", lambda e: e.tensor_tensor(out=dd[0:nr, :], in0=raw[0:nr, 0:S_], in1=raw[0:nr, 1:S_ + 1],
                                                      op=ALU.subtract), r=["raw"], w=["dd"])
                S.op("dve", lambda e: e.scalar_tensor_tensor(out=dd[0:nr, :], in0=dd[0:nr, :], scalar=mu2[0:nr, mcol:mcol + 1],
                                                             in1=raw[0:nr, 1:S_ + 1], op0=ALU.mult, op1=ALU.add),
                     r=["dd", "raw", "mu2"], w=["dd"])
                if kind == "w":
                    S.op("act", lambda e: e.activation(out=tw[:], in_=dd[0:96, :], func=AF.Tanh), r=["dd"], w=["tw"])
                elif kind == "a":
                    S.op("act", lambda e: e.copy(out=adb[:], in_=dd[0:96, :]), r=["dd"], w=["adb"])
                else:
                    j = 0 if kind == "g0" else 1
                    S.op("act", lambda e: e.activation(out=sgb[:, j, :], in_=dd[:, :], func=AF.Sigmoid), r=["dd"], w=["sgb"])
            S.barrier()

        KT = sbt(es, nc, "KT", [128, S_], BF16)
        BT = sbt(es, nc, "BT", [128, S_], BF16)
        AR = sbt(es, nc, "AR", [128, NCH, 128], BF16)
        AT = sbt(es, nc, "AT", [128, S_], BF16)
        DR = sbt(es, nc, "DR", [128, NCH, 128], BF16)
        KBr = sbt(es, nc, "KBr", [128, S_], BF16)
        BBr = sbt(es, nc, "BBr", [128, S_], BF16)
        VB = sbt(es, nc, "VB", [128, S_], BF16)
        RK = sbt(es, nc, "RK", [128, S_], BF16)
        vsf = sbt(es, nc, "vsf", [128, S_], F32)
        for h in range(8):
          if h % 2 == 0:
            pr = h // 2
            with ExitStack() as e1:
                T = [sbt(e1, nc, f"T{i}", [128, S_ + 1], F32) for i in range(3)]
                U = [sbt(e1, nc, f"U{i}", [128, S_], F32) for i in range(8)]
                SQ = sbt(e1, nc, "SQ", [128, S_], BF16)
                WC = sbt(e1, nc, "WC", [128, NCH], F32)
                cC = sbt(e1, nc, "cC", [128, NCH], F32)
                VS = sbt(e1, nc, "VS", [128, 16, 128], F32)

                def P(j):
                    return pk[:, pr, j:j + 1]

                rows = (0, 608, 1120)
                for ti in range(3):
                    S.op("dve", lambda e: e.memset(T[ti][:, 0:1], 0.0), w=[f"T{ti}"])
                for ti in range(3):
                    S.dma("sp", ("x0", "x1", "q0")[ti], T[ti][:, 1:S_ + 1],
                          G.pdT[rows[ti] + pr * 128:rows[ti] + (pr + 1) * 128, :], w=[f"T{ti}"])

                def shift(ti, mu_j, dst, dname, tmp, tname):
                    S.op("pool", lambda e: e.tensor_tensor(out=tmp[:], in0=T[ti][:, 0:S_], in1=T[ti][:, 1:S_ + 1],
                                                           op=ALU.subtract), r=[f"T{ti}"], w=[tname])
                    S.op("dve", lambda e: e.scalar_tensor_tensor(out=dst, in0=tmp[:], scalar=P(mu_j), in1=T[ti][:, 1:S_ + 1],
                                                                 op0=ALU.mult, op1=ALU.add), r=[tname, f"T{ti}", "pk"], w=[dname])

                rs, ks, lw, asg, kkn = U[0], U[1], U[2], U[3], U[4]
                shift(0, 0, rs[:], "U0", U[5], "U5")
                shift(1, 1, ks[:], "U1", U[6], "U6")
                shift(2, 2, vsf[:], "vsf", U[7], "U7")
                cum = T[0]
                S.op("act", lambda e: e.copy(out=VB[:], in_=vsf[:]), r=["vsf"], w=["VB"])
                for half in range(2):
                    for q4 in range(2):
                        b = S.rr("st", 4)
                        for t4 in range(4):
                            tt = half * 8 + q4 * 4 + t4
                            S.op("pe", lambda e: e.transpose(out=G.ps[b][:, t4 * 128:(t4 + 1) * 128], in_=vsf[:, tt * 128:(tt + 1) * 128],
                                                             identity=G.identf[:, :]), r=["vsf", "identf"], w=[f"ps{b}"])
                        S.op("act", lambda e: e.copy(out=VS[:, half * 8 + q4 * 4:half * 8 + q4 * 4 + 4, :].rearrange("p a b -> p (a b)"),
                                                     in_=G.ps[b][:, :]), r=[f"ps{b}"], w=["VS"])
                S.dma("sp", "y1", G.vtk.rearrange("(tt p) v -> p tt v", p=128)[:, :, pr * 128:(pr + 1) * 128], VS[:, :, :],
                      r=["VS"])
                for tc in range(4):
                    b = S.rr("st", 4)
                    S.op("pe", lambda e: e.matmul(G.ps[b][:, :], lhsT=w2b[:, pr * 128:(pr + 1) * 128],
                                                  rhs=tw[:, tc * 512:(tc + 1) * 512], start=True, stop=True),
                         r=["w2b", "tw"], w=[f"ps{b}"])
                    S.op("act", lambda e: e.activation(out=lw[:, tc * 512:(tc + 1) * 512], in_=G.ps[b][:, :],
                                                       func=AF.Sigmoid, bias=P(3)), r=[f"ps{b}", "pk"], w=["U2"])
                    b = S.rr("st", 4)
                    S.op("pe", lambda e: e.matmul(G.ps[b][:, :], lhsT=a2b[:, pr * 128:(pr + 1) * 128],
                                                  rhs=adb[:, tc * 512:(tc + 1) * 512], start=True, stop=True),
                         r=["a2b", "adb"], w=[f"ps{b}"])
                    S.op("act", lambda e: e.activation(out=asg[:, tc * 512:(tc + 1) * 512], in_=G.ps[b][:, :],
                                                       func=AF.Sigmoid, bias=P(4)), r=[f"ps{b}", "pk"], w=["U3"])
                S.op("pool", lambda e: e.tensor_scalar(out=lw[:], in0=lw[:], scalar1=-math.exp(-0.5), scalar2=0.0,
                                                       op0=ALU.mult, op1=ALU.add), r=["U2"], w=["U2"])
                S.op("dve", lambda e: e.tensor_scalar(out=kkn[:], in0=ks[:], scalar1=P(5), scalar2=None, op0=ALU.mult),
                     r=["U1", "pk"], w=["U4"])
                S.op("pool", lambda e: e.tensor_tensor(out=SQ[:], in0=kkn[:], in1=kkn[:], op=ALU.mult), r=["U4"], w=["SQ"])
                for tc in range(4):
                    b = S.rr("st", 4)
                    S.op("pe", lambda e: e.matmul(G.ps[b][:, :], lhsT=ones2[:], rhs=SQ[:, tc * 512:(tc + 1) * 512],
                                                  start=True, stop=True), r=["ones2", "SQ"], w=[f"ps{b}"])
                    S.op("act", lambda e: e.activation(out=U[5][:, tc * 512:(tc + 1) * 512], in_=G.ps[b][:, :],
                                                       func=AF.Sqrt), r=[f"ps{b}"], w=["U5"])
                S.op("dve", lambda e: e.tensor_scalar(out=U[5][:], in0=U[5][:], scalar1=1e-12, scalar2=None, op0=ALU.max),
                     r=["U5"], w=["U5"])
                S.op("dve", lambda e: e.reciprocal(out=U[5][:], in_=U[5][:]), r=["U5"], w=["U5"])
                S.op("pool", lambda e: e.tensor_tensor(out=kkn[:], in0=kkn[:], in1=U[5][:], op=ALU.mult),
                     r=["U4", "U5"], w=["U4"])
                S.op("dve", lambda e: e.tensor_scalar(out=U[6][:], in0=asg[:], scalar1=P(6), scalar2=omk[:, pr:pr + 1],
                                                      op0=ALU.mult, op1=ALU.add), r=["U3", "pk", "omk"], w=["U6"])
                S.op("pool", lambda e: e.tensor_tensor(out=ks[:], in0=ks[:], in1=U[6][:], op=ALU.mult),
                     r=["U1", "U6"], w=["U1"])
                bb = U[7]
                S.op("dve", lambda e: e.tensor_tensor(out=bb[:], in0=kkn[:], in1=asg[:], op=ALU.mult),
                     r=["U4", "U3"], w=["U7"])
                S.op("dve", lambda e: e.scalar_tensor_tensor(out=RK[:], in0=rs[:], scalar=P(7), in1=ks[:], op0=ALU.mult,
                                                             op1=ALU.mult), r=["U0", "U1", "pk"], w=["RK"])
                b = S.rr("st", 4)
                for tt in range(16):
                    S.op("pe", lambda e: e.matmul(G.ps[b][:, 2 * tt:2 * tt + 2], lhsT=RK[:, tt * 128:(tt + 1) * 128], rhs=bones[:, :],
                                                  start=True, stop=True), r=["RK", "bones"], w=[f"ps{b}"])
                S.op("act", lambda e: e.copy(out=bon[:, :, 2 * pr:2 * pr + 2], in_=G.ps[b][:, 0:32].rearrange("p (t h) -> p t h", h=2)),
                     r=[f"ps{b}"], w=["bon"])
                S.op("dve", lambda e: e.tensor_tensor_scan(out=cum[:, 0:S_], data0=rst[:], data1=lw[:], initial=0.0,
                                                           op0=ALU.mult, op1=ALU.add), r=["rst", "U2"], w=["T0"])
                cum3 = cum[:, 0:S_].rearrange("p (c t) -> p c t", t=64)
                S.op("dve", lambda e: e.tensor_copy(out=cC[:], in_=cum3[:, :, 63]), r=["T0"], w=["cC"])
                S.op("act", lambda e: e.activation(out=WC[:], in_=cC[:], func=AF.Exp), r=["cC"], w=["WC"])
                S.op("dve", lambda e: e.tensor_tensor(out=DR[:, :, 0:64],
                                                      in0=E2[:, :].unsqueeze(1).to_broadcast([128, NCH, 64]),
                                                      in1=WC[:].unsqueeze(2).to_broadcast([128, NCH, 64]), op=ALU.mult),
                     r=["E2", "WC"], w=["DR"])
                ex = T[1]
                ex2 = T[2]
                S.op("act", lambda e: e.activation(out=ex[:, 0:S_], in_=cum[:, 0:S_], func=AF.Exp), r=["T0"], w=["T1"])
                S.op("act", lambda e: e.activation(out=ex2[:, 0:S_], in_=cum[:, 0:S_], func=AF.Exp, scale=-1.0),
                     r=["T0", "vsf"], w=["T2"])
                S.op("pool", lambda e: e.tensor_tensor(out=rs[:], in0=rs[:], in1=ex[:, 0:S_], op=ALU.mult),
                     r=["U0", "T1"], w=["U0"])
                S.op("act", lambda e: e.copy(out=AR[:, :, 0:64], in_=rs[:].rearrange("p (c t) -> p c t", t=64)),
                     r=["U0"], w=["AR"])
                S.op("act", lambda e: e.copy(out=DR[:, :, 64:128], in_=rs[:].rearrange("p (c t) -> p c t", t=64)),
                     r=["U0"], w=["DR"])
                S.op("pool", lambda e: e.tensor_tensor(out=KT[:], in0=ks[:], in1=ex2[:, 0:S_], op=ALU.mult),
                     r=["U1", "T2"], w=["KT"])
                S.op("dve", lambda e: e.tensor_tensor(out=BT[:], in0=bb[:], in1=ex2[:, 0:S_], op=ALU.mult),
                     r=["U7", "T2"], w=["BT"])
                S.op("pool", lambda e: e.tensor_tensor(out=lw[:], in0=cum[:, 0:S_], in1=lw[:], op=ALU.subtract),
                     r=["T0", "U2"], w=["U2"])
                S.op("act", lambda e: e.activation(out=lw[:], in_=lw[:], func=AF.Exp), r=["U2"], w=["U2"])
                S.op("dve", lambda e: e.scalar_tensor_tensor(out=AT[:], in0=kkn[:], scalar=-1.0, in1=lw[:],
                                                             op0=ALU.mult, op1=ALU.mult), r=["U4", "U2"], w=["AT"])
                S.op("act", lambda e: e.copy(out=AR[:, :, 64:128], in_=AT[:].rearrange("p (c t) -> p c t", t=64)),
                     r=["AT"], w=["AR"])
                S.op("dve", lambda e: e.tensor_tensor(out=ex[:, 0:S_].rearrange("p (c t) -> p c t", t=64),
                                                      in0=cC[:].unsqueeze(2).to_broadcast([128, NCH, 64]), in1=cum3,
                                                      op=ALU.subtract), r=["cC", "T0", "T1"], w=["T1"])
                S.op("act", lambda e: e.activation(out=ex[:, 0:S_], in_=ex[:, 0:S_], func=AF.Exp), r=["T1"], w=["T1"])
                S.op("pool", lambda e: e.tensor_tensor(out=KBr[:], in0=ks[:], in1=ex[:, 0:S_], op=ALU.mult),
                     r=["U1", "T1"], w=["KBr"])
                S.op("dve", lambda e: e.tensor_tensor(out=BBr[:], in0=bb[:], in1=ex[:, 0:S_], op=ALU.mult),
                     r=["U7", "T1"], w=["BBr"])
                S.barrier()
          if True:
            base = 64 * (h % 2)
            hp = slice(base, base + 64)
            with ExitStack() as e2:
                NQ = NCH // 2
                Xp = sbt(e2, nc, "Xp", [128, NQ, 128], BF16)
                W1p = sbt(e2, nc, "W1p", [128, NQ, 128], BF16)
                W2p = sbt(e2, nc, "W2p", [128, NQ, 128], BF16)
                Vtp = sbt(e2, nc, "Vtp", [128, NQ, 64], BF16)
                AK = sbt(e2, nc, "AK", [128, NQ, 128], BF16)
                AN = [sbt(e2, nc, f"AN{i}", [128, NQ, 256], BF16) for i in range(2)]
                GRT = sbt(e2, nc, "GRT", [64, NCH, 128], BF16)
                HYb = sbt(e2, nc, "HYb", [128, NCH, 64], BF16)
                YD = sbt(e2, nc, "YD", [128, NCH, 64], F32)
                DRs = sbt(e2, nc, "DRs", [64, NCH, 128], BF16)
                STb = [sbt(e2, nc, f"STb{i}", [64, 64], BF16) for i in range(2)]
                idbb = G.identb[hp, hp]
                if base == 0:
                    DRv = DR[0:64, :, :]
                else:
                    for c4 in range(NCH // 4):
                        b = S.rr("all", 8)
                        S.op("pe", lambda e: e.matmul(G.ps[b][0:64, :], lhsT=G.identb[:, 64:128],
                                                      rhs=DR[:, c4 * 4:(c4 + 1) * 4, :].rearrange("p c x -> p (c x)"),
                                                      start=True, stop=True), r=["DR", "identb"], w=[f"ps{b}"])
                        o_ = DRs[:, c4 * 4:(c4 + 1) * 4, :].rearrange("p c x -> p (c x)")
                        if c4 % 2 == 0:
                            S.op("act", lambda e: e.copy(out=o_, in_=G.ps[b][0:64, :]), r=[f"ps{b}"], w=["DRs"])
                        else:
                            S.op("dve", lambda e: e.tensor_copy(out=o_, in_=G.ps[b][0:64, :]), r=[f"ps{b}"], w=["DRs"])
                    DRv = DRs[:, :, :]
                for (src, sname, dst3, dname, eng) in ((AT, "AT", Xp[:, :, 0:64], "XpA", "act"), (BBr, "BBr", W1p[:, :, 0:64], "W1A", "dve"),
                                                       (KBr, "KBr", W2p[:, :, 0:64], "W2A", "act"), (VB, "VB", Vtp[:, :, :], "Vtp", "dve")):
                    b = S.rr("all", 8)
                    pv = G.ps[b][:].bitcast(BF16)
                    for q in range(NQ):
                        S.op("pe", lambda e: e.transpose(out=pv[:, q * 64:(q + 1) * 64], in_=src[hp, q * 128:(q + 1) * 128],
                                                         identity=idbb), r=[sname, "identb"], w=[f"ps{b}"])
                    i_ = pv[:, 0:1024].rearrange("p (q k) -> p q k", k=64)
                    if eng == "act":
                        S.op("act", lambda e: e.copy(out=dst3, in_=i_), r=[f"ps{b}"], w=[dname])
                    else:
                        S.op("dve", lambda e: e.tensor_copy(out=dst3, in_=i_), r=[f"ps{b}"], w=[dname])

                def group_steps(gq):
                    q0 = gq * 4
                    g = f"_{gq}"
                    steps = []

                    def s0():
                        for (srcT, sn, Wp, wtok, dstbd, dtok) in ((BT, "BT", W1p, "W1" + g, AN[0], "AN0" + g),
                                                                    (KT, "KT", W2p, "W2" + g, AK, "AK" + g)):
                            for hb2 in range(2):
                                b = S.rr("all", 8)
                                for qi in range(2):
                                    q = q0 + hb2 * 2 + qi
                                    S.op("pe", lambda e: e.matmul(G.ps[b][:, qi * 256:(qi + 1) * 256], lhsT=srcT[hp, q * 128:(q + 1) * 128],
                                                                  rhs=AR[hp, 2 * q:2 * q + 2, :].rearrange("p c x -> p (c x)"),
                                                                  start=True, stop=True), r=[sn, "AR"], w=[f"ps{b}"])
                                qs = slice(q0 + hb2 * 2, q0 + hb2 * 2 + 2)
                                pq = G.ps[b][:, :].rearrange("p (q x) -> p q x", q=2)
                                for par in range(2):
                                    rows = slice(par * 64, par * 64 + 64)
                                    S.op("dve", lambda e: e.tensor_tensor(out=Wp[rows, qs, 64:128], in0=pq[rows, :, par * 128:par * 128 + 64],
                                                                          in1=mi2[rows, :].unsqueeze(1).to_broadcast([64, 2, 64]), op=ALU.mult),
                                         r=[f"ps{b}", "mi2"], w=[wtok])
                                p4 = G.ps[b][:, :].rearrange("p (q a x) -> p q a x", q=2, a=2)
                                if dstbd is AK:
                                    o4 = AK[:, qs, :].rearrange("p q (a x) -> p q a x", a=2)
                                else:
                                    o4 = AN[0][:, qs, 128:256].rearrange("p q (a x) -> p q a x", a=2)
                                S.op("dve", lambda e: e.tensor_tensor(out=o4, in0=p4[:, :, :, 64:128],
                                                                      in1=msbd[:, :].rearrange("p (a x) -> p a x", a=2).unsqueeze(1).to_broadcast([128, 2, 2, 64]),
                                                                      op=ALU.mult), r=[f"ps{b}", "msbd"], w=[dtok])
                        b = S.rr("all", 8)
                        for qi in range(4):
                            q = q0 + qi
                            S.op("pe", lambda e: e.matmul(G.ps[b][:, qi * 128:(qi + 1) * 128], lhsT=AT[hp, q * 128:(q + 1) * 128],
                                                          rhs=BT[hp, q * 128:(q + 1) * 128], start=True, stop=True), r=["AT", "BT"], w=[f"ps{b}"])
                        S.op("dve", lambda e: e.tensor_tensor(out=AN[0][:, q0:q0 + 4, 0:128],
                                                              in0=G.ps[b][:, :].rearrange("p (q x) -> p q x", q=4),
                                                              in1=msbdT[:, :].unsqueeze(1).to_broadcast([128, 4, 128]), op=ALU.mult),
                             r=[f"ps{b}", "msbdT"], w=["AN0" + g])
                    steps.append(s0)

                    def s1():
                        b = S.rr("all", 8)
                        for qi in range(4):
                            q = q0 + qi
                            S.op("pe", lambda e: e.matmul(G.ps[b][:, qi * 64:(qi + 1) * 64], lhsT=AK[:, q, :], rhs=Vtp[:, q, :],
                                                          start=True, stop=True), r=["AK" + g, "Vtp"], w=[f"ps{b}"])
                        S.op("act", lambda e: e.copy(out=Xp[:, q0:q0 + 4, 64:128],
                                                     in_=G.ps[b][:, 0:256].rearrange("p (q x) -> p q x", q=4)),
                             r=[f"ps{b}"], w=["Xp" + g])
                    steps.append(s1)

                    def mkx(j):
                        def sx():
                            an = AN[j % 2]
                            ann = f"AN{j % 2}" + g
                            b = S.rr("all", 8)
                            for qi in range(4):
                                q = q0 + qi
                                S.op("pe", lambda e: e.matmul(G.ps[b][:, qi * 128:(qi + 1) * 128], lhsT=an[:, q, 128:256], rhs=Xp[:, q, :],
                                                              start=True, stop=True), r=[ann, "Xp" + g, "XpA"], w=[f"ps{b}"])
                            if j < 5:
                                for hb2 in range(2):
                                    bq = S.rr("all", 8)
                                    for qi in range(2):
                                        q = q0 + hb2 * 2 + qi
                                        S.op("pe", lambda e: e.matmul(G.ps[bq][:, qi * 256:qi * 256 + 128], lhsT=an[:, q, 128:256],
                                                                      rhs=an[:, q, 0:128], start=True, stop=True), r=[ann], w=[f"ps{bq}"])
                                        S.op("pe", lambda e: e.matmul(G.ps[bq][:, qi * 256 + 128:(qi + 1) * 256], lhsT=an[:, q, 0:128],
                                                                      rhs=an[:, q, 128:256], start=True, stop=True), r=[ann], w=[f"ps{bq}"])
                                    S.op("act", lambda e: e.copy(out=AN[(j + 1) % 2][:, q0 + hb2 * 2:q0 + hb2 * 2 + 2, :],
                                                                 in_=G.ps[bq][:, :].rearrange("p (q x) -> p q x", q=2)),
                                         r=[f"ps{bq}"], w=[f"AN{(j + 1) % 2}" + g])
                            S.op("dve", lambda e: e.tensor_tensor(out=Xp[:, q0:q0 + 4, :], in0=Xp[:, q0:q0 + 4, :],
                                                                  in1=G.ps[b][:, :].rearrange("p (q x) -> p q x", q=4), op=ALU.add),
                                 r=[f"ps{b}", "Xp" + g, "XpA"], w=["Xp" + g, "XpA" + g])
                        return sx
                    for j in range(6):
                        steps.append(mkx(j))

                    def s8():
                        for par in range(2):
                            rows = slice(par * 64, par * 64 + 64)
                            b = S.rr("all", 8)
                            for qi in range(4):
                                q = q0 + qi
                                S.op("pe", lambda e: e.matmul(G.ps[b][0:64, qi * 128:(qi + 1) * 128], lhsT=Xp[rows, q, 0:64],
                                                              rhs=W1p[rows, q, 0:128], start=True, stop=True),
                                     r=["Xp" + g, "W1" + g, "W1A"], w=[f"ps{b}"])
                            cs = slice(2 * q0 + par, 2 * q0 + 8, 2)
                            S.op("dve", lambda e: e.tensor_tensor(out=GRT[:, cs, :], in0=G.ps[b][0:64, :].rearrange("p (c x) -> p c x", c=4),
                                                                  in1=DRv[:, cs, :], op=ALU.add), r=[f"ps{b}", "DRs", "DR"], w=["GRT" + g])
                        for par in range(2):
                            rows = slice(par * 64, par * 64 + 64)
                            b = S.rr("all", 8)
                            for qi in range(4):
                                q = q0 + qi
                                S.op("pe", lambda e: e.matmul(G.ps[b][:, qi * 64:(qi + 1) * 64], lhsT=W1p[rows, q, 0:128],
                                                              rhs=Xp[rows, q, 64:128], start=True, stop=False),
                                     r=["Xp" + g, "W1" + g, "W1A"], w=[f"ps{b}"])
                                S.op("pe", lambda e: e.matmul(G.ps[b][:, qi * 64:(qi + 1) * 64], lhsT=W2p[rows, q, 0:128],
                                                              rhs=Vtp[rows, q, :], start=False, stop=True),
                                     r=["Vtp", "W2" + g, "W2A"], w=[f"ps{b}"])
                            cs = slice(2 * q0 + par, 2 * q0 + 8, 2)
                            S.op("act", lambda e: e.copy(out=HYb[:, cs, :], in_=G.ps[b][:, 0:256].rearrange("p (c x) -> p c x", c=4)),
                                 r=[f"ps{b}"], w=["HYb" + g])
                    steps.append(s8)
                    return steps

                for batch in range(2):
                    lists = [group_steps(batch * 2 + gg) for gg in range(2)]
                    for si in range(len(lists[0])):
                        for gg in range(2):
                            lists[gg][si]()
                S.op("dve", lambda e: e.memset(STb[0][:], 0.0), w=["STb0"])
                for c in range(NCH):
                    si = c % 2
                    g = f"_{c // 8}"
                    b = S.rr("all", 8)
                    S.op("pe", lambda e: e.matmul(G.ps[b][:, 0:64], lhsT=GRT[:, c, :], rhs=STb[si][:], start=True, stop=False),
                         r=["GRT" + g, f"STb{si}"], w=[f"ps{b}"])
                    S.op("pe", lambda e: e.matmul(G.ps[b][:, 0:64], lhsT=G.identb[:, :], rhs=HYb[:, c, :], start=False, stop=True),
                         r=["HYb" + g, "identb"], w=[f"ps{b}"])
                    S.op("dve", lambda e: e.tensor_copy(out=STb[1 - si][:], in_=G.ps[b][0:64, 0:64]),
                         r=[f"ps{b}"], w=[f"STb{1 - si}"])
                    S.op("act", lambda e: e.copy(out=YD[64:128, c, :], in_=G.ps[b][64:128, 0:64]), r=[f"ps{b}"], w=["YD"])
                S.dma("sp", "y0", G.ydr.rearrange("(c t) v -> t c v", t=64)[:, :, h * 64:(h + 1) * 64], YD[64:128, :, :],
                      r=["YD"])
                S.barrier()

        with ExitStack() as e3:
            lgx = sbt(e3, nc, "lgx", [128, GW], F32)
            lbx = sbt(e3, nc, "lbx", [128, GW], F32)
            yt = [sbt(e3, nc, f"eyt{i}", [128, GW], F32) for i in range(2)]
            vt = [sbt(e3, nc, f"evt{i}", [128, GW], F32) for i in range(2)]
            sqs = [sbt(e3, nc, f"esq{i}", [128, GW], F32) for i in range(2)]
            yo = [sbt(e3, nc, f"eyo{i}", [128, GW], F32) for i in range(2)]
            stts = [sbt(e3, nc, f"est{i}", [128, 8, 8], F32) for i in range(2)]
            S.dma("sp", "c1", lgx[:], I["rwkv_lnx_g"][l:l + 1, :].partition_broadcast(128), w=["lgx"])
            S.dma("sp", "c1", lbx[:], I["rwkv_lnx_b"][l:l + 1, :].partition_broadcast(128), w=["lbx"])

            def ep_steps(tt, i):
                y, sq, stt = yt[i], sqs[i], stts[i]
                yn, sqn, stn = f"eyt{i}", f"esq{i}", f"est{i}"
                y3 = y[:].rearrange("p (h v) -> p h v", h=8)
                st = []
                st.append(lambda: S.dma("sp", f"x{i}", y[:], G.ydr[tt * 128:(tt + 1) * 128, :], w=[yn]))
                st.append(lambda: S.dma("sp", f"q{i}", vt[i][:], G.vtk[tt * 128:(tt + 1) * 128, :], w=[f"evt{i}"]))
                st.append(lambda: S.op("dve", lambda e: e.tensor_reduce(out=stt[:, 0, :], in_=y3, axis=AX.X, op=ALU.add), r=[yn], w=[stn]))
                st.append(lambda: S.op("act", lambda e: e.activation(out=sq[:], in_=y[:], func=AF.Square), r=[yn], w=[sqn]))
                st.append(lambda: S.op("dve", lambda e: e.tensor_reduce(out=stt[:, 1, :], in_=sq[:].rearrange("p (h v) -> p h v", h=8),
                                                                        axis=AX.X, op=ALU.add), r=[sqn], w=[stn]))
                st.append(lambda: S.op("dve", lambda e: e.tensor_scalar(out=stt[:, 2, :], in0=stt[:, 0, :], scalar1=1.0 / 64, scalar2=None,
                                                                        op0=ALU.mult), r=[stn], w=[stn]))
                st.append(lambda: S.op("dve", lambda e: e.tensor_tensor(out=stt[:, 3, :], in0=stt[:, 2, :], in1=stt[:, 2, :], op=ALU.mult),
                                       r=[stn], w=[stn]))
                st.append(lambda: S.op("dve", lambda e: e.scalar_tensor_tensor(out=stt[:, 4, :], in0=stt[:, 1, :], scalar=1.0 / 64,
                                                                               in1=stt[:, 3, :], op0=ALU.mult, op1=ALU.subtract),
                                       r=[stn], w=[stn]))
                st.append(lambda: S.op("dve", lambda e: e.tensor_scalar(out=stt[:, 4, :], in0=stt[:, 4, :], scalar1=64e-5, scalar2=None,
                                                                        op0=ALU.add), r=[stn], w=[stn]))
                st.append(lambda: S.op("act", lambda e: e.activation(out=stt[:, 5, :], in_=stt[:, 4, :], func=AF.Sqrt), r=[stn], w=[stn]))
                st.append(lambda: S.op("dve", lambda e: e.reciprocal(out=stt[:, 6, :], in_=stt[:, 5, :]), r=[stn], w=[stn]))
                st.append(lambda: S.op("dve", lambda e: e.tensor_tensor(out=y3, in0=y3, in1=stt[:, 2, :].unsqueeze(2).to_broadcast([128, 8, 64]),
                                                                        op=ALU.subtract), r=[yn, stn], w=[yn]))
                st.append(lambda: S.op("dve", lambda e: e.tensor_tensor(out=y3, in0=y3, in1=stt[:, 6, :].unsqueeze(2).to_broadcast([128, 8, 64]),
                                                                        op=ALU.mult), r=[yn, stn], w=[yn]))
                st.append(lambda: S.op("pool", lambda e: e.tensor_tensor(out=sq[:].rearrange("p (h v) -> p h v", h=8),
                                                                         in0=vt[i][:].rearrange("p (h v) -> p h v", h=8),
                                                                         in1=bon[:, tt, :].unsqueeze(2).to_broadcast([128, 8, 64]), op=ALU.mult),
                                       r=[f"evt{i}", "bon", sqn], w=[sqn]))
                st.append(lambda: S.op("pool", lambda e: e.tensor_tensor(out=y[:], in0=y[:], in1=lgx[:], op=ALU.mult), r=[yn, "lgx"], w=[yn]))
                st.append(lambda: S.op("pool", lambda e: e.tensor_tensor(out=y[:], in0=y[:], in1=lbx[:], op=ALU.add), r=[yn, "lbx"], w=[yn]))
                st.append(lambda: S.op("pool", lambda e: e.tensor_tensor(out=y[:], in0=y[:], in1=sq[:], op=ALU.add), r=[yn, sqn], w=[yn]))

                def gate():
                    b = 4 + S.rr("pp", 4)
                    for j in range(2):
                        S.op("pe", lambda e: e.matmul(G.ps[b][:, :], lhsT=sgb[:, j, tt * 128:(tt + 1) * 128], rhs=g2b[:, j, :],
                                                      start=(j == 0), stop=(j == 1)), r=["sgb", "g2b"], w=[f"ps{b}"])
                    S.op("dve", lambda e: e.tensor_tensor(out=yo[i][:], in0=G.ps[b][:, :], in1=y[:], op=ALU.mult),
                         r=[f"ps{b}", yn], w=[f"eyo{i}"])
                st.append(gate)
                st.append(lambda: S.dma("sp", f"y{i}", G.ycat[tt * 128:(tt + 1) * 128, 1536:2048], yo[i][:], r=[f"eyo{i}"]))
                return st

            for t0 in range(0, 16, 2):
                lockstep([ep_steps(t0 + i, i) for i in range(2)])
            S.barrier()


def prepare_inputs(inp):
    f = lambda a: np.ascontiguousarray(np.asarray(a, dtype=np.float32))
    shared = {}
    for k in ("norm_mix_g", "w_in", "pos_bias", "sgu_ln_g", "rwkv_w2", "rwkv_a2", "rwkv_g2", "rwkv_lnx_g", "rwkv_lnx_b",
              "branch_norm_g", "w_out", "norm_ffn_g", "w_gate", "w_up", "w_down"):
        shared[k] = f(inp[k])
    shared["norm_final_g"] = f(inp["norm_final_g"]).reshape(1, D_)
    shared["sgu_wT"] = f(np.transpose(np.asarray(inp["sgu_w"]), (0, 1, 3, 2)))
    shared["sgu_bT"] = f(np.transpose(np.asarray(inp["sgu_b"]), (0, 2, 1)))
    mu = np.asarray(inp["rwkv_mu"], dtype=np.float32)
    hk = lambda a: np.asarray(a, dtype=np.float32).reshape(DEPTH, 8, 64).transpose(0, 2, 1)
    pk = np.stack([hk(mu[:, 0:512]), hk(mu[:, 608:1120]), hk(mu[:, 1120:1632]), hk(inp["rwkv_w0"]), hk(inp["rwkv_a0"]),
                   hk(inp["rwkv_k_k"]), hk(inp["rwkv_k_a"]), hk(inp["rwkv_r_k"])], axis=-1)
    pk = pk.reshape(DEPTH, 64, 4, 2, 8).transpose(0, 3, 1, 2, 4).reshape(DEPTH, 128, 4, 8)
    shared["rwkv_pk"] = f(pk)
    mu2 = np.zeros((DEPTH, 128, 4), np.float32)
    mu2[:, 0:96, 0] = mu[:, 512:608]
    mu2[:, 0:96, 1] = mu[:, 1632:1728]
    mu2[:, :, 2] = mu[:, 1728:1856]
    mu2[:, :, 3] = mu[:, 1856:1984]
    shared["rwkv_mu2"] = mu2
    shared.update(host_constants())
    x = f(inp["x"])
    return [dict(shared, x=x[b]) for b in range(NCORES)]


def kernel(**inputs):
    if "nc" not in _PROG:
        _PROG["nc"] = build()
    in_maps = prepare_inputs(inputs)
    res = run_bass_kernel_spmd(_PROG["nc"], in_maps, core_ids=list(range(NCORES)))
    return np.stack([np.asarray(r["out"], dtype=np.float32) for r in res.results], axis=0)
```

```python
import math
import numpy as np
import concourse.bass as bass
import concourse.mybir as mybir
from concourse.bass_utils import run_bass_kernel_spmd

F32 = mybir.dt.float32
BF16 = mybir.dt.bfloat16
AF = mybir.ActivationFunctionType
ALU = mybir.AluOpType
AX = mybir.AxisListType

S_ = 2048
D_ = 2048
DEPTH = 2
NCORES = 8
GW = 512
DIN = 6080
DFF = 5632
NKC = 16
LU = 2560
TW = 2432
BIG = 30000.0


class Sched:
    def __init__(self, nc):
        self.nc = nc
        self.eng = {"pe": nc.tensor, "dve": nc.vector, "act": nc.scalar, "pool": nc.gpsimd, "sp": nc.sync}
        self.sem = {k: nc.alloc_semaphore(f"se_{k}") for k in self.eng}
        self.cnt = {k: 0 for k in self.eng}
        self.dsem = {}
        self.dtot = {}
        self.waited = {k: {} for k in self.eng}
        self.last_w = {}
        self.readers = {}
        self.nbuf = 0
        self.rot = {}

    def sb(self, name, shape, dt):
        return self.nc.sbuf_tensor(name, list(shape), dt).__enter__()

    def _deps(self, r, w):
        evs = []
        for t in r:
            if t in self.last_w:
                evs.append(self.last_w[t])
        for t in w:
            if t in self.last_w:
                evs.append(self.last_w[t])
            evs.extend(self.readers.get(t, ()))
        return evs

    def _wait(self, e, evs):
        need = {}
        for (key, val) in evs:
            if key == e and e == "pe":
                continue
            if val > need.get(key, 0):
                need[key] = val
        for key, val in need.items():
            if self.waited[e].get(key, 0) >= val:
                continue
            if key in self.sem:
                sem = self.sem[key]
            else:
                sem = self.dsem[key]
                val = max(val, self.dtot[key])
            self.eng[e].wait_ge(sem, val)
            self.waited[e][key] = val

    def _commit(self, ev, r, w):
        for t in w:
            self.last_w[t] = ev
            self.readers[t] = []
        for t in r:
            self.readers.setdefault(t, []).append(ev)

    def op(self, e, fn, r=(), w=()):
        self._wait(e, self._deps(r, w))
        inst = fn(self.eng[e])
        inst.then_inc(self.sem[e], 1)
        self.cnt[e] += 1
        self._commit((e, self.cnt[e]), r, w)

    def dma(self, q, key, out, in_, r=(), w=(), **kw):
        if key not in self.dsem:
            self.dsem[key] = self.nc.alloc_semaphore(f"sd_{key}")
            self.dtot[key] = 0
        self._wait(q, self._deps(r, w))
        self.eng[q].dma_start(out=out, in_=in_, **kw).then_inc(self.dsem[key], 16)
        self.dtot[key] += 16
        self._commit((key, self.dtot[key]), r, w)

    def barrier(self):
        evs = [(k, v) for k, v in self.cnt.items() if v > 0]
        evs += [(k, v) for k, v in self.dtot.items() if v > 0]
        for e in self.eng:
            self._wait(e, [ev for ev in evs if ev[0] != e])
        self.last_w = {}
        self.readers = {}

    def rr(self, name, n):
        i = self.rot.get(name, 0)
        self.rot[name] = (i + 1) % n
        return i


def t5_bucket_np(dist):
    dist = np.maximum(dist, 0)
    d = np.maximum(dist, 1).astype(np.float32)
    large = 16 + (np.log(d / np.float32(16)) / np.float32(math.log(2048 / 16)) * np.float32(16)).astype(np.int32)
    large = np.minimum(large, 31)
    return np.where(dist < 16, dist, large)


def host_constants():
    c = {}
    c["c_ident"] = np.eye(128, dtype=np.float32)
    d = np.arange(LU) - 511
    bk = t5_bucket_np(d)
    cntA = np.zeros(LU, np.float32)
    for (wdw, dil) in ((128, 1), (512, 4), (2048, 16)):
        cntA += ((d >= 0) & (d % dil == 0) & (d <= wdw)).astype(np.float32)
    ohA = np.zeros((32, LU), np.float32)
    ohC = np.zeros((32, LU), np.float32)
    ohA[bk, np.arange(LU)] = cntA
    ohC[bk, np.arange(LU)] = (d >= 0).astype(np.float32)
    c["c_ohA"] = ohA
    c["c_ohC"] = ohC
    s = np.arange(128)
    c["c_tril"] = (s[:, None] <= s[None, :]).astype(np.float32)
    ohb = np.zeros((8, 16, 128), np.float32)
    for kb in range(16):
        ohb[kb // 2, kb, :] = 1.0
    c["c_ohb"] = ohb.reshape(8, 16 * 128)
    i = np.arange(64)
    m = np.zeros((64, 128), np.float32)
    m[:, :64] = (i[:, None] <= i[None, :])
    m[:, 64:] = (i[:, None] < i[None, :])
    c["c_rmask"] = m
    c["c_rmaskT"] = (i[:, None] > i[None, :]).astype(np.float32)
    p_ = np.arange(128)
    par, ii = p_ // 64, p_ % 64
    c["c_mi2"] = (ii[:, None] <= i[None, :]).astype(np.float32)
    c["c_msbd"] = ((par[:, None] == par[None, :]) & (ii[:, None] < ii[None, :])).astype(np.float32)
    c["c_msbdT"] = ((par[:, None] == par[None, :]) & (ii[:, None] > ii[None, :])).astype(np.float32)
    rs = np.ones((1, S_), np.float32)
    rs[0, ::64] = 0.0
    c["c_reset"] = rs
    return c


CONST_SHAPES = {"c_ident": [128, 128], "c_ohA": [32, LU], "c_ohC": [32, LU], "c_tril": [128, 128],
                "c_ohb": [8, 2048], "c_rmask": [64, 128], "c_rmaskT": [64, 64], "c_reset": [1, S_],
                "c_mi2": [128, 64], "c_msbd": [128, 128], "c_msbdT": [128, 128]}

IN_SHAPES = {
    "x": [S_, D_], "norm_mix_g": [DEPTH, D_], "w_in": [DEPTH, D_, DIN], "pos_bias": [32, 16],
    "sgu_ln_g": [DEPTH, GW], "sgu_wT": [DEPTH, 8, 128, 128], "sgu_bT": [DEPTH, 128, 8],
    "rwkv_mu": [DEPTH, 1984], "rwkv_w0": [DEPTH, GW], "rwkv_w2": [DEPTH, 96, GW], "rwkv_a0": [DEPTH, GW],
    "rwkv_a2": [DEPTH, 96, GW], "rwkv_g2": [DEPTH, 256, GW], "rwkv_k_k": [DEPTH, GW], "rwkv_k_a": [DEPTH, GW],
    "rwkv_r_k": [DEPTH, GW], "rwkv_lnx_g": [DEPTH, GW], "rwkv_lnx_b": [DEPTH, GW],
    "branch_norm_g": [DEPTH, D_], "w_out": [DEPTH, D_, D_], "norm_ffn_g": [DEPTH, D_],
    "w_gate": [DEPTH, D_, DFF], "w_up": [DEPTH, D_, DFF], "w_down": [DEPTH, DFF, D_], "norm_final_g": [1, D_],
}


_UID = [0]
_PROG = {}


def sbt(es, nc, name, shape, dt):
    _UID[0] += 1
    return es.enter_context(nc.sbuf_tensor(f"{name}_u{_UID[0]}", list(shape), dt))


class Ctx:
    pass


def build(debug=False, stages=("pre", "in", "A", "B", "C", "D", "out", "ffn", "fin"), depth=DEPTH):
    from contextlib import ExitStack
    nc = bass.Bass("TRN2", target_bir_lowering=False)
    I = {}
    for k, shp in list(IN_SHAPES.items()) + list(CONST_SHAPES.items()):
        I[k] = nc.dram_tensor(k, list(shp), F32, kind="ExternalInput").ap()
    out = nc.dram_tensor("out", [S_, D_], F32, kind="ExternalOutput").ap()
    skind = "ExternalOutput" if debug else "Internal"

    def scr(name, shape, dt):
        return nc.dram_tensor(name, list(shape), dt, kind=skind).ap()

    G = Ctx()
    G.nc = nc
    G.I = I
    G.out = out
    G.xres = scr("xres", [S_, D_], F32)
    G.qkA = scr("qkA", [1024, S_], BF16)
    G.vA = scr("vA", [S_, GW], BF16)
    G.qkC = scr("qkC", [1024, S_], BF16)
    G.vC = scr("vC", [S_, GW], BF16)
    G.pb = scr("pb", [S_, 1024], F32)
    G.pdT = scr("pdT", [1984, S_], F32)
    G.ycat = scr("ycat", [S_, D_], F32)
    G.actT = scr("actT", [4, 128, (DFF // 128) * 512], BF16)
    G.u2 = scr("u2", [2, 8 * LU], BF16)
    G.uA = scr("uA", [8, 128, LU], BF16)
    G.uC = scr("uC", [8, 128, LU], BF16)
    G.ydr = scr("ydr", [S_, GW], F32)
    G.vtk = scr("vtk", [S_, GW], F32)
    S = Sched(nc)
    G.S = S
    G.ps = [nc.psum_tensor(f"ps{i}", [128, 512], F32).__enter__() for i in range(8)]

    with ExitStack() as es0:
        G.identb = sbt(es0, nc, "identb", [128, 128], BF16)
        G.identf = sbt(es0, nc, "identf", [128, 128], F32)
        S.dma("pool", "c0", G.identb[:], I["c_ident"], w=["identb"])
        S.dma("sp", "c1", G.identf[:], I["c_ident"], w=["identf"])
        if "pre" in stages:
            stage_pre(G)
        S.barrier()
        for l in range(depth):
            xsrc = I["x"] if l == 0 else G.xres
            if "in" in stages:
                stage_inproj(G, l, xsrc)
                S.barrier()
            if "A" in stages:
                stage_attn(G, l, moba=False)
                S.barrier()
            if "B" in stages:
                stage_sgu(G, l)
                S.barrier()
            if "C" in stages:
                stage_attn(G, l, moba=True)
                S.barrier()
            if "D" in stages:
                stage_rwkv(G, l)
                S.barrier()
            if "out" in stages:
                stage_outproj(G, l, xsrc)
                S.barrier()
                with ExitStack() as esl:
                    h2T = sbt(esl, nc, "h2T", [128, NKC, S_], BF16)
                    if "ffn" in stages:
                        stage_ffn_up(G, l, h2T)
                S.barrier()
                if "ffn" in stages:
                    stage_ffn_down(G, l)
                    S.barrier()
        if "fin" in stages:
            stage_final(G)
        S.barrier()
    _PROG['G'] = G
    return nc


def stage_pre(G):
    from contextlib import ExitStack
    nc, S, I = G.nc, G.S, G.I
    with ExitStack() as es:
        pbias = sbt(es, nc, "pbias", [32, 16], F32)
        eb = sbt(es, nc, "eb", [32, 16], F32)
        oh = sbt(es, nc, "oh", [32, 2, LU], F32)
        ub = sbt(es, nc, "ub", [8, 2, LU], BF16)
        S.dma("sp", "c1", pbias[:], I["pos_bias"], w=["pbias"])
        S.dma("sp", "c1", oh[:, 0, :], I["c_ohA"], w=["oh0"])
        S.dma("sp", "c1", oh[:, 1, :], I["c_ohC"], w=["oh1"])
        S.op("act", lambda e: e.activation(out=eb[:], in_=pbias[:], func=AF.Exp), r=["pbias"], w=["eb"])
        for m in range(2):
            for ch in range(LU // 512):
                b = 4 + S.rr("pp", 4)
                S.op("pe", lambda e: e.matmul(G.ps[b][0:8, :], lhsT=eb[:, m * 8:(m + 1) * 8],
                                              rhs=oh[:, m, ch * 512:(ch + 1) * 512], start=True, stop=True),
                     r=["eb", f"oh{m}"], w=[f"ps{b}"])
                S.op("dve", lambda e: e.tensor_copy(out=ub[:, m, ch * 512:(ch + 1) * 512], in_=G.ps[b][0:8, :]),
                     r=[f"ps{b}"], w=[f"ub{m}"])
        big = sbt(es, nc, "ubig", [128, 8 * LU], BF16)
        for m, dst in ((0, G.uA), (1, G.uC)):
            S.dma("sp", "c1", G.u2[m:m + 1, :].rearrange("o (h i) -> (o h) i", h=8), ub[:, m, :], r=[f"ub{m}"], w=["u2"])
            S.dma("sp", "c1", big[:], G.u2[m:m + 1, :].partition_broadcast(128), r=["u2"], w=["ubig"])
            S.dma("sp", "c1", dst.rearrange("h r i -> r h i"), big[:].rearrange("p (h i) -> p h i", h=8), r=["ubig"], w=["uAC"])


def rms_to_T(G, es, src, g_ap, hT, hname, tag, after_chunk=None):
    nc, S = G.nc, G.S
    gb = sbt(es, nc, tag + "gb", [128, D_], F32)
    xt = [sbt(es, nc, tag + f"xt{i}", [128, D_], F32) for i in range(4)]
    hb2 = [sbt(es, nc, tag + f"hb{i}", [128, D_], BF16) for i in range(3)]
    st = sbt(es, nc, tag + "st", [128, 64], F32)
    S.dma("sp", "c1", gb[:], g_ap.partition_broadcast(128), w=[tag + "gb"])

    def load_x(t_):
        S.dma("sp", f"x{t_ % 4}", xt[t_ % 4][:], src[t_ * 128:(t_ + 1) * 128, :], w=[tag + f"xt{t_ % 4}"])

    for t_ in range(4):
        load_x(t_)
    def stage_a(tt):
        i = tt % 4
        hbt = hb2[tt % 3]
        S.op("act", lambda e: e.activation(out=hbt[:], in_=xt[i][:], func=AF.Square,
                                           accum_out=st[:, 4 * tt:4 * tt + 1]),
             r=[tag + f"xt{i}"], w=[tag + f"hb{tt % 3}", tag + f"st{tt}"])
        S.op("dve", lambda e: e.tensor_scalar(out=st[:, 4 * tt + 1:4 * tt + 2], in0=st[:, 4 * tt:4 * tt + 1],
                                              scalar1=1.0 / D_, scalar2=1e-6, op0=ALU.mult, op1=ALU.add),
             r=[tag + f"st{tt}"], w=[tag + f"st{tt}"])
        S.op("act", lambda e: e.activation(out=st[:, 4 * tt + 2:4 * tt + 3], in_=st[:, 4 * tt + 1:4 * tt + 2],
                                           func=AF.Sqrt), r=[tag + f"st{tt}"], w=[tag + f"st{tt}"])
        S.op("dve", lambda e: e.reciprocal(out=st[:, 4 * tt + 3:4 * tt + 4], in_=st[:, 4 * tt + 2:4 * tt + 3]),
             r=[tag + f"st{tt}"], w=[tag + f"st{tt}"])
        S.op("act", lambda e: e.activation(out=xt[i][:], in_=xt[i][:], func=AF.Identity, scale=st[:, 4 * tt + 3:4 * tt + 4]),
             r=[tag + f"xt{i}", tag + f"st{tt}"], w=[tag + f"xt{i}"])
        S.op("dve" if tt % 2 == 0 else "pool",
             lambda e: e.tensor_tensor(out=hbt[:], in0=xt[i][:], in1=gb[:], op=ALU.mult),
             r=[tag + f"xt{i}", tag + "gb"], w=[tag + f"hb{tt % 3}"])
        if tt + 4 < 16:
            load_x(tt + 4)

    def stage_b(tt):
        transpose_rows(G, hb2[tt % 3], tag + f"hb{tt % 3}", hT, f"{hname}{tt // 4}", tt)
        if after_chunk is not None and tt % 4 == 3:
            after_chunk(tt // 4)

    for tt in range(16):
        stage_a(tt)
        if tt >= 1:
            stage_b(tt - 1)
    stage_b(15)


def transpose_rows(G, hb, hbname, hT, hname, tt):
    S = G.S
    for half in range(2):
        b = S.rr("tp", 4)
        pv = G.ps[b][:].bitcast(BF16)
        for k8 in range(8):
            kc = half * 8 + k8
            S.op("pe", lambda e: e.transpose(out=pv[:, k8 * 128:(k8 + 1) * 128], in_=hb[:, kc * 128:(kc + 1) * 128],
                                             identity=G.identb[:]),
                 r=[hbname, "identb"], w=[f"ps{b}"])
        eng = "act" if half == 0 else "dve"
        dst = hT[:, half * 8:(half + 1) * 8, tt * 128:(tt + 1) * 128]
        srcv = pv.rearrange("p (a b) -> p a b", a=8)
        if eng == "act":
            S.op("act", lambda e: e.copy(out=dst, in_=srcv), r=[f"ps{b}"], w=[hname])
        else:
            S.op("dve", lambda e: e.tensor_copy(out=dst, in_=srcv), r=[f"ps{b}"], w=[hname])


def load_w(G, wt, wname, src3, ncols, nk=NKC):
    G.S.dma("pool", wname, wt[:, 0:nk, 0:ncols], src3, w=[wname])


def stage_inproj(G, l, xsrc):
    from contextlib import ExitStack
    nc, S, I = G.nc, G.S, G.I
    groups = [
        ("F", 0, 512, G.qkA, 0, BF16), ("F", 512, 512, G.qkA, 512, BF16), ("T", 1024, 512, G.vA, 0, BF16),
        ("T", 1536, 512, G.pb, 0, F32), ("T", 2048, 512, G.pb, 512, F32),
        ("F", 2560, 512, G.qkC, 0, BF16), ("F", 3072, 512, G.qkC, 512, BF16), ("T", 3584, 512, G.vC, 0, BF16),
        ("F", 4096, 512, G.pdT, 0, F32), ("F", 4608, 96, G.pdT, 512, F32), ("F", 4704, 512, G.pdT, 608, F32),
        ("F", 5216, 512, G.pdT, 1120, F32), ("F", 5728, 96, G.pdT, 1632, F32), ("F", 5824, 256, G.pdT, 1728, F32),
    ]
    with ExitStack() as es:
        hT = sbt(es, nc, "hT", [128, NKC, S_], BF16)
        wt = [sbt(es, nc, f"wt{i}", [128, NKC, 512], BF16) for i in range(2)]
        sg32 = [sbt(es, nc, f"sg32_{i}", [128, 512], F32) for i in range(3)]
        sg16 = [sbt(es, nc, f"sg16_{i}", [128, 512], BF16) for i in range(3)]
        w3 = I["w_in"][l].rearrange("(kc p) c -> p kc c", p=128)

        def issue(gi):
            mode, c0, ncol, dst, d0, dt = groups[gi]
            load_w(G, wt[gi % 2], f"wt{gi % 2}", w3[:, :, c0:c0 + ncol], ncol)

        evc = [0]

        def emit_tile(gi, a, m, tc):
            mode, c0, ncol, dst, d0, dt = groups[gi]
            w = wt[gi % 2]
            wn = f"wt{gi % 2}"
            b = 4 + S.rr("pp", 4)
            for kc in range(NKC):
                if mode == "F":
                    S.op("pe", lambda e: e.matmul(G.ps[b][0:m, :], lhsT=w[:, kc, a * 128:a * 128 + m],
                                                  rhs=hT[:, kc, tc * 512:(tc + 1) * 512],
                                                  start=(kc == 0), stop=(kc == NKC - 1)),
                         r=[wn, f"hT{tc}"], w=[f"ps{b}"])
                else:
                    S.op("pe", lambda e: e.matmul(G.ps[b][:, 0:ncol], lhsT=hT[:, kc, a * 128:(a + 1) * 128],
                                                  rhs=w[:, kc, 0:ncol], start=(kc == 0), stop=(kc == NKC - 1)),
                         r=[wn, f"hT{a // 4}"], w=[f"ps{b}"])
            si = S.rr("sg" + ("32" if dt == F32 else "16"), 3)
            sg = sg32[si] if dt == F32 else sg16[si]
            sgn = ("sg32_" if dt == F32 else "sg16_") + str(si)
            ncl = 512 if mode == "F" else ncol
            evc[0] += 1
            if evc[0] % 2 == 0:
                S.op("act", lambda e: e.copy(out=sg[0:m, 0:ncl], in_=G.ps[b][0:m, 0:ncl]), r=[f"ps{b}"], w=[sgn])
            else:
                S.op("dve", lambda e: e.tensor_copy(out=sg[0:m, 0:ncl], in_=G.ps[b][0:m, 0:ncl]), r=[f"ps{b}"], w=[sgn])
            if mode == "F":
                dap = dst[d0 + a * 128:d0 + a * 128 + m, tc * 512:(tc + 1) * 512]
            else:
                dap = dst[a * 128:(a + 1) * 128, d0:d0 + ncol]
            S.dma("sp", sgn, dap, sg[0:m, 0:ncl], r=[sgn])

        def group_tiles(gi):
            mode, c0, ncol, dst, d0, dt = groups[gi]
            tiles = []
            if mode == "F":
                for mt in range((ncol + 127) // 128):
                    for tc in range(4):
                        tiles.append((mt, min(128, ncol - mt * 128), tc))
            else:
                for tt in range(16):
                    tiles.append((tt, 128, 0))
            return tiles

        issue(0)
        issue(1)

        def after_chunk(tc):
            for gi in (0, 1):
                for (a, m, tcc) in group_tiles(gi):
                    if tcc == tc:
                        emit_tile(gi, a, m, tc)

        rms_to_T(G, es, xsrc, I["norm_mix_g"][l:l + 1, :], hT, "hT", "n", after_chunk=after_chunk)
        issue(2)
        for gi in range(2, len(groups)):
            if gi + 1 < len(groups):
                issue(gi + 1)
            for (a, m, tc) in group_tiles(gi):
                emit_tile(gi, a, m, tc)


def stage_attn(G, l, moba):
    from contextlib import ExitStack
    nc, S, I = G.nc, G.S, G.I
    qk = G.qkC if moba else G.qkA
    vsrc = G.vC if moba else G.vA
    usrc = G.uC if moba else G.uA
    ycol0 = 1024 if moba else 0
    tg = "C" if moba else "A"
    with ExitStack() as es:
        qT = [sbt(es, nc, f"qT{i}", [64, S_], BF16) for i in range(2)]
        kT = [sbt(es, nc, f"kT{i}", [64, S_], BF16) for i in range(2)]
        va = [sbt(es, nc, f"va{i}", [128, 16, 65], BF16) for i in range(2)]
        Tm = [sbt(es, nc, f"Tm{i}", [128, TW], BF16) for i in range(2)]
        Pe = [sbt(es, nc, f"Pe{i}", [128, 512], BF16) for i in range(3)]
        Pb = [sbt(es, nc, f"Pb{i}", [128, 16, 512], BF16) for i in range(2)]
        YH = [sbt(es, nc, f"YH{i}", [128, 16, 64], F32) for i in range(2)]
        rd = sbt(es, nc, "rd", [128, 16], F32)
        if moba:
            ohb = sbt(es, nc, "ohb", [8, 16, 128], BF16)
            kb32 = sbt(es, nc, "kb32", [64, 8], F32)
            kbb = sbt(es, nc, "kbb", [64, 8], BF16)
            gm = sbt(es, nc, "gm", [128, 16, 8], F32)
            mx = sbt(es, nc, "mx", [128, 16, 8], F32)
            nm = sbt(es, nc, "nm", [128, 16, 8], BF16)
            nmT = sbt(es, nc, "nmT", [8, 1024], BF16)
            S.dma("pool", "c0", ohb[:], I["c_ohb"].rearrange("n (a b) -> n a b", a=16), w=["ohb"])
        for i in range(2):
            S.op("dve", lambda e: e.memset(va[i][:, :, 64:65], 1.0), w=[f"va{i}"])

        def load_head(h):
            i = h % 2
            S.dma("sp", f"q{i}", qT[i][:], qk[h * 64:(h + 1) * 64, :], w=[f"qT{i}"])
            S.dma("sp", f"k{i}", kT[i][:], qk[512 + h * 64:512 + (h + 1) * 64, :], w=[f"kT{i}"])
            S.dma("sp", f"v{i}", va[i][:, :, 0:64],
                  vsrc.rearrange("(kb p) c -> p kb c", p=128)[:, :, h * 64:(h + 1) * 64], w=[f"va{i}"])
            tsrc = bass.AP(tensor=usrc.tensor, offset=h * 128 * LU + 127, ap=[[LU - 1, 128], [1, TW]])
            S.dma("sp", f"t{i}", Tm[i][:], tsrc, w=[f"Tm{i}"])

        ust = {}

        def s_phase(h, c):
            i = h % 2
            qn, kn, vn, tn, yn = f"qT{i}", f"kT{i}", f"va{i}", f"Tm{i}", f"YH{i}"
            if c == 0 and moba:
                S.op("dve", lambda e: e.tensor_reduce(out=kb32[:], in_=kT[i][:].rearrange("p (n s) -> p n s", n=8),
                                                      axis=AX.X, op=ALU.add), r=[kn], w=["kb32"])
                S.op("dve", lambda e: e.tensor_scalar(out=kbb[:], in0=kb32[:], scalar1=1.0 / 256, scalar2=None,
                                                      op0=ALU.mult), r=["kb32"], w=["kbb"])
                b = S.rr("st", 4)
                for tt in range(16):
                    S.op("pe", lambda e: e.matmul(G.ps[b][:, tt * 8:(tt + 1) * 8], lhsT=qT[i][:, tt * 128:(tt + 1) * 128],
                                                  rhs=kbb[:], start=True, stop=True), r=[qn, "kbb"], w=[f"ps{b}"])
                S.op("dve", lambda e: e.tensor_copy(out=gm[:].rearrange("p a b -> p (a b)"), in_=G.ps[b][:, 0:128]),
                     r=[f"ps{b}"], w=["gm"] + [f"gm{t_}" for t_ in range(8, 16)] + [f"mx{t_}" for t_ in range(8, 16)])
                S.op("dve", lambda e: e.memset(nm[:], 0.0), w=["nm"] + [f"nm{t_}" for t_ in range(8, 16)])
                for tt in range(8, 16):
                    ob = tt // 2
                    S.op("dve", lambda e: e.memset(gm[:, tt, ob:8], -1e30), r=[], w=[f"gm{tt}"])
                for tt in range(8, 16):
                    S.op("dve", lambda e: e.max(out=mx[:, tt, :], in_=gm[:, tt, :]), r=["gm", f"gm{tt}"], w=[f"mx{tt}"])
                for tt in range(8, 16):
                    ob = tt // 2
                    S.op("dve", lambda e: e.tensor_scalar(out=nm[:, tt, 0:ob], in0=gm[:, tt, 0:ob],
                                                          scalar1=mx[:, tt, 2:3], scalar2=-BIG,
                                                          op0=ALU.is_lt, op1=ALU.mult), r=["gm", f"gm{tt}", f"mx{tt}"], w=[f"nm{tt}"])
                b = S.rr("st", 4)
                pv = G.ps[b][:].bitcast(BF16)
                for tt in range(8, 16):
                    S.op("pe", lambda e: e.transpose(out=pv[0:8, (tt - 8) * 128:(tt - 7) * 128], in_=nm[:, tt, :],
                                                     identity=G.identb[:]), r=["nm", f"nm{tt}", "identb"], w=[f"ps{b}"])
                S.op("dve", lambda e: e.tensor_copy(out=nmT[:], in_=pv[0:8, 0:1024]), r=[f"ps{b}"], w=["nmT"])
            pbi = S.rr("pb", 2)
            pbn = f"Pb{pbi}"
            q0s = {}
            for kb in range(4 * c + 4):
                q0 = max(512 * c, 128 * kb)
                ncol = 512 * c + 512 - q0
                q0s[kb] = q0
                b = S.rr("st", 4)
                mm2 = moba and c >= 2
                S.op("pe", lambda e: e.matmul(G.ps[b][:, 0:ncol], lhsT=kT[i][:, kb * 128:(kb + 1) * 128],
                                              rhs=qT[i][:, q0:q0 + ncol], start=True, stop=not mm2),
                     r=[qn, kn], w=[f"ps{b}"])
                if mm2:
                    S.op("pe", lambda e: e.matmul(G.ps[b][:, 0:ncol], lhsT=ohb[:, kb, :],
                                                  rhs=nmT[:, q0 - 1024:q0 - 1024 + ncol], start=False, stop=True),
                         r=["ohb", "nmT"], w=[f"ps{b}"])
                pi = S.rr("pe_", 3)
                S.op("act", lambda e: e.activation(out=Pe[pi][:, 0:ncol], in_=G.ps[b][:, 0:ncol], func=AF.Exp,
                                                   scale=0.125), r=[f"ps{b}"], w=[f"Pe{pi}"])
                j0 = q0 - 128 * kb + 384
                S.op("dve" if kb % 2 == 0 else "pool",
                     lambda e: e.tensor_tensor(out=Pb[pbi][:, kb, 0:ncol], in0=Pe[pi][:, 0:ncol],
                                               in1=Tm[i][:, j0:j0 + ncol], op=ALU.mult),
                     r=[f"Pe{pi}", tn], w=[pbn])
            ust[(h, c)] = (pbi, q0s)

        def pv_phase(h, c):
            i = h % 2
            vn, yn = f"va{i}", f"YH{i}"
            pbi, q0s = ust.pop((h, c))
            pbn = f"Pb{pbi}"
            for qb in range(4 * c, 4 * c + 4):
                b = 4 + S.rr("pvb", 4)
                for kb in range(qb + 1):
                    off = qb * 128 - q0s[kb]
                    S.op("pe", lambda e: e.matmul(G.ps[b][:, 0:65], lhsT=Pb[pbi][:, kb, off:off + 128],
                                                  rhs=va[i][:, kb, :], start=(kb == 0), stop=(kb == qb)),
                         r=[pbn, vn], w=[f"ps{b}"])
                S.op("dve", lambda e: e.reciprocal(out=rd[:, qb:qb + 1], in_=G.ps[b][:, 64:65]),
                     r=[f"ps{b}"], w=[f"rd{qb}"])
                S.op("dve", lambda e: e.tensor_scalar(out=YH[i][:, qb, :], in0=G.ps[b][:, 0:64],
                                                      scalar1=rd[:, qb:qb + 1], scalar2=None, op0=ALU.mult),
                     r=[f"ps{b}", f"rd{qb}"], w=[yn])
            if c == 3:
                S.dma("sp", f"y{i}", G.ycat.rearrange("(qb p) c -> p qb c", p=128)[:, :, ycol0 + h * 64:ycol0 + (h + 1) * 64],
                      YH[i][:], r=[yn])
                if h + 2 < 8:
                    load_head(h + 2)

        load_head(0)
        load_head(1)
        units = [(h, c) for h in range(8) for c in range(4)]
        for u, (h, c) in enumerate(units):
            s_phase(h, c)
            if u > 0:
                pv_phase(*units[u - 1])
        pv_phase(*units[-1])


def lockstep(lists):
    n = max(len(x) for x in lists)
    for si in range(n):
        for x in lists:
            if si < len(x):
                x[si]()


def stage_sgu(G, l):
    from contextlib import ExitStack
    nc, S, I = G.nc, G.S, G.I
    NS = 4
    with ExitStack() as es:
        wsT = sbt(es, nc, "wsT", [128, 8, 128], BF16)
        wsf = sbt(es, nc, "wsf", [128, 8, 128], F32)
        tril = sbt(es, nc, "tril", [128, 128], F32)
        bT = sbt(es, nc, "bT", [128, 8], F32)
        lg = sbt(es, nc, "lg", [128, GW], F32)
        zt = [sbt(es, nc, f"zt{i}", [128, 1024], F32) for i in range(NS)]
        t1s = [sbt(es, nc, f"t1_{i}", [128, 1024], F32) for i in range(NS)]
        t2s = [sbt(es, nc, f"t2_{i}", [128, 1024], F32) for i in range(NS)]
        vns = [sbt(es, nc, f"vn{i}", [128, GW], BF16) for i in range(NS)]
        yos = [sbt(es, nc, f"yo{i}", [128, GW], F32) for i in range(NS)]
        bss = [sbt(es, nc, f"bs{i}", [128, 6], F32) for i in range(NS)]
        mvs = [sbt(es, nc, f"mv{i}", [128, 4], F32) for i in range(NS)]
        S.dma("sp", "c1", wsf[:], I["sgu_wT"][l].rearrange("g s t -> s g t"), w=["wsf"])
        S.dma("sp", "c1", tril[:], I["c_tril"], w=["tril"])
        S.dma("sp", "c1", bT[:], I["sgu_bT"][l], w=["bT"])
        S.dma("sp", "c1", lg[:], I["sgu_ln_g"][l:l + 1, :].partition_broadcast(128), w=["lg"])
        for g in range(8):
            S.op("dve", lambda e: e.tensor_tensor(out=wsT[:, g, :], in0=wsf[:, g, :], in1=tril[:], op=ALU.mult),
                 r=["wsf", "tril"], w=["wsT"])

        def tile_steps(tt, i):
            z, t1, t2, vn, yo, bs, mv = zt[i], t1s[i], t2s[i], vns[i], yos[i], bss[i], mvs[i]
            zn, t1n, t2n, vnn, yon, bsn, mvn = f"zt{i}", f"t1_{i}", f"t2_{i}", f"vn{i}", f"yo{i}", f"bs{i}", f"mv{i}"
            st = []
            st.append(lambda: S.dma("sp", f"x{i}", z[:], G.pb[tt * 128:(tt + 1) * 128, :], w=[zn]))
            st.append(lambda: S.op("act", lambda e: e.activation(out=t1[:], in_=z[:], func=AF.Square), r=[zn], w=[t1n]))
            st.append(lambda: S.op("dve", lambda e: e.tensor_scalar(out=t1[:], in0=t1[:], scalar1=0.044715, scalar2=1.0, op0=ALU.mult,
                                                                    op1=ALU.add), r=[t1n], w=[t1n]))
            st.append(lambda: S.op("pool", lambda e: e.tensor_tensor(out=t1[:], in0=t1[:], in1=z[:], op=ALU.mult), r=[t1n, zn], w=[t1n]))
            st.append(lambda: S.op("act", lambda e: e.activation(out=t2[:], in_=t1[:], func=AF.Sigmoid, scale=1.5957691216057308),
                                   r=[t1n], w=[t2n]))
            st.append(lambda: S.op("dve", lambda e: e.tensor_tensor(out=t2[:], in0=t2[:], in1=z[:], op=ALU.mult), r=[t2n, zn], w=[t2n]))
            st.append(lambda: S.op("dve", lambda e: e.bn_stats(out=bs[:], in_=t2[:, 512:1024]), r=[t2n], w=[bsn]))
            st.append(lambda: S.op("dve", lambda e: e.bn_aggr(out=mv[:, 0:2], in_=bs[:]), r=[bsn], w=[mvn]))
            st.append(lambda: S.op("dve", lambda e: e.tensor_scalar(out=mv[:, 2:3], in0=mv[:, 1:2], scalar1=1e-5, scalar2=None,
                                                                    op0=ALU.add), r=[mvn], w=[mvn]))
            st.append(lambda: S.op("act", lambda e: e.activation(out=mv[:, 2:3], in_=mv[:, 2:3], func=AF.Sqrt), r=[mvn], w=[mvn]))
            st.append(lambda: S.op("dve", lambda e: e.reciprocal(out=mv[:, 3:4], in_=mv[:, 2:3]), r=[mvn], w=[mvn]))
            st.append(lambda: S.op("dve", lambda e: e.tensor_scalar(out=t1[:, 0:512], in0=t2[:, 512:1024], scalar1=mv[:, 0:1],
                                                                    scalar2=mv[:, 3:4], op0=ALU.subtract, op1=ALU.mult),
                                   r=[t2n, mvn], w=[t1n]))
            st.append(lambda: S.op("pool", lambda e: e.tensor_tensor(out=vn[:], in0=t1[:, 0:512], in1=lg[:], op=ALU.mult),
                                   r=[t1n, "lg"], w=[vnn]))

            def mm():
                b = S.rr("st", 4)
                for g in range(8):
                    S.op("pe", lambda e: e.matmul(G.ps[b][:, g * 64:(g + 1) * 64], lhsT=wsT[:, g, :],
                                                  rhs=vn[:, g * 64:(g + 1) * 64], start=True, stop=True),
                         r=["wsT", vnn], w=[f"ps{b}"])
                S.op("dve", lambda e: e.tensor_tensor(out=t1[:, 512:1024].rearrange("p (g c) -> p g c", g=8),
                                                      in0=G.ps[b][:].rearrange("p (g c) -> p g c", g=8),
                                                      in1=bT[:].unsqueeze(2).to_broadcast([128, 8, 64]), op=ALU.add),
                     r=[f"ps{b}", "bT"], w=[t1n])
            st.append(mm)
            st.append(lambda: S.op("pool", lambda e: e.tensor_tensor(out=yo[:], in0=t1[:, 512:1024], in1=t2[:, 0:512], op=ALU.mult),
                                   r=[t1n, t2n], w=[yon]))
            st.append(lambda: S.dma("sp", f"y{i}", G.ycat[tt * 128:(tt + 1) * 128, 512:1024], yo[:], r=[yon]))
            return st

        for t0 in range(0, 16, NS):
            lockstep([tile_steps(t0 + i, i) for i in range(NS)])


def stage_outproj(G, l, xsrc):
    from contextlib import ExitStack
    nc, S, I = G.nc, G.S, G.I
    with ExitStack() as es:
        yT = sbt(es, nc, "yT", [128, NKC, S_], BF16)
        wt = [sbt(es, nc, f"wo{i}", [128, NKC, 512], BF16) for i in range(2)]
        w3 = I["w_out"][l].rearrange("(kc p) c -> p kc c", p=128)
        load_w(G, wt[0], "wt0", w3[:, :, 0:512], 512)
        load_w(G, wt[1], "wt1", w3[:, :, 512:1024], 512)
        gb = sbt(es, nc, "bgb", [128, D_], F32)
        yt = [sbt(es, nc, f"byt{i}", [128, D_], F32) for i in range(4)]
        yb = [sbt(es, nc, f"byb{i}", [128, D_], BF16) for i in range(3)]
        st = sbt(es, nc, "bst", [128, 16, 16], F32)
        xo = [sbt(es, nc, f"xo{i}", [128, 512], F32) for i in range(3)]
        xn = [sbt(es, nc, f"xn{i}", [128, 512], F32) for i in range(3)]
        order = [(cg, tc * 4 + t4) for tc in range(4) for cg in (0, 1) for t4 in range(4)]
        order += [(cg, tt) for cg in (2, 3) for tt in range(16)]
        pos = [0]

        def load_xo(k):
            cg, tt = order[k]
            S.dma("sp", f"xo{k % 3}", xo[k % 3][:], xsrc[tt * 128:(tt + 1) * 128, cg * 512:(cg + 1) * 512], w=[f"xo{k % 3}"])

        def emit_tile():
            k = pos[0]
            pos[0] += 1
            cg, tt = order[k]
            w = wt[cg % 2]
            wn = f"wt{cg % 2}"
            si = k % 3
            if k + 2 < len(order):
                load_xo(k + 2)
            b = 4 + S.rr("pp", 4)
            for kc in range(NKC):
                S.op("pe", lambda e: e.matmul(G.ps[b][:, :], lhsT=yT[:, kc, tt * 128:(tt + 1) * 128], rhs=w[:, kc, :],
                                              start=(kc == 0), stop=(kc == NKC - 1)), r=[wn, f"yT{tt // 4}"], w=[f"ps{b}"])
            S.op("dve", lambda e: e.tensor_tensor(out=xn[si][:], in0=G.ps[b][:, :], in1=xo[si][:], op=ALU.add),
                 r=[f"ps{b}", f"xo{si}"], w=[f"xn{si}"])
            S.dma("sp", f"xn{si}", G.xres[tt * 128:(tt + 1) * 128, cg * 512:(cg + 1) * 512], xn[si][:], r=[f"xn{si}"])

        S.dma("sp", "c1", gb[:], I["branch_norm_g"][l:l + 1, :].partition_broadcast(128), w=["bgb"])
        load_xo(0)
        load_xo(1)

        def load_y(t_):
            S.dma("sp", f"x{t_ % 4}", yt[t_ % 4][:], G.ycat[t_ * 128:(t_ + 1) * 128, :], w=[f"byt{t_ % 4}"])

        for t_ in range(4):
            load_y(t_)
        def stage_a(tt):
            i = tt % 4
            i2 = tt % 3
            for br in range(4):
                S.op("act", lambda e: e.activation(out=yb[i2][:, br * 512:(br + 1) * 512],
                                                   in_=yt[i][:, br * 512:(br + 1) * 512], func=AF.Square,
                                                   accum_out=st[:, tt, br:br + 1]),
                     r=[f"byt{i}"], w=[f"byb{i2}", f"bst{tt}"])
            S.op("dve", lambda e: e.tensor_scalar(out=st[:, tt, 4:8], in0=st[:, tt, 0:4], scalar1=1.0 / GW,
                                                  scalar2=1e-6, op0=ALU.mult, op1=ALU.add),
                 r=[f"bst{tt}"], w=[f"bst{tt}"])
            S.op("act", lambda e: e.activation(out=st[:, tt, 8:12], in_=st[:, tt, 4:8], func=AF.Sqrt),
                 r=[f"bst{tt}"], w=[f"bst{tt}"])
            S.op("dve", lambda e: e.reciprocal(out=st[:, tt, 12:16], in_=st[:, tt, 8:12]),
                 r=[f"bst{tt}"], w=[f"bst{tt}"])
            for br in range(4):
                S.op("dve", lambda e: e.scalar_tensor_tensor(out=yb[i2][:, br * 512:(br + 1) * 512],
                                                             in0=yt[i][:, br * 512:(br + 1) * 512],
                                                             scalar=st[:, tt, 12 + br:13 + br],
                                                             in1=gb[:, br * 512:(br + 1) * 512],
                                                             op0=ALU.mult, op1=ALU.mult),
                     r=[f"byt{i}", f"bst{tt}", "bgb"], w=[f"byb{i2}"])
            if tt + 4 < 16:
                load_y(tt + 4)

        def stage_b(tt):
            transpose_rows(G, yb[tt % 3], f"byb{tt % 3}", yT, f"yT{tt // 4}", tt)
            if tt % 4 == 3:
                for _ in range(8):
                    emit_tile()

        for tt in range(16):
            stage_a(tt)
            if tt >= 1:
                stage_b(tt - 1)
        stage_b(15)
        load_w(G, wt[0], "wt0", w3[:, :, 1024:1536], 512)
        load_w(G, wt[1], "wt1", w3[:, :, 1536:2048], 512)
        while pos[0] < len(order):
            emit_tile()


def stage_ffn_up(G, l, h2T):
    from contextlib import ExitStack
    nc, S, I = G.nc, G.S, G.I
    with ExitStack() as es:
        wg = [sbt(es, nc, f"wg{i}", [128, NKC, 512], BF16) for i in range(2)]
        wu = [sbt(es, nc, f"wu{i}", [128, NKC, 512], BF16) for i in range(2)]
        sg = [sbt(es, nc, f"fs{i}", [128, 512], F32) for i in range(3)]
        ao = [sbt(es, nc, f"ao{i}", [128, 512], BF16) for i in range(3)]
        g3 = I["w_gate"][l].rearrange("(kc p) c -> p kc c", p=128)
        u3 = I["w_up"][l].rearrange("(kc p) c -> p kc c", p=128)

        def issue(gi):
            load_w(G, wg[gi % 2], f"wg{gi % 2}", g3[:, :, gi * 512:(gi + 1) * 512], 512)
            load_w(G, wu[gi % 2], f"wu{gi % 2}", u3[:, :, gi * 512:(gi + 1) * 512], 512)

        def emit_tile(gi, mt, tc):
            j = gi % 2
            bg = S.rr("st", 4)
            bu = 4 + S.rr("pp", 4)
            for kc in range(NKC):
                S.op("pe", lambda e: e.matmul(G.ps[bg][:, :], lhsT=wg[j][:, kc, mt * 128:(mt + 1) * 128],
                                              rhs=h2T[:, kc, tc * 512:(tc + 1) * 512], start=(kc == 0),
                                              stop=(kc == NKC - 1)), r=[f"wg{j}", f"h2T{tc}"], w=[f"ps{bg}"])
            for kc in range(NKC):
                S.op("pe", lambda e: e.matmul(G.ps[bu][:, :], lhsT=wu[j][:, kc, mt * 128:(mt + 1) * 128],
                                              rhs=h2T[:, kc, tc * 512:(tc + 1) * 512], start=(kc == 0),
                                              stop=(kc == NKC - 1)), r=[f"wu{j}", f"h2T{tc}"], w=[f"ps{bu}"])
            si = S.rr("fs", 3)
            S.op("act", lambda e: e.activation(out=sg[si][:], in_=G.ps[bg][:, :], func=AF.Silu),
                 r=[f"ps{bg}"], w=[f"fs{si}"])
            S.op("dve", lambda e: e.tensor_tensor(out=ao[si][:], in0=G.ps[bu][:, :], in1=sg[si][:], op=ALU.mult),
                 r=[f"ps{bu}", f"fs{si}"], w=[f"ao{si}"])
            jj = gi * 4 + mt
            S.dma("sp", f"ao{si}", G.actT[tc, :, jj * 512:(jj + 1) * 512], ao[si][:], r=[f"ao{si}"])

        issue(0)
        issue(1)

        def after_chunk(tc):
            for mt in range(4):
                emit_tile(0, mt, tc)

        rms_to_T(G, es, G.xres, I["norm_ffn_g"][l:l + 1, :], h2T, "h2T", "f", after_chunk=after_chunk)
        for gi in range(1, DFF // 512):
            if gi + 1 < DFF // 512:
                issue(gi + 1)
            for mt in range(4):
                for tc in range(4):
                    emit_tile(gi, mt, tc)


def stage_ffn_down(G, l):
    from contextlib import ExitStack
    nc, S, I = G.nc, G.S, G.I
    NJ = DFF // 128
    with ExitStack() as es:
        wd = [sbt(es, nc, f"wd{i}", [128, NJ, 512], BF16) for i in range(2)]
        at = [sbt(es, nc, f"at{i}", [128, NJ, 512], BF16) for i in range(2)]
        xo = [sbt(es, nc, f"dxo{i}", [128, 512], F32) for i in range(3)]
        xn = [sbt(es, nc, f"dxn{i}", [128, 512], F32) for i in range(3)]
        d3 = I["w_down"][l].rearrange("(j p) c -> p j c", p=128)

        def issue(cg):
            for hf in range(2):
                G.S.dma("pool", f"wd{cg % 2}", wd[cg % 2][:, hf * (NJ // 2):(hf + 1) * (NJ // 2), :],
                        d3[:, hf * (NJ // 2):(hf + 1) * (NJ // 2), cg * 512:(cg + 1) * 512], w=[f"wd{cg % 2}"])

        issue(0)
        units = [(cg, tc) for cg in range(4) for tc in range(4)]

        def load_at(u):
            cg, tc = units[u]
            S.dma("sp", f"at{u % 2}", at[u % 2][:].rearrange("p j t -> p (j t)"), G.actT[tc], w=[f"at{u % 2}"])

        def load_xo(k):
            u, t4 = divmod(k, 4)
            cg, tc = units[u]
            tt = tc * 4 + t4
            S.dma("sp", f"xo{k % 3}", xo[k % 3][:], G.xres[tt * 128:(tt + 1) * 128, cg * 512:(cg + 1) * 512],
                  w=[f"dxo{k % 3}"])

        load_at(0)
        load_xo(0)
        load_xo(1)
        for u, (cg, tc) in enumerate(units):
            if tc == 0 and cg + 1 < 4:
                issue(cg + 1)
            if u + 1 < len(units):
                load_at(u + 1)
            w = wd[cg % 2]
            wn = f"wd{cg % 2}"
            ai = u % 2
            for t4 in range(4):
                k = u * 4 + t4
                tt = tc * 4 + t4
                si = k % 3
                if k + 2 < 4 * len(units):
                    load_xo(k + 2)
                b = 4 + S.rr("pp", 4)
                for j in range(NJ):
                    S.op("pe", lambda e: e.matmul(G.ps[b][:, :], lhsT=at[ai][:, j, t4 * 128:(t4 + 1) * 128],
                                                  rhs=w[:, j, :], start=(j == 0), stop=(j == NJ - 1)),
                         r=[wn, f"at{ai}"], w=[f"ps{b}"])
                S.op("dve", lambda e: e.tensor_tensor(out=xn[si][:], in0=G.ps[b][:, :], in1=xo[si][:], op=ALU.add),
                     r=[f"ps{b}", f"dxo{si}"], w=[f"dxn{si}"])
                S.dma("sp", f"xn{si}", G.xres[tt * 128:(tt + 1) * 128, cg * 512:(cg + 1) * 512], xn[si][:],
                      r=[f"dxn{si}"])


def stage_final(G):
    from contextlib import ExitStack
    nc, S, I = G.nc, G.S, G.I
    with ExitStack() as es:
        gb = sbt(es, nc, "fgb", [128, D_], F32)
        xt = [sbt(es, nc, f"fxt{i}", [128, D_], F32) for i in range(2)]
        ot = [sbt(es, nc, f"fot{i}", [128, D_], F32) for i in range(2)]
        st = sbt(es, nc, "fst", [128, 64], F32)
        S.dma("sp", "c1", gb[:], I["norm_final_g"][0:1, :].partition_broadcast(128), w=["fgb"])
        for tt in range(16):
            i = tt % 2
            S.dma("sp", f"x{i}", xt[i][:], G.xres[tt * 128:(tt + 1) * 128, :], w=[f"fxt{i}"])
            S.op("act", lambda e: e.activation(out=ot[i][:], in_=xt[i][:], func=AF.Square, accum_out=st[:, 4 * tt:4 * tt + 1]),
                 r=[f"fxt{i}"], w=[f"fot{i}", f"fst{tt}"])
            S.op("dve", lambda e: e.tensor_scalar(out=st[:, 4 * tt + 1:4 * tt + 2], in0=st[:, 4 * tt:4 * tt + 1],
                                                  scalar1=1.0 / D_, scalar2=1e-6, op0=ALU.mult, op1=ALU.add),
                 r=[f"fst{tt}"], w=[f"fst{tt}"])
            S.op("act", lambda e: e.activation(out=st[:, 4 * tt + 2:4 * tt + 3], in_=st[:, 4 * tt + 1:4 * tt + 2],
                                               func=AF.Sqrt), r=[f"fst{tt}"], w=[f"fst{tt}"])
            S.op("dve", lambda e: e.reciprocal(out=st[:, 4 * tt + 3:4 * tt + 4], in_=st[:, 4 * tt + 2:4 * tt + 3]),
                 r=[f"fst{tt}"], w=[f"fst{tt}"])
            S.op("dve", lambda e: e.scalar_tensor_tensor(out=ot[i][:], in0=xt[i][:], scalar=st[:, 4 * tt + 3:4 * tt + 4],
                                                         in1=gb[:], op0=ALU.mult, op1=ALU.mult),
                 r=[f"fxt{i}", f"fst{tt}", "fgb"], w=[f"fot{i}"])
            S.dma("sp", f"y{i}", G.out[tt * 128:(tt + 1) * 128, :], ot[i][:], r=[f"fot{i}"])


IN_SHAPES["rwkv_pk"] = [DEPTH, 128, 4, 8]
IN_SHAPES["rwkv_mu2"] = [DEPTH, 128, 4]
for _k in ("rwkv_mu", "rwkv_w0", "rwkv_a0", "rwkv_k_k", "rwkv_k_a", "rwkv_r_k"):
    IN_SHAPES.pop(_k)
CONST_SHAPES["c_rmask"] = [64, 128]


def stage_rwkv(G, l):
    from contextlib import ExitStack
    nc, S, I = G.nc, G.S, G.I
    NCH = 32
    with ExitStack() as es:
        tw = sbt(es, nc, "tw", [96, S_], BF16)
        adb = sbt(es, nc, "adb", [96, S_], BF16)
        sgb = sbt(es, nc, "sgb", [128, 2, S_], BF16)
        w2b = sbt(es, nc, "w2b", [96, GW], BF16)
        a2b = sbt(es, nc, "a2b", [96, GW], BF16)
        g2b = sbt(es, nc, "g2b", [128, 2, GW], BF16)
        pk = sbt(es, nc, "pk", [128, 4, 8], F32)
        omk = sbt(es, nc, "omk", [128, 4], F32)
        ones2 = sbt(es, nc, "ones2", [128, 128], BF16)
        bones = sbt(es, nc, "bones", [128, 2], BF16)
        E2 = sbt(es, nc, "E2", [128, 64], F32)
        mu2 = sbt(es, nc, "mu2", [128, 4], F32)
        rst = sbt(es, nc, "rst", [128, S_], F32)
        mi2 = sbt(es, nc, "mi2", [128, 64], F32)
        msbd = sbt(es, nc, "msbd", [128, 128], F32)
        msbdT = sbt(es, nc, "msbdT", [128, 128], F32)
        ones = sbt(es, nc, "ones", [64, 64], BF16)
        bon = sbt(es, nc, "bon", [128, 16, 8], F32)
        S.dma("pool", "c0", w2b[:], I["rwkv_w2"][l], w=["w2b"])
        S.dma("pool", "c0", a2b[:], I["rwkv_a2"][l], w=["a2b"])
        S.dma("pool", "c0", g2b[:], I["rwkv_g2"][l].rearrange("(j p) c -> p j c", p=128), w=["g2b"])
        S.dma("sp", "c1", pk[:], I["rwkv_pk"][l], w=["pk"])
        S.dma("sp", "c1", mu2[:], I["rwkv_mu2"][l], w=["mu2"])
        S.dma("sp", "c1", rst[:], I["c_reset"].partition_broadcast(128), w=["rst"])
        S.dma("sp", "c1", mi2[:], I["c_mi2"], w=["mi2"])
        S.dma("sp", "c1", msbd[:], I["c_msbd"], w=["msbd"])
        S.dma("sp", "c1", msbdT[:], I["c_msbdT"], w=["msbdT"])
        S.op("dve", lambda e: e.memset(ones[:], 1.0), w=["ones"])
        S.op("dve", lambda e: e.memset(ones2[:], 0.0), w=["ones2"])
        S.op("dve", lambda e: e.memset(bones[:], 0.0), w=["bones"])
        for hh in range(2):
            S.op("dve", lambda e: e.memset(ones2[hh * 64:(hh + 1) * 64, hh * 64:(hh + 1) * 64], 1.0), w=["ones2"])
            S.op("dve", lambda e: e.memset(bones[hh * 64:(hh + 1) * 64, hh:hh + 1], 1.0), w=["bones"])
        S.op("dve", lambda e: e.tensor_tensor(out=E2[:], in0=G.identf[:, 0:64], in1=G.identf[:, 64:128], op=ALU.add),
             r=["identf"], w=["E2"])
        S.op("dve", lambda e: e.tensor_scalar(out=omk[:], in0=pk[:, :, 6], scalar1=-1.0, scalar2=1.0, op0=ALU.mult,
                                              op1=ALU.add), r=["pk"], w=["omk"])
        with ExitStack() as e1:
            raw = sbt(e1, nc, "raw", [128, S_ + 1], F32)
            dd = sbt(e1, nc, "dd", [128, S_], F32)
            for (r0, nr, mcol, kind) in ((512, 96, 0, "w"), (1632, 96, 1, "a"), (1728, 128, 2, "g0"), (1856, 128, 3, "g1")):
                S.op("dve", lambda e: e.memset(raw[0:nr, 0:1], 0.0), w=["raw"])
                S.dma("sp", "x0", raw[0:nr, 1:S_ + 1], G.pdT[r0:r0 + nr, :], w=["raw"])
                S.op("dve", lambda e: e.tensor_tensor(out=dd[0:nr, :], in0=raw[0:nr, 0:S_], in1=raw[0:nr, 1:S_ + 1],
                                                      op=ALU.subtract), r=["raw"], w=["dd"])
                S.op("dve", lambda e: e.scalar_tensor_tensor(out=dd[0:nr, :], in0=dd[0:nr, :], scalar=mu2[0:nr, mcol:mcol + 1],
                                                             in1=raw[0:nr, 1:S_ + 1], op0=ALU.mult, op1=ALU.add),
                     r=["dd", "raw", "mu2"], w=["dd"])
                if kind == "w":
                    S.op("act", lambda e: e.activation(out=tw[:], in_=dd[0:96, :], func=AF.Tanh), r=["dd"], w=["tw"])
                elif kind == "a":
                    S.op("act", lambda e: e.copy(out=adb[:], in_=dd[0:96, :]), r=["dd"], w=["adb"])
                else:
                    j = 0 if kind == "g0" else 1
                    S.op("act", lambda e: e.activation(out=sgb[:, j, :], in_=dd[:, :], func=AF.Sigmoid), r=["dd"], w=["sgb"])
            S.barrier()

        KT = sbt(es, nc, "KT", [128, S_], BF16)
        BT = sbt(es, nc, "BT", [128, S_], BF16)
        AR = sbt(es, nc, "AR", [128, NCH, 128], BF16)
        AT = sbt(es, nc, "AT", [128, S_], BF16)
        DR = sbt(es, nc, "DR", [128, NCH, 128], BF16)
        KBr = sbt(es, nc, "KBr", [128, S_], BF16)
        BBr = sbt(es, nc, "BBr", [128, S_], BF16)
        VB = sbt(es, nc, "VB", [128, S_], BF16)
        RK = sbt(es, nc, "RK", [128, S_], BF16)
        vsf = sbt(es, nc, "vsf", [128, S_], F32)
        for h in range(8):
          if h % 2 == 0:
            pr = h // 2
            with ExitStack() as e1:
                T = [sbt(e1, nc, f"T{i}", [128, S_ + 1], F32) for i in range(3)]
                U = [sbt(e1, nc, f"U{i}", [128, S_], F32) for i in range(8)]
                SQ = sbt(e1, nc, "SQ", [128, S_], BF16)
                WC = sbt(e1, nc, "WC", [128, NCH], F32)
                cC = sbt(e1, nc, "cC", [128, NCH], F32)
                VS = sbt(e1, nc, "VS", [128, 16, 128], F32)

                def P(j):
                    return pk[:, pr, j:j + 1]

                rows = (0, 608, 1120)
                for ti in range(3):
                    S.op("dve", lambda e: e.memset(T[ti][:, 0:1], 0.0), w=[f"T{ti}"])
                for ti in range(3):
                    S.dma("sp", ("x0", "x1", "q0")[ti], T[ti][:, 1:S_ + 1],
                          G.pdT[rows[ti] + pr * 128:rows[ti] + (pr + 1) * 128, :], w=[f"T{ti}"])

                def shift(ti, mu_j, dst, dname, tmp, tname):
                    S.op("pool", lambda e: e.tensor_tensor(out=tmp[:], in0=T[ti][:, 0:S_], in1=T[ti][:, 1:S_ + 1],
                                                           op=ALU.subtract), r=[f"T{ti}"], w=[tname])
                    S.op("dve", lambda e: e.scalar_tensor_tensor(out=dst, in0=tmp[:], scalar=P(mu_j), in1=T[ti][:, 1:S_ + 1],
                                                                 op0=ALU.mult, op1=ALU.add), r=[tname, f"T{ti}", "pk"], w=[dname])

                rs, ks, lw, asg, kkn = U[0], U[1], U[2], U[3], U[4]
                shift(0, 0, rs[:], "U0", U[5], "U5")
                shift(1, 1, ks[:], "U1", U[6], "U6")
                shift(2, 2, vsf[:], "vsf", U[7], "U7")
                cum = T[0]
                S.op("act", lambda e: e.copy(out=VB[:], in_=vsf[:]), r=["vsf"], w=["VB"])
                for half in range(2):
                    for q4 in range(2):
                        b = S.rr("st", 4)
                        for t4 in range(4):
                            tt = half * 8 + q4 * 4 + t4
                            S.op("pe", lambda e: e.transpose(out=G.ps[b][:, t4 * 128:(t4 + 1) * 128], in_=vsf[:, tt * 128:(tt + 1) * 128],
                                                             identity=G.identf[:, :]), r=["vsf", "identf"], w=[f"ps{b}"])
                        S.op("act", lambda e: e.copy(out=VS[:, half * 8 + q4 * 4:half * 8 + q4 * 4 + 4, :].rearrange("p a b -> p (a b)"),
                                                     in_=G.ps[b][:, :]), r=[f"ps{b}"], w=["VS"])
                S.dma("sp", "y1", G.vtk.rearrange("(tt p) v -> p tt v", p=128)[:, :, pr * 128:(pr + 1) * 128], VS[:, :, :],
                      r=["VS"])
                for tc in range(4):
                    b = S.rr("st", 4)
                    S.op("pe", lambda e: e.matmul(G.ps[b][:, :], lhsT=w2b[:, pr * 128:(pr + 1) * 128],
                                                  rhs=tw[:, tc * 512:(tc + 1) * 512], start=True, stop=True),
                         r=["w2b", "tw"], w=[f"ps{b}"])
                    S.op("act", lambda e: e.activation(out=lw[:, tc * 512:(tc + 1) * 512], in_=G.ps[b][:, :],
                                                       func=AF.Sigmoid, bias=P(3)), r=[f"ps{b}", "pk"], w=["U2"])
                    b = S.rr("st", 4)
                    S.op("pe", lambda e: e.matmul(G.ps[b][:, :], lhsT=a2b[:, pr * 128:(pr + 1) * 128],
                                                  rhs=adb[:, tc * 512:(tc + 1) * 512], start=True, stop=True),
                         r=["a2b", "adb"], w=[f"ps{b}"])
                    S.op("act", lambda e: e.activation(out=asg[:, tc * 512:(tc + 1) * 512], in_=G.ps[b][:, :],
                                                       func=AF.Sigmoid, bias=P(4)), r=[f"ps{b}", "pk"], w=["U3"])
                S.op("pool", lambda e: e.tensor_scalar(out=lw[:], in0=lw[:], scalar1=-math.exp(-0.5), scalar2=0.0,
                                                       op0=ALU.mult, op1=ALU.add), r=["U2"], w=["U2"])
                S.op("dve", lambda e: e.tensor_scalar(out=kkn[:], in0=ks[:], scalar1=P(5), scalar2=None, op0=ALU.mult),
                     r=["U1", "pk"], w=["U4"])
                S.op("pool", lambda e: e.tensor_tensor(out=SQ[:], in0=kkn[:], in1=kkn[:], op=ALU.mult), r=["U4"], w=["SQ"])
                for tc in range(4):
                    b = S.rr("st", 4)
                    S.op("pe", lambda e: e.matmul(G.ps[b][:, :], lhsT=ones2[:], rhs=SQ[:, tc * 512:(tc + 1) * 512],
                                                  start=True, stop=True), r=["ones2", "SQ"], w=[f"ps{b}"])
                    S.op("act", lambda e: e.activation(out=U[5][:, tc * 512:(tc + 1) * 512], in_=G.ps[b][:, :],
                                                       func=AF.Sqrt), r=[f"ps{b}"], w=["U5"])
                S.op("dve", lambda e: e.tensor_scalar(out=U[5][:], in0=U[5][:], scalar1=1e-12, scalar2=None, op0=ALU.max),
                     r=["U5"], w=["U5"])
                S.op("dve", lambda e: e.reciprocal(out=U[5][:], in_=U[5][:]), r=["U5"], w=["U5"])
                S.op("pool", lambda e: e.tensor_tensor(out=kkn[:], in0=kkn[:], in1=U[5][:], op=ALU.mult),
                     r=["U4", "U5"], w=["U4"])
                S.op("dve", lambda e: e.tensor_scalar(out=U[6][:], in0=asg[:], scalar1=P(6), scalar2=omk[:, pr:pr + 1],
                                                      op0=ALU.mult, op1=ALU.add), r=["U3", "pk", "omk"], w=["U6"])
                S.op("pool", lambda e: e.tensor_tensor(out=ks[:], in0=ks[:], in1=U[6][:], op=ALU.mult),
                     r=["U1", "U6"], w=["U1"])
                bb = U[7]
                S.op("dve", lambda e: e.tensor_tensor(out=bb[:], in0=kkn[:], in1=asg[:], op=ALU.mult),
                     r=["U4", "U3"], w=["U7"])
                S.op("dve", lambda e: e.scalar_tensor_tensor(out=RK[:], in0=rs[:], scalar=P(7), in1=ks[:], op0=ALU.mult,
                                                             op1=ALU.mult), r=["U0", "U1", "pk"], w=["RK"])
                b = S.rr("st", 4)
                for tt in range(16):
                    S.op("pe", lambda e: e.matmul(G.ps[b][:, 2 * tt:2 * tt + 2], lhsT=RK[:, tt * 128:(tt + 1) * 128], rhs=bones[:, :],
                                                  start=True, stop=True), r=["RK", "bones"], w=[f"ps{b}"])
                S.op("act", lambda e: e.copy(out=bon[:, :, 2 * pr:2 * pr + 2], in_=G.ps[b][:, 0:32].rearrange("p (t h) -> p t h", h=2)),
                     r=[f"ps{b}"], w=["bon"])
                S.op("dve", lambda e: e.tensor_tensor_scan(out=cum[:, 0:S_], data0=rst[:], data1=lw[:], initial=0.0,
                                                           op0=ALU.mult, op1=ALU.add), r=["rst", "U2"], w=["T0"])
                cum3 = cum[:, 0:S_].rearrange("p (c t) -> p c t", t=64)
                S.op("dve", lambda e: e.tensor_copy(out=cC[:], in_=cum3[:, :, 63]), r=["T0"], w=["cC"])
                S.op("act", lambda e: e.activation(out=WC[:], in_=cC[:], func=AF.Exp), r=["cC"], w=["WC"])
                S.op("dve", lambda e: e.tensor_tensor(out=DR[:, :, 0:64],
                                                      in0=E2[:, :].unsqueeze(1).to_broadcast([128, NCH, 64]),
                                                      in1=WC[:].unsqueeze(2).to_broadcast([128, NCH, 64]), op=ALU.mult),
                     r=["E2", "WC"], w=["DR"])
                ex = T[1]
                ex2 = T[2]
                S.op("act", lambda e: e.activation(out=ex[:, 0:S_], in_=cum[:, 0:S_], func=AF.Exp), r=["T0"], w=["T1"])
                S.op("act", lambda e: e.activation(out=ex2[:, 0:S_], in_=cum[:, 0:S_], func=AF.Exp, scale=-1.0),
                     r=["T0", "vsf"], w=["T2"])
                S.op("pool", lambda e: e.tensor_tensor(out=rs[:], in0=rs[:], in1=ex[:, 0:S_], op=ALU.mult),
                     r=["U0", "T1"], w=["U0"])
                S.op("act", lambda e: e.copy(out=AR[:, :, 0:64], in_=rs[:].rearrange("p (c t) -> p c t", t=64)),
                     r=["U0"], w=["AR"])
                S.op("act", lambda e: e.copy(out=DR[:, :, 64:128], in_=rs[:].rearrange("p (c t) -> p c t", t=64)),
                     r=["U0"], w=["DR"])
                S.op("pool", lambda e: e.tensor_tensor(out=KT[:], in0=ks[:], in1=ex2[:, 0:S_], op=ALU.mult),
                     r=["U1", "T2"], w=["KT"])
                S.op("dve", lambda e: e.tensor_tensor(out=BT[:], in0=bb[:], in1=ex2[:, 0:S_], op=ALU.mult),
                     r=["U7", "T2"], w=["BT"])
                S.op("pool", lambda e: e.tensor_tensor(out=lw[:], in0=cum[:, 0:S_], in1=lw[:], op=ALU.subtract),
                     r=["T0", "U2"], w=["U2"])
                S.op("act", lambda e: e.activation(out=lw[:], in_=lw[:], func=AF.Exp), r=["U2"], w=["U2"])
                S.op("dve", lambda e: e.scalar_tensor_tensor(out=AT[:], in0=kkn[:], scalar=-1.0, in1=lw[:],
                                                             op0=ALU.mult, op1=ALU.mult), r=["U4", "U2"], w=["AT"])
                S.op("act", lambda e: e.copy(out=AR[:, :, 64:128], in_=AT[:].rearrange("p (c t) -> p c t", t=64)),
                     r=["AT"], w=["AR"])
                S.op("dve", lambda e: e.tensor_tensor(out=ex[:, 0:S_].rearrange("p (c t) -> p c t", t=64),
                                                      in0=cC[:].unsqueeze(2).to_broadcast([128, NCH, 64]), in1=cum3,
                                                      op=ALU.subtract), r=["cC", "T0", "T1"], w=["T1"])
                S.op("act", lambda e: e.activation(out=ex[:, 0:S_], in_=ex[:, 0:S_], func=AF.Exp), r=["T1"], w=["T1"])
                S.op("pool", lambda e: e.tensor_tensor(out=KBr[:], in0=ks[:], in1=ex[:, 0:S_], op=ALU.mult),
                     r=["U1", "T1"], w=["KBr"])
                S.op("dve", lambda e: e.tensor_tensor(out=BBr[:], in0=bb[:], in1=ex[:, 0:S_], op=ALU.mult),
                     r=["U7", "T1"], w=["BBr"])
                S.barrier()
          if True:
            base = 64 * (h % 2)
            hp = slice(base, base + 64)
            with ExitStack() as e2:
                NQ = NCH // 2
                Xp = sbt(e2, nc, "Xp", [128, NQ, 128], BF16)
                W1p = sbt(e2, nc, "W1p", [128, NQ, 128], BF16)
                W2p = sbt(e2, nc, "W2p", [128, NQ, 128], BF16)
                Vtp = sbt(e2, nc, "Vtp", [128, NQ, 64], BF16)
                AK = sbt(e2, nc, "AK", [128, NQ, 128], BF16)
                AN = [sbt(e2, nc, f"AN{i}", [128, NQ, 256], BF16) for i in range(2)]
                GRT = sbt(e2, nc, "GRT", [64, NCH, 128], BF16)
                HYb = sbt(e2, nc, "HYb", [128, NCH, 64], BF16)
                YD = sbt(e2, nc, "YD", [128, NCH, 64], F32)
                DRs = sbt(e2, nc, "DRs", [64, NCH, 128], BF16)
                STb = [sbt(e2, nc, f"STb{i}", [64, 64], BF16) for i in range(2)]
                idbb = G.identb[hp, hp]
                if base == 0:
                    DRv = DR[0:64, :, :]
                else:
                    for c4 in range(NCH // 4):
                        b = S.rr("all", 8)
                        S.op("pe", lambda e: e.matmul(G.ps[b][0:64, :], lhsT=G.identb[:, 64:128],
                                                      rhs=DR[:, c4 * 4:(c4 + 1) * 4, :].rearrange("p c x -> p (c x)"),
                                                      start=True, stop=True), r=["DR", "identb"], w=[f"ps{b}"])
                        o_ = DRs[:, c4 * 4:(c4 + 1) * 4, :].rearrange("p c x -> p (c x)")
                        if c4 % 2 == 0:
                            S.op("act", lambda e: e.copy(out=o_, in_=G.ps[b][0:64, :]), r=[f"ps{b}"], w=["DRs"])
                        else:
                            S.op("dve", lambda e: e.tensor_copy(out=o_, in_=G.ps[b][0:64, :]), r=[f"ps{b}"], w=["DRs"])
                    DRv = DRs[:, :, :]
                for (src, sname, dst3, dname, eng) in ((AT, "AT", Xp[:, :, 0:64], "XpA", "act"), (BBr, "BBr", W1p[:, :, 0:64], "W1A", "dve"),
                                                       (KBr, "KBr", W2p[:, :, 0:64], "W2A", "act"), (VB, "VB", Vtp[:, :, :], "Vtp", "dve")):
                    b = S.rr("all", 8)
                    pv = G.ps[b][:].bitcast(BF16)
                    for q in range(NQ):
                        S.op("pe", lambda e: e.transpose(out=pv[:, q * 64:(q + 1) * 64], in_=src[hp, q * 128:(q + 1) * 128],
                                                         identity=idbb), r=[sname, "identb"], w=[f"ps{b}"])
                    i_ = pv[:, 0:1024].rearrange("p (q k) -> p q k", k=64)
                    if eng == "act":
                        S.op("act", lambda e: e.copy(out=dst3, in_=i_), r=[f"ps{b}"], w=[dname])
                    else:
                        S.op("dve", lambda e: e.tensor_copy(out=dst3, in_=i_), r=[f"ps{b}"], w=[dname])

                def group_steps(gq):
                    q0 = gq * 4
                    g = f"_{gq}"
                    steps = []

                    def s0():
                        for (srcT, sn, Wp, wtok, dstbd, dtok) in ((BT, "BT", W1p, "W1" + g, AN[0], "AN0" + g),
                                                                    (KT, "KT", W2p, "W2" + g, AK, "AK" + g)):
                            for hb2 in range(2):
                                b = S.rr("all", 8)
                                for qi in range(2):
                                    q = q0 + hb2 * 2 + qi
                                    S.op("pe", lambda e: e.matmul(G.ps[b][:, qi * 256:(qi + 1) * 256], lhsT=srcT[hp, q * 128:(q + 1) * 128],
                                                                  rhs=AR[hp, 2 * q:2 * q + 2, :].rearrange("p c x -> p (c x)"),
                                                                  start=True, stop=True), r=[sn, "AR"], w=[f"ps{b}"])
                                qs = slice(q0 + hb2 * 2, q0 + hb2 * 2 + 2)
                                pq = G.ps[b][:, :].rearrange("p (q x) -> p q x", q=2)
                                for par in range(2):
                                    rows = slice(par * 64, par * 64 + 64)
                                    S.op("dve", lambda e: e.tensor_tensor(out=Wp[rows, qs, 64:128], in0=pq[rows, :, par * 128:par * 128 + 64],
                                                                          in1=mi2[rows, :].unsqueeze(1).to_broadcast([64, 2, 64]), op=ALU.mult),
                                         r=[f"ps{b}", "mi2"], w=[wtok])
                                p4 = G.ps[b][:, :].rearrange("p (q a x) -> p q a x", q=2, a=2)
                                if dstbd is AK:
                                    o4 = AK[:, qs, :].rearrange("p q (a x) -> p q a x", a=2)
                                else:
                                    o4 = AN[0][:, qs, 128:256].rearrange("p q (a x) -> p q a x", a=2)
                                S.op("dve", lambda e: e.tensor_tensor(out=o4, in0=p4[:, :, :, 64:128],
                                                                      in1=msbd[:, :].rearrange("p (a x) -> p a x", a=2).unsqueeze(1).to_broadcast([128, 2, 2, 64]),
                                                                      op=ALU.mult), r=[f"ps{b}", "msbd"], w=[dtok])
                        b = S.rr("all", 8)
                        for qi in range(4):
                            q = q0 + qi
                            S.op("pe", lambda e: e.matmul(G.ps[b][:, qi * 128:(qi + 1) * 128], lhsT=AT[hp, q * 128:(q + 1) * 128],
                                                          rhs=BT[hp, q * 128:(q + 1) * 128], start=True, stop=True), r=["AT", "BT"], w=[f"ps{b}"])
                        S.op("dve", lambda e: e.tensor_tensor(out=AN[0][:, q0:q0 + 4, 0:128],
                                                              in0=G.ps[b][:, :].rearrange("p (q x) -> p q x", q=4),
                                                              in1=msbdT[:, :].unsqueeze(1).to_broadcast([128, 4, 128]), op=ALU.mult),
                             r=[f"ps{b}", "msbdT"], w=["AN0" + g])
                    steps.append(s0)

                    def s1():
                        b = S.rr("all", 8)
                        for qi in range(4):
                            q = q0 + qi
                            S.op("pe", lambda e: e.matmul(G.ps[b][:, qi * 64:(qi + 1) * 64], lhsT=AK[:, q, :], rhs=Vtp[:, q, :],
                                                          start=True, stop=True), r=["AK" + g, "Vtp"], w=[f"ps{b}"])
                        S.op("act", lambda e: e.copy(out=Xp[:, q0:q0 + 4, 64:128],
                                                     in_=G.ps[b][:, 0:256].rearrange("p (q x) -> p q x", q=4)),
                             r=[f"ps{b}"], w=["Xp" + g])
                    steps.append(s1)

                    def mkx(j):
                        def sx():
                            an = AN[j % 2]
                            ann = f"AN{j % 2}" + g
                            b = S.rr("all", 8)
                            for qi in range(4):
                                q = q0 + qi
                                S.op("pe", lambda e: e.matmul(G.ps[b][:, qi * 128:(qi + 1) * 128], lhsT=an[:, q, 128:256], rhs=Xp[:, q, :],
                                                              start=True, stop=True), r=[ann, "Xp" + g, "XpA"], w=[f"ps{b}"])
                            if j < 5:
                                for hb2 in range(2):
                                    bq = S.rr("all", 8)
                                    for qi in range(2):
                                        q = q0 + hb2 * 2 + qi
                                        S.op("pe", lambda e: e.matmul(G.ps[bq][:, qi * 256:qi * 256 + 128], lhsT=an[:, q, 128:256],
                                                                      rhs=an[:, q, 0:128], start=True, stop=True), r=[ann], w=[f"ps{bq}"])
                                        S.op("pe", lambda e: e.matmul(G.ps[bq][:, qi * 256 + 128:(qi + 1) * 256], lhsT=an[:, q, 0:128],
                                                                      rhs=an[:, q, 128:256], start=True, stop=True), r=[ann], w=[f"ps{bq}"])
                                    S.op("act", lambda e: e.copy(out=AN[(j + 1) % 2][:, q0 + hb2 * 2:q0 + hb2 * 2 + 2, :],
                                                                 in_=G.ps[bq][:, :].rearrange("p (q x) -> p q x", q=2)),
                                         r=[f"ps{bq}"], w=[f"AN{(j + 1) % 2}" + g])
                            S.op("dve", lambda e: e.tensor_tensor(out=Xp[:, q0:q0 + 4, :], in0=Xp[:, q0:q0 + 4, :],
                                                                  in1=G.ps[b][:, :].rearrange("p (q x) -> p q x", q=4), op=ALU.add),
                                 r=[f"ps{b}", "Xp" + g, "XpA"], w=["Xp" + g, "XpA" + g])
                        return sx
                    for j in range(6):
                        steps.append(mkx(j))

                    def s8():
                        for par in range(2):
                            rows = slice(par * 64, par * 64 + 64)
                            b = S.rr("all", 8)
                            for qi in range(4):
                                q = q0 + qi
                                S.op("pe", lambda e: e.matmul(G.ps[b][0:64, qi * 128:(qi + 1) * 128], lhsT=Xp[rows, q, 0:64],
                                                              rhs=W1p[rows, q, 0:128], start=True, stop=True),
                                     r=["Xp" + g, "W1" + g, "W1A"], w=[f"ps{b}"])
                            cs = slice(2 * q0 + par, 2 * q0 + 8, 2)
                            S.op("dve", lambda e: e.tensor_tensor(out=GRT[:, cs, :], in0=G.ps[b][0:64, :].rearrange("p (c x) -> p c x", c=4),
                                                                  in1=DRv[:, cs, :], op=ALU.add), r=[f"ps{b}", "DRs", "DR"], w=["GRT" + g])
                        for par in range(2):
                            rows = slice(par * 64, par * 64 + 64)
                            b = S.rr("all", 8)
                            for qi in range(4):
                                q = q0 + qi
                                S.op("pe", lambda e: e.matmul(G.ps[b][:, qi * 64:(qi + 1) * 64], lhsT=W1p[rows, q, 0:128],
                                                              rhs=Xp[rows, q, 64:128], start=True, stop=False),
                                     r=["Xp" + g, "W1" + g, "W1A"], w=[f"ps{b}"])
                                S.op("pe", lambda e: e.matmul(G.ps[b][:, qi * 64:(qi + 1) * 64], lhsT=W2p[rows, q, 0:128],
                                                              rhs=Vtp[rows, q, :], start=False, stop=True),
                                     r=["Vtp", "W2" + g, "W2A"], w=[f"ps{b}"])
                            cs = slice(2 * q0 + par, 2 * q0 + 8, 2)
                            S.op("act", lambda e: e.copy(out=HYb[:, cs, :], in_=G.ps[b][:, 0:256].rearrange("p (c x) -> p c x", c=4)),
                                 r=[f"ps{b}"], w=["HYb" + g])
                    steps.append(s8)
                    return steps

                for batch in range(2):
                    lists = [group_steps(batch * 2 + gg) for gg in range(2)]
                    for si in range(len(lists[0])):
                        for gg in range(2):
                            lists[gg][si]()
                S.op("dve", lambda e: e.memset(STb[0][:], 0.0), w=["STb0"])
                for c in range(NCH):
                    si = c % 2
                    g = f"_{c // 8}"
                    b = S.rr("all", 8)
                    S.op("pe", lambda e: e.matmul(G.ps[b][:, 0:64], lhsT=GRT[:, c, :], rhs=STb[si][:], start=True, stop=False),
                         r=["GRT" + g, f"STb{si}"], w=[f"ps{b}"])
                    S.op("pe", lambda e: e.matmul(G.ps[b][:, 0:64], lhsT=G.identb[:, :], rhs=HYb[:, c, :], start=False, stop=True),
                         r=["HYb" + g, "identb"], w=[f"ps{b}"])
                    S.op("dve", lambda e: e.tensor_copy(out=STb[1 - si][:], in_=G.ps[b][0:64, 0:64]),
                         r=[f"ps{b}"], w=[f"STb{1 - si}"])
                    S.op("act", lambda e: e.copy(out=YD[64:128, c, :], in_=G.ps[b][64:128, 0:64]), r=[f"ps{b}"], w=["YD"])
                S.dma("sp", "y0", G.ydr.rearrange("(c t) v -> t c v", t=64)[:, :, h * 64:(h + 1) * 64], YD[64:128, :, :],
                      r=["YD"])
                S.barrier()

        with ExitStack() as e3:
            lgx = sbt(e3, nc, "lgx", [128, GW], F32)
            lbx = sbt(e3, nc, "lbx", [128, GW], F32)
            yt = [sbt(e3, nc, f"eyt{i}", [128, GW], F32) for i in range(2)]
            vt = [sbt(e3, nc, f"evt{i}", [128, GW], F32) for i in range(2)]
            sqs = [sbt(e3, nc, f"esq{i}", [128, GW], F32) for i in range(2)]
            yo = [sbt(e3, nc, f"eyo{i}", [128, GW], F32) for i in range(2)]
            stts = [sbt(e3, nc, f"est{i}", [128, 8, 8], F32) for i in range(2)]
            S.dma("sp", "c1", lgx[:], I["rwkv_lnx_g"][l:l + 1, :].partition_broadcast(128), w=["lgx"])
            S.dma("sp", "c1", lbx[:], I["rwkv_lnx_b"][l:l + 1, :].partition_broadcast(128), w=["lbx"])

            def ep_steps(tt, i):
                y, sq, stt = yt[i], sqs[i], stts[i]
                yn, sqn, stn = f"eyt{i}", f"esq{i}", f"est{i}"
                y3 = y[:].rearrange("p (h v) -> p h v", h=8)
                st = []
                st.append(lambda: S.dma("sp", f"x{i}", y[:], G.ydr[tt * 128:(tt + 1) * 128, :], w=[yn]))
                st.append(lambda: S.dma("sp", f"q{i}", vt[i][:], G.vtk[tt * 128:(tt + 1) * 128, :], w=[f"evt{i}"]))
                st.append(lambda: S.op("dve", lambda e: e.tensor_reduce(out=stt[:, 0, :], in_=y3, axis=AX.X, op=ALU.add), r=[yn], w=[stn]))
                st.append(lambda: S.op("act", lambda e: e.activation(out=sq[:], in_=y[:], func=AF.Square), r=[yn], w=[sqn]))
                st.append(lambda: S.op("dve", lambda e: e.tensor_reduce(out=stt[:, 1, :], in_=sq[:].rearrange("p (h v) -> p h v", h=8),
                                                                        axis=AX.X, op=ALU.add), r=[sqn], w=[stn]))
                st.append(lambda: S.op("dve", lambda e: e.tensor_scalar(out=stt[:, 2, :], in0=stt[:, 0, :], scalar1=1.0 / 64, scalar2=None,
                                                                        op0=ALU.mult), r=[stn], w=[stn]))
                st.append(lambda: S.op("dve", lambda e: e.tensor_tensor(out=stt[:, 3, :], in0=stt[:, 2, :], in1=stt[:, 2, :], op=ALU.mult),
                                       r=[stn], w=[stn]))
                st.append(lambda: S.op("dve", lambda e: e.scalar_tensor_tensor(out=stt[:, 4, :], in0=stt[:, 1, :], scalar=1.0 / 64,
                                                                               in1=stt[:, 3, :], op0=ALU.mult, op1=ALU.subtract),
                                       r=[stn], w=[stn]))
                st.append(lambda: S.op("dve", lambda e: e.tensor_scalar(out=stt[:, 4, :], in0=stt[:, 4, :], scalar1=64e-5, scalar2=None,
                                                                        op0=ALU.add), r=[stn], w=[stn]))
                st.append(lambda: S.op("act", lambda e: e.activation(out=stt[:, 5, :], in_=stt[:, 4, :], func=AF.Sqrt), r=[stn], w=[stn]))
                st.append(lambda: S.op("dve", lambda e: e.reciprocal(out=stt[:, 6, :], in_=stt[:, 5, :]), r=[stn], w=[stn]))
                st.append(lambda: S.op("dve", lambda e: e.tensor_tensor(out=y3, in0=y3, in1=stt[:, 2, :].unsqueeze(2).to_broadcast([128, 8, 64]),
                                                                        op=ALU.subtract), r=[yn, stn], w=[yn]))
                st.append(lambda: S.op("dve", lambda e: e.tensor_tensor(out=y3, in0=y3, in1=stt[:, 6, :].unsqueeze(2).to_broadcast([128, 8, 64]),
                                                                        op=ALU.mult), r=[yn, stn], w=[yn]))
                st.append(lambda: S.op("pool", lambda e: e.tensor_tensor(out=sq[:].rearrange("p (h v) -> p h v", h=8),
                                                                         in0=vt[i][:].rearrange("p (h v) -> p h v", h=8),
                                                                         in1=bon[:, tt, :].unsqueeze(2).to_broadcast([128, 8, 64]), op=ALU.mult),
                                       r=[f"evt{i}", "bon", sqn], w=[sqn]))
                st.append(lambda: S.op("pool", lambda e: e.tensor_tensor(out=y[:], in0=y[:], in1=lgx[:], op=ALU.mult), r=[yn, "lgx"], w=[yn]))
                st.append(lambda: S.op("pool", lambda e: e.tensor_tensor(out=y[:], in0=y[:], in1=lbx[:], op=ALU.add), r=[yn, "lbx"], w=[yn]))
                st.append(lambda: S.op("pool", lambda e: e.tensor_tensor(out=y[:], in0=y[:], in1=sq[:], op=ALU.add), r=[yn, sqn], w=[yn]))

                def gate():
                    b = 4 + S.rr("pp", 4)
                    for j in range(2):
                        S.op("pe", lambda e: e.matmul(G.ps[b][:, :], lhsT=sgb[:, j, tt * 128:(tt + 1) * 128], rhs=g2b[:, j, :],
                                                      start=(j == 0), stop=(j == 1)), r=["sgb", "g2b"], w=[f"ps{b}"])
                    S.op("dve", lambda e: e.tensor_tensor(out=yo[i][:], in0=G.ps[b][:, :], in1=y[:], op=ALU.mult),
                         r=[f"ps{b}", yn], w=[f"eyo{i}"])
                st.append(gate)
                st.append(lambda: S.dma("sp", f"y{i}", G.ycat[tt * 128:(tt + 1) * 128, 1536:2048], yo[i][:], r=[f"eyo{i}"]))
                return st

            for t0 in range(0, 16, 2):
                lockstep([ep_steps(t0 + i, i) for i in range(2)])
            S.barrier()


def prepare_inputs(inp):
    f = lambda a: np.ascontiguousarray(np.asarray(a, dtype=np.float32))
    shared = {}
    for k in ("norm_mix_g", "w_in", "pos_bias", "sgu_ln_g", "rwkv_w2", "rwkv_a2", "rwkv_g2", "rwkv_lnx_g", "rwkv_lnx_b",
              "branch_norm_g", "w_out", "norm_ffn_g", "w_gate", "w_up", "w_down"):
        shared[k] = f(inp[k])
    shared["norm_final_g"] = f(inp["norm_final_g"]).reshape(1, D_)
    shared["sgu_wT"] = f(np.transpose(np.asarray(inp["sgu_w"]), (0, 1, 3, 2)))
    shared["sgu_bT"] = f(np.transpose(np.asarray(inp["sgu_b"]), (0, 2, 1)))
    mu = np.asarray(inp["rwkv_mu"], dtype=np.float32)
    hk = lambda a: np.asarray(a, dtype=np.float32).reshape(DEPTH, 8, 64).transpose(0, 2, 1)
    pk = np.stack([hk(mu[:, 0:512]), hk(mu[:, 608:1120]), hk(mu[:, 1120:1632]), hk(inp["rwkv_w0"]), hk(inp["rwkv_a0"]),
                   hk(inp["rwkv_k_k"]), hk(inp["rwkv_k_a"]), hk(inp["rwkv_r_k"])], axis=-1)
    pk = pk.reshape(DEPTH, 64, 4, 2, 8).transpose(0, 3, 1, 2, 4).reshape(DEPTH, 128, 4, 8)
    shared["rwkv_pk"] = f(pk)
    mu2 = np.zeros((DEPTH, 128, 4), np.float32)
    mu2[:, 0:96, 0] = mu[:, 512:608]
    mu2[:, 0:96, 1] = mu[:, 1632:1728]
    mu2[:, :, 2] = mu[:, 1728:1856]
    mu2[:, :, 3] = mu[:, 1856:1984]
    shared["rwkv_mu2"] = mu2
    shared.update(host_constants())
    x = f(inp["x"])
    return [dict(shared, x=x[b]) for b in range(NCORES)]


def kernel(**inputs):
    if "nc" not in _PROG:
        _PROG["nc"] = build()
    in_maps = prepare_inputs(inputs)
    res = run_bass_kernel_spmd(_PROG["nc"], in_maps, core_ids=list(range(NCORES)))
    return np.stack([np.asarray(r["out"], dtype=np.float32) for r in res.results], axis=0)
```

```python
import math
import numpy as np
import concourse.bass as bass
import concourse.mybir as mybir
from concourse.bass_utils import run_bass_kernel_spmd

F32 = mybir.dt.float32
BF16 = mybir.dt.bfloat16
AF = mybir.ActivationFunctionType
ALU = mybir.AluOpType
AX = mybir.AxisListType

S_ = 2048
D_ = 2048
DEPTH = 2
NCORES = 8
GW = 512
DIN = 6080
DFF = 5632
NKC = 16
LU = 2560
TW = 2432
BIG = 30000.0


class Sched:
    def __init__(self, nc):
        self.nc = nc
        self.eng = {"pe": nc.tensor, "dve": nc.vector, "act": nc.scalar, "pool": nc.gpsimd, "sp": nc.sync}
        self.sem = {k: nc.alloc_semaphore(f"se_{k}") for k in self.eng}
        self.cnt = {k: 0 for k in self.eng}
        self.dsem = {}
        self.dtot = {}
        self.waited = {k: {} for k in self.eng}
        self.last_w = {}
        self.readers = {}
        self.nbuf = 0
        self.rot = {}

    def sb(self, name, shape, dt):
        return self.nc.sbuf_tensor(name, list(shape), dt).__enter__()

    def _deps(self, r, w):
        evs = []
        for t in r:
            if t in self.last_w:
                evs.append(self.last_w[t])
        for t in w:
            if t in self.last_w:
                evs.append(self.last_w[t])
            evs.extend(self.readers.get(t, ()))
        return evs

    def _wait(self, e, evs):
        need = {}
        for (key, val) in evs:
            if key == e and e == "pe":
                continue
            if val > need.get(key, 0):
                need[key] = val
        for key, val in need.items():
            if self.waited[e].get(key, 0) >= val:
                continue
            if key in self.sem:
                sem = self.sem[key]
            else:
                sem = self.dsem[key]
                val = max(val, self.dtot[key])
            self.eng[e].wait_ge(sem, val)
            self.waited[e][key] = val

    def _commit(self, ev, r, w):
        for t in w:
            self.last_w[t] = ev
            self.readers[t] = []
        for t in r:
            self.readers.setdefault(t, []).append(ev)

    def op(self, e, fn, r=(), w=()):
        self._wait(e, self._deps(r, w))
        inst = fn(self.eng[e])
        inst.then_inc(self.sem[e], 1)
        self.cnt[e] += 1
        self._commit((e, self.cnt[e]), r, w)

    def dma(self, q, key, out, in_, r=(), w=(), **kw):
        if key not in self.dsem:
            self.dsem[key] = self.nc.alloc_semaphore(f"sd_{key}")
            self.dtot[key] = 0
        self._wait(q, self._deps(r, w))
        self.eng[q].dma_start(out=out, in_=in_, **kw).then_inc(self.dsem[key], 16)
        self.dtot[key] += 16
        self._commit((key, self.dtot[key]), r, w)

    def barrier(self):
        evs = [(k, v) for k, v in self.cnt.items() if v > 0]
        evs += [(k, v) for k, v in self.dtot.items() if v > 0]
        for e in self.eng:
            self._wait(e, [ev for ev in evs if ev[0] != e])
        self.last_w = {}
        self.readers = {}

    def rr(self, name, n):
        i = self.rot.get(name, 0)
        self.rot[name] = (i + 1) % n
        return i


def t5_bucket_np(dist):
    dist = np.maximum(dist, 0)
    d = np.maximum(dist, 1).astype(np.float32)
    large = 16 + (np.log(d / np.float32(16)) / np.float32(math.log(2048 / 16)) * np.float32(16)).astype(np.int32)
    large = np.minimum(large, 31)
    return np.where(dist < 16, dist, large)


def host_constants():
    c = {}
    c["c_ident"] = np.eye(128, dtype=np.float32)
    d = np.arange(LU) - 511
    bk = t5_bucket_np(d)
    cntA = np.zeros(LU, np.float32)
    for (wdw, dil) in ((128, 1), (512, 4), (2048, 16)):
        cntA += ((d >= 0) & (d % dil == 0) & (d <= wdw)).astype(np.float32)
    ohA = np.zeros((32, LU), np.float32)
    ohC = np.zeros((32, LU), np.float32)
    ohA[bk, np.arange(LU)] = cntA
    ohC[bk, np.arange(LU)] = (d >= 0).astype(np.float32)
    c["c_ohA"] = ohA
    c["c_ohC"] = ohC
    s = np.arange(128)
    c["c_tril"] = (s[:, None] <= s[None, :]).astype(np.float32)
    ohb = np.zeros((8, 16, 128), np.float32)
    for kb in range(16):
        ohb[kb // 2, kb, :] = 1.0
    c["c_ohb"] = ohb.reshape(8, 16 * 128)
    i = np.arange(64)
    m = np.zeros((64, 128), np.float32)
    m[:, :64] = (i[:, None] <= i[None, :])
    m[:, 64:] = (i[:, None] < i[None, :])
    c["c_rmask"] = m
    c["c_rmaskT"] = (i[:, None] > i[None, :]).astype(np.float32)
    p_ = np.arange(128)
    par, ii = p_ // 64, p_ % 64
    c["c_mi2"] = (ii[:, None] <= i[None, :]).astype(np.float32)
    c["c_msbd"] = ((par[:, None] == par[None, :]) & (ii[:, None] < ii[None, :])).astype(np.float32)
    c["c_msbdT"] = ((par[:, None] == par[None, :]) & (ii[:, None] > ii[None, :])).astype(np.float32)
    rs = np.ones((1, S_), np.float32)
    rs[0, ::64] = 0.0
    c["c_reset"] = rs
    return c


CONST_SHAPES = {"c_ident": [128, 128], "c_ohA": [32, LU], "c_ohC": [32, LU], "c_tril": [128, 128],
                "c_ohb": [8, 2048], "c_rmask": [64, 128], "c_rmaskT": [64, 64], "c_reset": [1, S_],
                "c_mi2": [128, 64], "c_msbd": [128, 128], "c_msbdT": [128, 128]}

IN_SHAPES = {
    "x": [S_, D_], "norm_mix_g": [DEPTH, D_], "w_in": [DEPTH, D_, DIN], "pos_bias": [32, 16],
    "sgu_ln_g": [DEPTH, GW], "sgu_wT": [DEPTH, 8, 128, 128], "sgu_bT": [DEPTH, 128, 8],
    "rwkv_mu": [DEPTH, 1984], "rwkv_w0": [DEPTH, GW], "rwkv_w2": [DEPTH, 96, GW], "rwkv_a0": [DEPTH, GW],
    "rwkv_a2": [DEPTH, 96, GW], "rwkv_g2": [DEPTH, 256, GW], "rwkv_k_k": [DEPTH, GW], "rwkv_k_a": [DEPTH, GW],
    "rwkv_r_k": [DEPTH, GW], "rwkv_lnx_g": [DEPTH, GW], "rwkv_lnx_b": [DEPTH, GW],
    "branch_norm_g": [DEPTH, D_], "w_out": [DEPTH, D_, D_], "norm_ffn_g": [DEPTH, D_],
    "w_gate": [DEPTH, D_, DFF], "w_up": [DEPTH, D_, DFF], "w_down": [DEPTH, DFF, D_], "norm_final_g": [1, D_],
}


_UID = [0]
_PROG = {}


def sbt(es, nc, name, shape, dt):
    _UID[0] += 1
    return es.enter_context(nc.sbuf_tensor(f"{name}_u{_UID[0]}", list(shape), dt))


class Ctx:
    pass


def build(debug=False, stages=("pre", "in", "A", "B", "C", "D", "out", "ffn", "fin"), depth=DEPTH):
    from contextlib import ExitStack
    nc = bass.Bass("TRN2", target_bir_lowering=False)
    I = {}
    for k, shp in list(IN_SHAPES.items()) + list(CONST_SHAPES.items()):
        I[k] = nc.dram_tensor(k, list(shp), F32, kind="ExternalInput").ap()
    out = nc.dram_tensor("out", [S_, D_], F32, kind="ExternalOutput").ap()
    skind = "ExternalOutput" if debug else "Internal"

    def scr(name, shape, dt):
        return nc.dram_tensor(name, list(shape), dt, kind=skind).ap()

    G = Ctx()
    G.nc = nc
    G.I = I
    G.out = out
    G.xres = scr("xres", [S_, D_], F32)
    G.qkA = scr("qkA", [1024, S_], BF16)
    G.vA = scr("vA", [S_, GW], BF16)
    G.qkC = scr("qkC", [1024, S_], BF16)
    G.vC = scr("vC", [S_, GW], BF16)
    G.pb = scr("pb", [S_, 1024], F32)
    G.pdT = scr("pdT", [1984, S_], F32)
    G.ycat = scr("ycat", [S_, D_], F32)
    G.actT = scr("actT", [4, 128, (DFF // 128) * 512], BF16)
    G.u2 = scr("u2", [2, 8 * LU], BF16)
    G.uA = scr("uA", [8, 128, LU], BF16)
    G.uC = scr("uC", [8, 128, LU], BF16)
    G.ydr = scr("ydr", [S_, GW], F32)
    G.vtk = scr("vtk", [S_, GW], F32)
    S = Sched(nc)
    G.S = S
    G.ps = [nc.psum_tensor(f"ps{i}", [128, 512], F32).__enter__() for i in range(8)]

    with ExitStack() as es0:
        G.identb = sbt(es0, nc, "identb", [128, 128], BF16)
        G.identf = sbt(es0, nc, "identf", [128, 128], F32)
        S.dma("pool", "c0", G.identb[:], I["c_ident"], w=["identb"])
        S.dma("sp", "c1", G.identf[:], I["c_ident"], w=["identf"])
        if "pre" in stages:
            stage_pre(G)
        S.barrier()
        for l in range(depth):
            xsrc = I["x"] if l == 0 else G.xres
            if "in" in stages:
                stage_inproj(G, l, xsrc)
                S.barrier()
            if "A" in stages:
                stage_attn(G, l, moba=False)
                S.barrier()
            if "B" in stages:
                stage_sgu(G, l)
                S.barrier()
            if "C" in stages:
                stage_attn(G, l, moba=True)
                S.barrier()
            if "D" in stages:
                stage_rwkv(G, l)
                S.barrier()
            if "out" in stages:
                stage_outproj(G, l, xsrc)
                S.barrier()
                with ExitStack() as esl:
                    h2T = sbt(esl, nc, "h2T", [128, NKC, S_], BF16)
                    if "ffn" in stages:
                        stage_ffn_up(G, l, h2T)
                S.barrier()
                if "ffn" in stages:
                    stage_ffn_down(G, l)
                    S.barrier()
        if "fin" in stages:
            stage_final(G)
        S.barrier()
    _PROG['G'] = G
    return nc


def stage_pre(G):
    from contextlib import ExitStack
    nc, S, I = G.nc, G.S, G.I
    with ExitStack() as es:
        pbias = sbt(es, nc, "pbias", [32, 16], F32)
        eb = sbt(es, nc, "eb", [32, 16], F32)
        oh = sbt(es, nc, "oh", [32, 2, LU], F32)
        ub = sbt(es, nc, "ub", [8, 2, LU], BF16)
        S.dma("sp", "c1", pbias[:], I["pos_bias"], w=["pbias"])
        S.dma("sp", "c1", oh[:, 0, :], I["c_ohA"], w=["oh0"])
        S.dma("sp", "c1", oh[:, 1, :], I["c_ohC"], w=["oh1"])
        S.op("act", lambda e: e.activation(out=eb[:], in_=pbias[:], func=AF.Exp), r=["pbias"], w=["eb"])
        for m in range(2):
            for ch in range(LU // 512):
                b = 4 + S.rr("pp", 4)
                S.op("pe", lambda e: e.matmul(G.ps[b][0:8, :], lhsT=eb[:, m * 8:(m + 1) * 8],
                                              rhs=oh[:, m, ch * 512:(ch + 1) * 512], start=True, stop=True),
                     r=["eb", f"oh{m}"], w=[f"ps{b}"])
                S.op("dve", lambda e: e.tensor_copy(out=ub[:, m, ch * 512:(ch + 1) * 512], in_=G.ps[b][0:8, :]),
                     r=[f"ps{b}"], w=[f"ub{m}"])
        big = sbt(es, nc, "ubig", [128, 8 * LU], BF16)
        for m, dst in ((0, G.uA), (1, G.uC)):
            S.dma("sp", "c1", G.u2[m:m + 1, :].rearrange("o (h i) -> (o h) i", h=8), ub[:, m, :], r=[f"ub{m}"], w=["u2"])
            S.dma("sp", "c1", big[:], G.u2[m:m + 1, :].partition_broadcast(128), r=["u2"], w=["ubig"])
            S.dma("sp", "c1", dst.rearrange("h r i -> r h i"), big[:].rearrange("p (h i) -> p h i", h=8), r=["ubig"], w=["uAC"])


def rms_to_T(G, es, src, g_ap, hT, hname, tag, after_chunk=None):
    nc, S = G.nc, G.S
    gb = sbt(es, nc, tag + "gb", [128, D_], F32)
    xt = [sbt(es, nc, tag + f"xt{i}", [128, D_], F32) for i in range(4)]
    hb2 = [sbt(es, nc, tag + f"hb{i}", [128, D_], BF16) for i in range(3)]
    st = sbt(es, nc, tag + "st", [128, 64], F32)
    S.dma("sp", "c1", gb[:], g_ap.partition_broadcast(128), w=[tag + "gb"])

    def load_x(t_):
        S.dma("sp", f"x{t_ % 4}", xt[t_ % 4][:], src[t_ * 128:(t_ + 1) * 128, :], w=[tag + f"xt{t_ % 4}"])

    for t_ in range(4):
        load_x(t_)
    def stage_a(tt):
        i = tt % 4
        hbt = hb2[tt % 3]
        S.op("act", lambda e: e.activation(out=hbt[:], in_=xt[i][:], func=AF.Square,
                                           accum_out=st[:, 4 * tt:4 * tt + 1]),
             r=[tag + f"xt{i}"], w=[tag + f"hb{tt % 3}", tag + f"st{tt}"])
        S.op("dve", lambda e: e.tensor_scalar(out=st[:, 4 * tt + 1:4 * tt + 2], in0=st[:, 4 * tt:4 * tt + 1],
                                              scalar1=1.0 / D_, scalar2=1e-6, op0=ALU.mult, op1=ALU.add),
             r=[tag + f"st{tt}"], w=[tag + f"st{tt}"])
        S.op("act", lambda e: e.activation(out=st[:, 4 * tt + 2:4 * tt + 3], in_=st[:, 4 * tt + 1:4 * tt + 2],
                                           func=AF.Sqrt), r=[tag + f"st{tt}"], w=[tag + f"st{tt}"])
        S.op("dve", lambda e: e.reciprocal(out=st[:, 4 * tt + 3:4 * tt + 4], in_=st[:, 4 * tt + 2:4 * tt + 3]),
             r=[tag + f"st{tt}"], w=[tag + f"st{tt}"])
        S.op("act", lambda e: e.activation(out=xt[i][:], in_=xt[i][:], func=AF.Identity, scale=st[:, 4 * tt + 3:4 * tt + 4]),
             r=[tag + f"xt{i}", tag + f"st{tt}"], w=[tag + f"xt{i}"])
        S.op("dve" if tt % 2 == 0 else "pool",
             lambda e: e.tensor_tensor(out=hbt[:], in0=xt[i][:], in1=gb[:], op=ALU.mult),
             r=[tag + f"xt{i}", tag + "gb"], w=[tag + f"hb{tt % 3}"])
        if tt + 4 < 16:
            load_x(tt + 4)

    def stage_b(tt):
        transpose_rows(G, hb2[tt % 3], tag + f"hb{tt % 3}", hT, f"{hname}{tt // 4}", tt)
        if after_chunk is not None and tt % 4 == 3:
            after_chunk(tt // 4)

    for tt in range(16):
        stage_a(tt)
        if tt >= 1:
            stage_b(tt - 1)
    stage_b(15)


def transpose_rows(G, hb, hbname, hT, hname, tt):
    S = G.S
    for half in range(2):
        b = S.rr("tp", 4)
        pv = G.ps[b][:].bitcast(BF16)
        for k8 in range(8):
            kc = half * 8 + k8
            S.op("pe", lambda e: e.transpose(out=pv[:, k8 * 128:(k8 + 1) * 128], in_=hb[:, kc * 128:(kc + 1) * 128],
                                             identity=G.identb[:]),
                 r=[hbname, "identb"], w=[f"ps{b}"])
        eng = "act" if half == 0 else "dve"
        dst = hT[:, half * 8:(half + 1) * 8, tt * 128:(tt + 1) * 128]
        srcv = pv.rearrange("p (a b) -> p a b", a=8)
        if eng == "act":
            S.op("act", lambda e: e.copy(out=dst, in_=srcv), r=[f"ps{b}"], w=[hname])
        else:
            S.op("dve", lambda e: e.tensor_copy(out=dst, in_=srcv), r=[f"ps{b}"], w=[hname])


def load_w(G, wt, wname, src3, ncols, nk=NKC):
    G.S.dma("pool", wname, wt[:, 0:nk, 0:ncols], src3, w=[wname])


def stage_inproj(G, l, xsrc):
    from contextlib import ExitStack
    nc, S, I = G.nc, G.S, G.I
    groups = [
        ("F", 0, 512, G.qkA, 0, BF16), ("F", 512, 512, G.qkA, 512, BF16), ("T", 1024, 512, G.vA, 0, BF16),
        ("T", 1536, 512, G.pb, 0, F32), ("T", 2048, 512, G.pb, 512, F32),
        ("F", 2560, 512, G.qkC, 0, BF16), ("F", 3072, 512, G.qkC, 512, BF16), ("T", 3584, 512, G.vC, 0, BF16),
        ("F", 4096, 512, G.pdT, 0, F32), ("F", 4608, 96, G.pdT, 512, F32), ("F", 4704, 512, G.pdT, 608, F32),
        ("F", 5216, 512, G.pdT, 1120, F32), ("F", 5728, 96, G.pdT, 1632, F32), ("F", 5824, 256, G.pdT, 1728, F32),
    ]
    with ExitStack() as es:
        hT = sbt(es, nc, "hT", [128, NKC, S_], BF16)
        wt = [sbt(es, nc, f"wt{i}", [128, NKC, 512], BF16) for i in range(2)]
        sg32 = [sbt(es, nc, f"sg32_{i}", [128, 512], F32) for i in range(3)]
        sg16 = [sbt(es, nc, f"sg16_{i}", [128, 512], BF16) for i in range(3)]
        w3 = I["w_in"][l].rearrange("(kc p) c -> p kc c", p=128)

        def issue(gi):
            mode, c0, ncol, dst, d0, dt = groups[gi]
            load_w(G, wt[gi % 2], f"wt{gi % 2}", w3[:, :, c0:c0 + ncol], ncol)

        evc = [0]

        def emit_tile(gi, a, m, tc):
            mode, c0, ncol, dst, d0, dt = groups[gi]
            w = wt[gi % 2]
            wn = f"wt{gi % 2}"
            b = 4 + S.rr("pp", 4)
            for kc in range(NKC):
                if mode == "F":
                    S.op("pe", lambda e: e.matmul(G.ps[b][0:m, :], lhsT=w[:, kc, a * 128:a * 128 + m],
                                                  rhs=hT[:, kc, tc * 512:(tc + 1) * 512],
                                                  start=(kc == 0), stop=(kc == NKC - 1)),
                         r=[wn, f"hT{tc}"], w=[f"ps{b}"])
                else:
                    S.op("pe", lambda e: e.matmul(G.ps[b][:, 0:ncol], lhsT=hT[:, kc, a * 128:(a + 1) * 128],
                                                  rhs=w[:, kc, 0:ncol], start=(kc == 0), stop=(kc == NKC - 1)),
                         r=[wn, f"hT{a // 4}"], w=[f"ps{b}"])
            si = S.rr("sg" + ("32" if dt == F32 else "16"), 3)
            sg = sg32[si] if dt == F32 else sg16[si]
            sgn = ("sg32_" if dt == F32 else "sg16_") + str(si)
            ncl = 512 if mode == "F" else ncol
            evc[0] += 1
            if evc[0] % 2 == 0:
                S.op("act", lambda e: e.copy(out=sg[0:m, 0:ncl], in_=G.ps[b][0:m, 0:ncl]), r=[f"ps{b}"], w=[sgn])
            else:
                S.op("dve", lambda e: e.tensor_copy(out=sg[0:m, 0:ncl], in_=G.ps[b][0:m, 0:ncl]), r=[f"ps{b}"], w=[sgn])
            if mode == "F":
                dap = dst[d0 + a * 128:d0 + a * 128 + m, tc * 512:(tc + 1) * 512]
            else:
                dap = dst[a * 128:(a + 1) * 128, d0:d0 + ncol]
            S.dma("sp", sgn, dap, sg[0:m, 0:ncl], r=[sgn])

        def group_tiles(gi):
            mode, c0, ncol, dst, d0, dt = groups[gi]
            tiles = []
            if mode == "F":
                for mt in range((ncol + 127) // 128):
                    for tc in range(4):
                        tiles.append((mt, min(128, ncol - mt * 128), tc))
            else:
                for tt in range(16):
                    tiles.append((tt, 128, 0))
            return tiles

        issue(0)
        issue(1)

        def after_chunk(tc):
            for gi in (0, 1):
                for (a, m, tcc) in group_tiles(gi):
                    if tcc == tc:
                        emit_tile(gi, a, m, tc)

        rms_to_T(G, es, xsrc, I["norm_mix_g"][l:l + 1, :], hT, "hT", "n", after_chunk=after_chunk)
        issue(2)
        for gi in range(2, len(groups)):
            if gi + 1 < len(groups):
                issue(gi + 1)
            for (a, m, tc) in group_tiles(gi):
                emit_tile(gi, a, m, tc)


def stage_attn(G, l, moba):
    from contextlib import ExitStack
    nc, S, I = G.nc, G.S, G.I
    qk = G.qkC if moba else G.qkA
    vsrc = G.vC if moba else G.vA
    usrc = G.uC if moba else G.uA
    ycol0 = 1024 if moba else 0
    tg = "C" if moba else "A"
    with ExitStack() as es:
        qT = [sbt(es, nc, f"qT{i}", [64, S_], BF16) for i in range(2)]
        kT = [sbt(es, nc, f"kT{i}", [64, S_], BF16) for i in range(2)]
        va = [sbt(es, nc, f"va{i}", [128, 16, 65], BF16) for i in range(2)]
        Tm = [sbt(es, nc, f"Tm{i}", [128, TW], BF16) for i in range(2)]
        Pe = [sbt(es, nc, f"Pe{i}", [128, 512], BF16) for i in range(3)]
        Pb = [sbt(es, nc, f"Pb{i}", [128, 16, 512], BF16) for i in range(2)]
        YH = [sbt(es, nc, f"YH{i}", [128, 16, 64], F32) for i in range(2)]
        rd = sbt(es, nc, "rd", [128, 16], F32)
        if moba:
            ohb = sbt(es, nc, "ohb", [8, 16, 128], BF16)
            kb32 = sbt(es, nc, "kb32", [64, 8], F32)
            kbb = sbt(es, nc, "kbb", [64, 8], BF16)
            gm = sbt(es, nc, "gm", [128, 16, 8], F32)
            mx = sbt(es, nc, "mx", [128, 16, 8], F32)
            nm = sbt(es, nc, "nm", [128, 16, 8], BF16)
            nmT = sbt(es, nc, "nmT", [8, 1024], BF16)
            S.dma("pool", "c0", ohb[:], I["c_ohb"].rearrange("n (a b) -> n a b", a=16), w=["ohb"])
        for i in range(2):
            S.op("dve", lambda e: e.memset(va[i][:, :, 64:65], 1.0), w=[f"va{i}"])

        def load_head(h):
            i = h % 2
            S.dma("sp", f"q{i}", qT[i][:], qk[h * 64:(h + 1) * 64, :], w=[f"qT{i}"])
            S.dma("sp", f"k{i}", kT[i][:], qk[512 + h * 64:512 + (h + 1) * 64, :], w=[f"kT{i}"])
            S.dma("sp", f"v{i}", va[i][:, :, 0:64],
                  vsrc.rearrange("(kb p) c -> p kb c", p=128)[:, :, h * 64:(h + 1) * 64], w=[f"va{i}"])
            tsrc = bass.AP(tensor=usrc.tensor, offset=h * 128 * LU + 127, ap=[[LU - 1, 128], [1, TW]])
            S.dma("sp", f"t{i}", Tm[i][:], tsrc, w=[f"Tm{i}"])

        ust = {}

        def s_phase(h, c):
            i = h % 2
            qn, kn, vn, tn, yn = f"qT{i}", f"kT{i}", f"va{i}", f"Tm{i}", f"YH{i}"
            if c == 0 and moba:
                S.op("dve", lambda e: e.tensor_reduce(out=kb32[:], in_=kT[i][:].rearrange("p (n s) -> p n s", n=8),
                                                      axis=AX.X, op=ALU.add), r=[kn], w=["kb32"])
                S.op("dve", lambda e: e.tensor_scalar(out=kbb[:], in0=kb32[:], scalar1=1.0 / 256, scalar2=None,
                                                      op0=ALU.mult), r=["kb32"], w=["kbb"])
                b = S.rr("st", 4)
                for tt in range(16):
                    S.op("pe", lambda e: e.matmul(G.ps[b][:, tt * 8:(tt + 1) * 8], lhsT=qT[i][:, tt * 128:(tt + 1) * 128],
                                                  rhs=kbb[:], start=True, stop=True), r=[qn, "kbb"], w=[f"ps{b}"])
                S.op("dve", lambda e: e.tensor_copy(out=gm[:].rearrange("p a b -> p (a b)"), in_=G.ps[b][:, 0:128]),
                     r=[f"ps{b}"], w=["gm"] + [f"gm{t_}" for t_ in range(8, 16)] + [f"mx{t_}" for t_ in range(8, 16)])
                S.op("dve", lambda e: e.memset(nm[:], 0.0), w=["nm"] + [f"nm{t_}" for t_ in range(8, 16)])
                for tt in range(8, 16):
                    ob = tt // 2
                    S.op("dve", lambda e: e.memset(gm[:, tt, ob:8], -1e30), r=[], w=[f"gm{tt}"])
                for tt in range(8, 16):
                    S.op("dve", lambda e: e.max(out=mx[:, tt, :], in_=gm[:, tt, :]), r=["gm", f"gm{tt}"], w=[f"mx{tt}"])
                for tt in range(8, 16):
                    ob = tt // 2
                    S.op("dve", lambda e: e.tensor_scalar(out=nm[:, tt, 0:ob], in0=gm[:, tt, 0:ob],
                                                          scalar1=mx[:, tt, 2:3], scalar2=-BIG,
                                                          op0=ALU.is_lt, op1=ALU.mult), r=["gm", f"gm{tt}", f"mx{tt}"], w=[f"nm{tt}"])
                b = S.rr("st", 4)
                pv = G.ps[b][:].bitcast(BF16)
                for tt in range(8, 16):
                    S.op("pe", lambda e: e.transpose(out=pv[0:8, (tt - 8) * 128:(tt - 7) * 128], in_=nm[:, tt, :],
                                                     identity=G.identb[:]), r=["nm", f"nm{tt}", "identb"], w=[f"ps{b}"])
                S.op("dve", lambda e: e.tensor_copy(out=nmT[:], in_=pv[0:8, 0:1024]), r=[f"ps{b}"], w=["nmT"])
            pbi = S.rr("pb", 2)
            pbn = f"Pb{pbi}"
            q0s = {}
            ust[(h, c)] = (pbi, q0s)
            return [(lambda kb=kb: s_step(h, c, kb, pbi, q0s)) for kb in range(4 * c + 4)]

        def s_step(h, c, kb, pbi, q0s):
            i = h % 2
            qn, kn, vn, tn, yn = f"qT{i}", f"kT{i}", f"va{i}", f"Tm{i}", f"YH{i}"
            pbn = f"Pb{pbi}"
            if True:
                q0 = max(512 * c, 128 * kb)
                ncol = 512 * c + 512 - q0
                q0s[kb] = q0
                b = S.rr("st", 4)
                mm2 = moba and c >= 2
                S.op("pe", lambda e: e.matmul(G.ps[b][:, 0:ncol], lhsT=kT[i][:, kb * 128:(kb + 1) * 128],
                                              rhs=qT[i][:, q0:q0 + ncol], start=True, stop=not mm2),
                     r=[qn, kn], w=[f"ps{b}"])
                if mm2:
                    S.op("pe", lambda e: e.matmul(G.ps[b][:, 0:ncol], lhsT=ohb[:, kb, :],
                                                  rhs=nmT[:, q0 - 1024:q0 - 1024 + ncol], start=False, stop=True),
                         r=["ohb", "nmT"], w=[f"ps{b}"])
                pi = S.rr("pe_", 3)
                S.op("act", lambda e: e.activation(out=Pe[pi][:, 0:ncol], in_=G.ps[b][:, 0:ncol], func=AF.Exp,
                                                   scale=0.125), r=[f"ps{b}"], w=[f"Pe{pi}"])
                j0 = q0 - 128 * kb + 384
                S.op("dve" if kb % 2 == 0 else "pool",
                     lambda e: e.tensor_tensor(out=Pb[pbi][:, kb, 0:ncol], in0=Pe[pi][:, 0:ncol],
                                               in1=Tm[i][:, j0:j0 + ncol], op=ALU.mult),
                     r=[f"Pe{pi}", tn], w=[pbn])

        def pv_phase(h, c):
            return [(lambda qb=qb: pv_step(h, c, qb)) for qb in range(4 * c, 4 * c + 4)]

        def pv_step(h, c, qb):
            i = h % 2
            vn, yn = f"va{i}", f"YH{i}"
            pbi, q0s = ust[(h, c)]
            pbn = f"Pb{pbi}"
            if True:
                b = 4 + S.rr("pvb", 4)
                for kb in range(qb + 1):
                    off = qb * 128 - q0s[kb]
                    S.op("pe", lambda e: e.matmul(G.ps[b][:, 0:65], lhsT=Pb[pbi][:, kb, off:off + 128],
                                                  rhs=va[i][:, kb, :], start=(kb == 0), stop=(kb == qb)),
                         r=[pbn, vn], w=[f"ps{b}"])
                S.op("dve", lambda e: e.reciprocal(out=rd[:, qb:qb + 1], in_=G.ps[b][:, 64:65]),
                     r=[f"ps{b}"], w=[f"rd{qb}"])
                S.op("dve", lambda e: e.tensor_scalar(out=YH[i][:, qb, :], in0=G.ps[b][:, 0:64],
                                                      scalar1=rd[:, qb:qb + 1], scalar2=None, op0=ALU.mult),
                     r=[f"ps{b}", f"rd{qb}"], w=[yn])
            if c == 3 and qb == 4 * c + 3:
                S.dma("sp", f"y{i}", G.ycat.rearrange("(qb p) c -> p qb c", p=128)[:, :, ycol0 + h * 64:ycol0 + (h + 1) * 64],
                      YH[i][:], r=[yn])
                if h + 2 < 8:
                    load_head(h + 2)

        load_head(0)
        load_head(1)
        units = [(h, c) for h in range(8) for c in range(4)]
        for u, (h, c) in enumerate(units):
            ss = s_phase(h, c)
            pp = pv_phase(*units[u - 1]) if u > 0 else []
            every = max(1, len(ss) // 4)
            k = 0
            for j, st_ in enumerate(ss):
                st_()
                if (j + 1) % every == 0 and k < len(pp):
                    pp[k]()
                    k += 1
            while k < len(pp):
                pp[k]()
                k += 1
        for st_ in pv_phase(*units[-1]):
            st_()


def lockstep(lists):
    n = max(len(x) for x in lists)
    for si in range(n):
        for x in lists:
            if si < len(x):
                x[si]()


def stage_sgu(G, l):
    from contextlib import ExitStack
    nc, S, I = G.nc, G.S, G.I
    NS = 4
    with ExitStack() as es:
        wsT = sbt(es, nc, "wsT", [128, 8, 128], BF16)
        wsf = sbt(es, nc, "wsf", [128, 8, 128], F32)
        tril = sbt(es, nc, "tril", [128, 128], F32)
        bT = sbt(es, nc, "bT", [128, 8], F32)
        lg = sbt(es, nc, "lg", [128, GW], F32)
        zt = [sbt(es, nc, f"zt{i}", [128, 1024], F32) for i in range(NS)]
        t1s = [sbt(es, nc, f"t1_{i}", [128, 1024], F32) for i in range(NS)]
        t2s = [sbt(es, nc, f"t2_{i}", [128, 1024], F32) for i in range(NS)]
        vns = [sbt(es, nc, f"vn{i}", [128, GW], BF16) for i in range(NS)]
        yos = [sbt(es, nc, f"yo{i}", [128, GW], F32) for i in range(NS)]
        bss = [sbt(es, nc, f"bs{i}", [128, 6], F32) for i in range(NS)]
        mvs = [sbt(es, nc, f"mv{i}", [128, 4], F32) for i in range(NS)]
        S.dma("sp", "c1", wsf[:], I["sgu_wT"][l].rearrange("g s t -> s g t"), w=["wsf"])
        S.dma("sp", "c1", tril[:], I["c_tril"], w=["tril"])
        S.dma("sp", "c1", bT[:], I["sgu_bT"][l], w=["bT"])
        S.dma("sp", "c1", lg[:], I["sgu_ln_g"][l:l + 1, :].partition_broadcast(128), w=["lg"])
        for g in range(8):
            S.op("dve", lambda e: e.tensor_tensor(out=wsT[:, g, :], in0=wsf[:, g, :], in1=tril[:], op=ALU.mult),
                 r=["wsf", "tril"], w=["wsT"])

        def tile_steps(tt, i):
            z, t1, t2, vn, yo, bs, mv = zt[i], t1s[i], t2s[i], vns[i], yos[i], bss[i], mvs[i]
            zn, t1n, t2n, vnn, yon, bsn, mvn = f"zt{i}", f"t1_{i}", f"t2_{i}", f"vn{i}", f"yo{i}", f"bs{i}", f"mv{i}"
            st = []
            st.append(lambda: S.dma("sp", f"x{i}", z[:], G.pb[tt * 128:(tt + 1) * 128, :], w=[zn]))
            st.append(lambda: S.op("act", lambda e: e.activation(out=t1[:], in_=z[:], func=AF.Square), r=[zn], w=[t1n]))
            st.append(lambda: S.op("dve", lambda e: e.tensor_scalar(out=t1[:], in0=t1[:], scalar1=0.044715, scalar2=1.0, op0=ALU.mult,
                                                                    op1=ALU.add), r=[t1n], w=[t1n]))
            st.append(lambda: S.op("pool", lambda e: e.tensor_tensor(out=t1[:], in0=t1[:], in1=z[:], op=ALU.mult), r=[t1n, zn], w=[t1n]))
            st.append(lambda: S.op("act", lambda e: e.activation(out=t2[:], in_=t1[:], func=AF.Sigmoid, scale=1.5957691216057308),
                                   r=[t1n], w=[t2n]))
            st.append(lambda: S.op("dve", lambda e: e.tensor_tensor(out=t2[:], in0=t2[:], in1=z[:], op=ALU.mult), r=[t2n, zn], w=[t2n]))
            st.append(lambda: S.op("dve", lambda e: e.bn_stats(out=bs[:], in_=t2[:, 512:1024]), r=[t2n], w=[bsn]))
            st.append(lambda: S.op("dve", lambda e: e.bn_aggr(out=mv[:, 0:2], in_=bs[:]), r=[bsn], w=[mvn]))
            st.append(lambda: S.op("dve", lambda e: e.tensor_scalar(out=mv[:, 2:3], in0=mv[:, 1:2], scalar1=1e-5, scalar2=None,
                                                                    op0=ALU.add), r=[mvn], w=[mvn]))
            st.append(lambda: S.op("act", lambda e: e.activation(out=mv[:, 2:3], in_=mv[:, 2:3], func=AF.Sqrt), r=[mvn], w=[mvn]))
            st.append(lambda: S.op("dve", lambda e: e.reciprocal(out=mv[:, 3:4], in_=mv[:, 2:3]), r=[mvn], w=[mvn]))
            st.append(lambda: S.op("dve", lambda e: e.tensor_scalar(out=t1[:, 0:512], in0=t2[:, 512:1024], scalar1=mv[:, 0:1],
                                                                    scalar2=mv[:, 3:4], op0=ALU.subtract, op1=ALU.mult),
                                   r=[t2n, mvn], w=[t1n]))
            st.append(lambda: S.op("pool", lambda e: e.tensor_tensor(out=vn[:], in0=t1[:, 0:512], in1=lg[:], op=ALU.mult),
                                   r=[t1n, "lg"], w=[vnn]))

            def mm():
                b = S.rr("st", 4)
                for g in range(8):
                    S.op("pe", lambda e: e.matmul(G.ps[b][:, g * 64:(g + 1) * 64], lhsT=wsT[:, g, :],
                                                  rhs=vn[:, g * 64:(g + 1) * 64], start=True, stop=True),
                         r=["wsT", vnn], w=[f"ps{b}"])
                S.op("dve", lambda e: e.tensor_tensor(out=t1[:, 512:1024].rearrange("p (g c) -> p g c", g=8),
                                                      in0=G.ps[b][:].rearrange("p (g c) -> p g c", g=8),
                                                      in1=bT[:].unsqueeze(2).to_broadcast([128, 8, 64]), op=ALU.add),
                     r=[f"ps{b}", "bT"], w=[t1n])
            st.append(mm)
            st.append(lambda: S.op("pool", lambda e: e.tensor_tensor(out=yo[:], in0=t1[:, 512:1024], in1=t2[:, 0:512], op=ALU.mult),
                                   r=[t1n, t2n], w=[yon]))
            st.append(lambda: S.dma("sp", f"y{i}", G.ycat[tt * 128:(tt + 1) * 128, 512:1024], yo[:], r=[yon]))
            return st

        for t0 in range(0, 16, NS):
            lockstep([tile_steps(t0 + i, i) for i in range(NS)])


def stage_outproj(G, l, xsrc):
    from contextlib import ExitStack
    nc, S, I = G.nc, G.S, G.I
    with ExitStack() as es:
        yT = sbt(es, nc, "yT", [128, NKC, S_], BF16)
        wt = [sbt(es, nc, f"wo{i}", [128, NKC, 512], BF16) for i in range(2)]
        w3 = I["w_out"][l].rearrange("(kc p) c -> p kc c", p=128)
        load_w(G, wt[0], "wt0", w3[:, :, 0:512], 512)
        load_w(G, wt[1], "wt1", w3[:, :, 512:1024], 512)
        gb = sbt(es, nc, "bgb", [128, D_], F32)
        yt = [sbt(es, nc, f"byt{i}", [128, D_], F32) for i in range(4)]
        yb = [sbt(es, nc, f"byb{i}", [128, D_], BF16) for i in range(3)]
        st = sbt(es, nc, "bst", [128, 16, 16], F32)
        xo = [sbt(es, nc, f"xo{i}", [128, 512], F32) for i in range(3)]
        xn = [sbt(es, nc, f"xn{i}", [128, 512], F32) for i in range(3)]
        order = [(cg, tc * 4 + t4) for tc in range(4) for cg in (0, 1) for t4 in range(4)]
        order += [(cg, tt) for cg in (2, 3) for tt in range(16)]
        pos = [0]

        def load_xo(k):
            cg, tt = order[k]
            S.dma("sp", f"xo{k % 3}", xo[k % 3][:], xsrc[tt * 128:(tt + 1) * 128, cg * 512:(cg + 1) * 512], w=[f"xo{k % 3}"])

        def emit_tile():
            k = pos[0]
            pos[0] += 1
            cg, tt = order[k]
            w = wt[cg % 2]
            wn = f"wt{cg % 2}"
            si = k % 3
            if k + 2 < len(order):
                load_xo(k + 2)
            b = 4 + S.rr("pp", 4)
            for kc in range(NKC):
                S.op("pe", lambda e: e.matmul(G.ps[b][:, :], lhsT=yT[:, kc, tt * 128:(tt + 1) * 128], rhs=w[:, kc, :],
                                              start=(kc == 0), stop=(kc == NKC - 1)), r=[wn, f"yT{tt // 4}"], w=[f"ps{b}"])
            S.op("dve", lambda e: e.tensor_tensor(out=xn[si][:], in0=G.ps[b][:, :], in1=xo[si][:], op=ALU.add),
                 r=[f"ps{b}", f"xo{si}"], w=[f"xn{si}"])
            S.dma("sp", f"xn{si}", G.xres[tt * 128:(tt + 1) * 128, cg * 512:(cg + 1) * 512], xn[si][:], r=[f"xn{si}"])

        S.dma("sp", "c1", gb[:], I["branch_norm_g"][l:l + 1, :].partition_broadcast(128), w=["bgb"])
        load_xo(0)
        load_xo(1)

        def load_y(t_):
            S.dma("sp", f"x{t_ % 4}", yt[t_ % 4][:], G.ycat[t_ * 128:(t_ + 1) * 128, :], w=[f"byt{t_ % 4}"])

        for t_ in range(4):
            load_y(t_)
        def stage_a(tt):
            i = tt % 4
            i2 = tt % 3
            for br in range(4):
                S.op("act", lambda e: e.activation(out=yb[i2][:, br * 512:(br + 1) * 512],
                                                   in_=yt[i][:, br * 512:(br + 1) * 512], func=AF.Square,
                                                   accum_out=st[:, tt, br:br + 1]),
                     r=[f"byt{i}"], w=[f"byb{i2}", f"bst{tt}"])
            S.op("dve", lambda e: e.tensor_scalar(out=st[:, tt, 4:8], in0=st[:, tt, 0:4], scalar1=1.0 / GW,
                                                  scalar2=1e-6, op0=ALU.mult, op1=ALU.add),
                 r=[f"bst{tt}"], w=[f"bst{tt}"])
            S.op("act", lambda e: e.activation(out=st[:, tt, 8:12], in_=st[:, tt, 4:8], func=AF.Sqrt),
                 r=[f"bst{tt}"], w=[f"bst{tt}"])
            S.op("dve", lambda e: e.reciprocal(out=st[:, tt, 12:16], in_=st[:, tt, 8:12]),
                 r=[f"bst{tt}"], w=[f"bst{tt}"])
            for br in range(4):
                S.op("dve", lambda e: e.scalar_tensor_tensor(out=yb[i2][:, br * 512:(br + 1) * 512],
                                                             in0=yt[i][:, br * 512:(br + 1) * 512],
                                                             scalar=st[:, tt, 12 + br:13 + br],
                                                             in1=gb[:, br * 512:(br + 1) * 512],
                                                             op0=ALU.mult, op1=ALU.mult),
                     r=[f"byt{i}", f"bst{tt}", "bgb"], w=[f"byb{i2}"])
            if tt + 4 < 16:
                load_y(tt + 4)

        def stage_b(tt):
            transpose_rows(G, yb[tt % 3], f"byb{tt % 3}", yT, f"yT{tt // 4}", tt)
            if tt % 4 == 3:
                for _ in range(8):
                    emit_tile()

        for tt in range(16):
            stage_a(tt)
            if tt >= 1:
                stage_b(tt - 1)
        stage_b(15)
        load_w(G, wt[0], "wt0", w3[:, :, 1024:1536], 512)
        load_w(G, wt[1], "wt1", w3[:, :, 1536:2048], 512)
        while pos[0] < len(order):
            emit_tile()


def stage_ffn_up(G, l, h2T):
    from contextlib import ExitStack
    nc, S, I = G.nc, G.S, G.I
    with ExitStack() as es:
        wg = [sbt(es, nc, f"wg{i}", [128, NKC, 512], BF16) for i in range(2)]
        wu = [sbt(es, nc, f"wu{i}", [128, NKC, 512], BF16) for i in range(2)]
        sg = [sbt(es, nc, f"fs{i}", [128, 512], F32) for i in range(3)]
        ao = [sbt(es, nc, f"ao{i}", [128, 512], BF16) for i in range(3)]
        g3 = I["w_gate"][l].rearrange("(kc p) c -> p kc c", p=128)
        u3 = I["w_up"][l].rearrange("(kc p) c -> p kc c", p=128)

        def issue(gi):
            load_w(G, wg[gi % 2], f"wg{gi % 2}", g3[:, :, gi * 512:(gi + 1) * 512], 512)
            load_w(G, wu[gi % 2], f"wu{gi % 2}", u3[:, :, gi * 512:(gi + 1) * 512], 512)

        def emit_tile(gi, mt, tc):
            j = gi % 2
            bg = S.rr("st", 4)
            bu = 4 + S.rr("pp", 4)
            for kc in range(NKC):
                S.op("pe", lambda e: e.matmul(G.ps[bg][:, :], lhsT=wg[j][:, kc, mt * 128:(mt + 1) * 128],
                                              rhs=h2T[:, kc, tc * 512:(tc + 1) * 512], start=(kc == 0),
                                              stop=(kc == NKC - 1)), r=[f"wg{j}", f"h2T{tc}"], w=[f"ps{bg}"])
            for kc in range(NKC):
                S.op("pe", lambda e: e.matmul(G.ps[bu][:, :], lhsT=wu[j][:, kc, mt * 128:(mt + 1) * 128],
                                              rhs=h2T[:, kc, tc * 512:(tc + 1) * 512], start=(kc == 0),
                                              stop=(kc == NKC - 1)), r=[f"wu{j}", f"h2T{tc}"], w=[f"ps{bu}"])
            si = S.rr("fs", 3)
            S.op("act", lambda e: e.activation(out=sg[si][:], in_=G.ps[bg][:, :], func=AF.Silu),
                 r=[f"ps{bg}"], w=[f"fs{si}"])
            S.op("dve", lambda e: e.tensor_tensor(out=ao[si][:], in0=G.ps[bu][:, :], in1=sg[si][:], op=ALU.mult),
                 r=[f"ps{bu}", f"fs{si}"], w=[f"ao{si}"])
            jj = gi * 4 + mt
            S.dma("sp", f"ao{si}", G.actT[tc, :, jj * 512:(jj + 1) * 512], ao[si][:], r=[f"ao{si}"])

        issue(0)
        issue(1)

        def after_chunk(tc):
            for mt in range(4):
                emit_tile(0, mt, tc)

        rms_to_T(G, es, G.xres, I["norm_ffn_g"][l:l + 1, :], h2T, "h2T", "f", after_chunk=after_chunk)
        for gi in range(1, DFF // 512):
            if gi + 1 < DFF // 512:
                issue(gi + 1)
            for mt in range(4):
                for tc in range(4):
                    emit_tile(gi, mt, tc)


def stage_ffn_down(G, l):
    from contextlib import ExitStack
    nc, S, I = G.nc, G.S, G.I
    NJ = DFF // 128
    with ExitStack() as es:
        wd = [sbt(es, nc, f"wd{i}", [128, NJ, 512], BF16) for i in range(2)]
        at = [sbt(es, nc, f"at{i}", [128, NJ, 512], BF16) for i in range(2)]
        xo = [sbt(es, nc, f"dxo{i}", [128, 512], F32) for i in range(3)]
        xn = [sbt(es, nc, f"dxn{i}", [128, 512], F32) for i in range(3)]
        d3 = I["w_down"][l].rearrange("(j p) c -> p j c", p=128)

        def issue(cg):
            for hf in range(2):
                G.S.dma("pool", f"wd{cg % 2}", wd[cg % 2][:, hf * (NJ // 2):(hf + 1) * (NJ // 2), :],
                        d3[:, hf * (NJ // 2):(hf + 1) * (NJ // 2), cg * 512:(cg + 1) * 512], w=[f"wd{cg % 2}"])

        issue(0)
        units = [(cg, tc) for cg in range(4) for tc in range(4)]

        def load_at(u):
            cg, tc = units[u]
            S.dma("sp", f"at{u % 2}", at[u % 2][:].rearrange("p j t -> p (j t)"), G.actT[tc], w=[f"at{u % 2}"])

        def load_xo(k):
            u, t4 = divmod(k, 4)
            cg, tc = units[u]
            tt = tc * 4 + t4
            S.dma("sp", f"xo{k % 3}", xo[k % 3][:], G.xres[tt * 128:(tt + 1) * 128, cg * 512:(cg + 1) * 512],
                  w=[f"dxo{k % 3}"])

        load_at(0)
        load_xo(0)
        load_xo(1)
        for u, (cg, tc) in enumerate(units):
            if tc == 0 and cg + 1 < 4:
                issue(cg + 1)
            if u + 1 < len(units):
                load_at(u + 1)
            w = wd[cg % 2]
            wn = f"wd{cg % 2}"
            ai = u % 2
            for t4 in range(4):
                k = u * 4 + t4
                tt = tc * 4 + t4
                si = k % 3
                if k + 2 < 4 * len(units):
                    load_xo(k + 2)
                b = 4 + S.rr("pp", 4)
                for j in range(NJ):
                    S.op("pe", lambda e: e.matmul(G.ps[b][:, :], lhsT=at[ai][:, j, t4 * 128:(t4 + 1) * 128],
                                                  rhs=w[:, j, :], start=(j == 0), stop=(j == NJ - 1)),
                         r=[wn, f"at{ai}"], w=[f"ps{b}"])
                S.op("dve", lambda e: e.tensor_tensor(out=xn[si][:], in0=G.ps[b][:, :], in1=xo[si][:], op=ALU.add),
                     r=[f"ps{b}", f"dxo{si}"], w=[f"dxn{si}"])
                S.dma("sp", f"xn{si}", G.xres[tt * 128:(tt + 1) * 128, cg * 512:(cg + 1) * 512], xn[si][:],
                      r=[f"dxn{si}"])


def stage_final(G):
    from contextlib import ExitStack
    nc, S, I = G.nc, G.S, G.I
    with ExitStack() as es:
        gb = sbt(es, nc, "fgb", [128, D_], F32)
        xt = [sbt(es, nc, f"fxt{i}", [128, D_], F32) for i in range(2)]
        ot = [sbt(es, nc, f"fot{i}", [128, D_], F32) for i in range(2)]
        st = sbt(es, nc, "fst", [128, 64], F32)
        S.dma("sp", "c1", gb[:], I["norm_final_g"][0:1, :].partition_broadcast(128), w=["fgb"])
        for tt in range(16):
            i = tt % 2
            S.dma("sp", f"x{i}", xt[i][:], G.xres[tt * 128:(tt + 1) * 128, :], w=[f"fxt{i}"])
            S.op("act", lambda e: e.activation(out=ot[i][:], in_=xt[i][:], func=AF.Square, accum_out=st[:, 4 * tt:4 * tt + 1]),
                 r=[f"fxt{i}"], w=[f"fot{i}", f"fst{tt}"])
            S.op("dve", lambda e: e.tensor_scalar(out=st[:, 4 * tt + 1:4 * tt + 2], in0=st[:, 4 * tt:4 * tt + 1],
                                                  scalar1=1.0 / D_, scalar2=1e-6, op0=ALU.mult, op1=ALU.add),
                 r=[f"fst{tt}"], w=[f"fst{tt}"])
            S.op("act", lambda e: e.activation(out=st[:, 4 * tt + 2:4 * tt + 3], in_=st[:, 4 * tt + 1:4 * tt + 2],
                                               func=AF.Sqrt), r=[f"fst{tt}"], w=[f"fst{tt}"])
            S.op("dve", lambda e: e.reciprocal(out=st[:, 4 * tt + 3:4 * tt + 4], in_=st[:, 4 * tt + 2:4 * tt + 3]),
                 r=[f"fst{tt}"], w=[f"fst{tt}"])
            S.op("dve", lambda e: e.scalar_tensor_tensor(out=ot[i][:], in0=xt[i][:], scalar=st[:, 4 * tt + 3:4 * tt + 4],
                                                         in1=gb[:], op0=ALU.mult, op1=ALU.mult),
                 r=[f"fxt{i}", f"fst{tt}", "fgb"], w=[f"fot{i}"])
            S.dma("sp", f"y{i}", G.out[tt * 128:(tt + 1) * 128, :], ot[i][:], r=[f"fot{i}"])


IN_SHAPES["rwkv_pk"] = [DEPTH, 128, 4, 8]
IN_SHAPES["rwkv_mu2"] = [DEPTH, 128, 4]
for _k in ("rwkv_mu", "rwkv_w0", "rwkv_a0", "rwkv_k_k", "rwkv_k_a", "rwkv_r_k"):
    IN_SHAPES.pop(_k)
CONST_SHAPES["c_rmask"] = [64, 128]


def stage_rwkv(G, l):
    from contextlib import ExitStack
    nc, S, I = G.nc, G.S, G.I
    NCH = 32
    with ExitStack() as es:
        tw = sbt(es, nc, "tw", [96, S_], BF16)
        adb = sbt(es, nc, "adb", [96, S_], BF16)
        sgb = sbt(es, nc, "sgb", [128, 2, S_], BF16)
        w2b = sbt(es, nc, "w2b", [96, GW], BF16)
        a2b = sbt(es, nc, "a2b", [96, GW], BF16)
        g2b = sbt(es, nc, "g2b", [128, 2, GW], BF16)
        pk = sbt(es, nc, "pk", [128, 4, 8], F32)
        omk = sbt(es, nc, "omk", [128, 4], F32)
        ones2 = sbt(es, nc, "ones2", [128, 128], BF16)
        bones = sbt(es, nc, "bones", [128, 2], BF16)
        E2 = sbt(es, nc, "E2", [128, 64], F32)
        mu2 = sbt(es, nc, "mu2", [128, 4], F32)
        rst = sbt(es, nc, "rst", [128, S_], F32)
        mi2 = sbt(es, nc, "mi2", [128, 64], F32)
        msbd = sbt(es, nc, "msbd", [128, 128], F32)
        msbdT = sbt(es, nc, "msbdT", [128, 128], F32)
        ones = sbt(es, nc, "ones", [64, 64], BF16)
        bon = sbt(es, nc, "bon", [128, 16, 8], F32)
        S.dma("pool", "c0", w2b[:], I["rwkv_w2"][l], w=["w2b"])
        S.dma("pool", "c0", a2b[:], I["rwkv_a2"][l], w=["a2b"])
        S.dma("pool", "c0", g2b[:], I["rwkv_g2"][l].rearrange("(j p) c -> p j c", p=128), w=["g2b"])
        S.dma("sp", "c1", pk[:], I["rwkv_pk"][l], w=["pk"])
        S.dma("sp", "c1", mu2[:], I["rwkv_mu2"][l], w=["mu2"])
        S.dma("sp", "c1", rst[:], I["c_reset"].partition_broadcast(128), w=["rst"])
        S.dma("sp", "c1", mi2[:], I["c_mi2"], w=["mi2"])
        S.dma("sp", "c1", msbd[:], I["c_msbd"], w=["msbd"])
        S.dma("sp", "c1", msbdT[:], I["c_msbdT"], w=["msbdT"])
        S.op("dve", lambda e: e.memset(ones[:], 1.0), w=["ones"])
        S.op("dve", lambda e: e.memset(ones2[:], 0.0), w=["ones2"])
        S.op("dve", lambda e: e.memset(bones[:], 0.0), w=["bones"])
        for hh in range(2):
            S.op("dve", lambda e: e.memset(ones2[hh * 64:(hh + 1) * 64, hh * 64:(hh + 1) * 64], 1.0), w=["ones2"])
            S.op("dve", lambda e: e.memset(bones[hh * 64:(hh + 1) * 64, hh:hh + 1], 1.0), w=["bones"])
        S.op("dve", lambda e: e.tensor_tensor(out=E2[:], in0=G.identf[:, 0:64], in1=G.identf[:, 64:128], op=ALU.add),
             r=["identf"], w=["E2"])
        S.op("dve", lambda e: e.tensor_scalar(out=omk[:], in0=pk[:, :, 6], scalar1=-1.0, scalar2=1.0, op0=ALU.mult,
                                              op1=ALU.add), r=["pk"], w=["omk"])
        with ExitStack() as e1:
            raw = sbt(e1, nc, "raw", [128, S_ + 1], F32)
            dd = sbt(e1, nc, "dd", [128, S_], F32)
            for (r0, nr, mcol, kind) in ((512, 96, 0, "w"), (1632, 96, 1, "a"), (1728, 128, 2, "g0"), (1856, 128, 3, "g1")):
                S.op("dve", lambda e: e.memset(raw[0:nr, 0:1], 0.0), w=["raw"])
                S.dma("sp", "x0", raw[0:nr, 1:S_ + 1], G.pdT[r0:r0 + nr, :], w=["raw"])
                S.op("dve", lambda e: e.tensor_tensor(out=dd[0:nr, :], in0=raw[0:nr, 0:S_], in1=raw[0:nr, 1:S_ + 1],
                                                      op=ALU.subtract), r=["raw"], w=["dd"])
                S.op("dve", lambda e: e.scalar_tensor_tensor(out=dd[0:nr, :], in0=dd[0:nr, :], scalar=mu2[0:nr, mcol:mcol + 1],
                                                             in1=raw[0:nr, 1:S_ + 1], op0=ALU.mult, op1=ALU.add),
                     r=["dd", "raw", "mu2"], w=["dd"])
                if kind == "w":
                    S.op("act", lambda e: e.activation(out=tw[:], in_=dd[0:96, :], func=AF.Tanh), r=["dd"], w=["tw"])
                elif kind == "a":
                    S.op("act", lambda e: e.copy(out=adb[:], in_=dd[0:96, :]), r=["dd"], w=["adb"])
                else:
                    j = 0 if kind == "g0" else 1
                    S.op("act", lambda e: e.activation(out=sgb[:, j, :], in_=dd[:, :], func=AF.Sigmoid), r=["dd"], w=["sgb"])
            S.barrier()

        KT = sbt(es, nc, "KT", [128, S_], BF16)
        BT = sbt(es, nc, "BT", [128, S_], BF16)
        AR = sbt(es, nc, "AR", [128, NCH, 128], BF16)
        AT = sbt(es, nc, "AT", [128, S_], BF16)
        DR = sbt(es, nc, "DR", [128, NCH, 128], BF16)
        KBr = sbt(es, nc, "KBr", [128, S_], BF16)
        BBr = sbt(es, nc, "BBr", [128, S_], BF16)
        VB = sbt(es, nc, "VB", [128, S_], BF16)
        RK = sbt(es, nc, "RK", [128, S_], BF16)
        vsf = sbt(es, nc, "vsf", [128, S_], F32)
        for h in range(8):
          if h % 2 == 0:
            pr = h // 2
            with ExitStack() as e1:
                T = [sbt(e1, nc, f"T{i}", [128, S_ + 1], F32) for i in range(3)]
                U = [sbt(e1, nc, f"U{i}", [128, S_], F32) for i in range(8)]
                SQ = sbt(e1, nc, "SQ", [128, S_], BF16)
                WC = sbt(e1, nc, "WC", [128, NCH], F32)
                cC = sbt(e1, nc, "cC", [128, NCH], F32)
                VS = sbt(e1, nc, "VS", [128, 16, 128], F32)

                def P(j):
                    return pk[:, pr, j:j + 1]

                rows = (0, 608, 1120)
                for ti in range(3):
                    S.op("dve", lambda e: e.memset(T[ti][:, 0:1], 0.0), w=[f"T{ti}"])
                for ti in range(3):
                    S.dma("sp", ("x0", "x1", "q0")[ti], T[ti][:, 1:S_ + 1],
                          G.pdT[rows[ti] + pr * 128:rows[ti] + (pr + 1) * 128, :], w=[f"T{ti}"])

                def shift(ti, mu_j, dst, dname, tmp, tname):
                    S.op("pool", lambda e: e.tensor_tensor(out=tmp[:], in0=T[ti][:, 0:S_], in1=T[ti][:, 1:S_ + 1],
                                                           op=ALU.subtract), r=[f"T{ti}"], w=[tname])
                    S.op("dve", lambda e: e.scalar_tensor_tensor(out=dst, in0=tmp[:], scalar=P(mu_j), in1=T[ti][:, 1:S_ + 1],
                                                                 op0=ALU.mult, op1=ALU.add), r=[tname, f"T{ti}", "pk"], w=[dname])

                rs, ks, lw, asg, kkn = U[0], U[1], U[2], U[3], U[4]
                shift(0, 0, rs[:], "U0", U[5], "U5")
                shift(1, 1, ks[:], "U1", U[6], "U6")
                shift(2, 2, vsf[:], "vsf", U[7], "U7")
                cum = T[0]
                S.op("act", lambda e: e.copy(out=VB[:], in_=vsf[:]), r=["vsf"], w=["VB"])
                for half in range(2):
                    for q4 in range(2):
                        b = S.rr("st", 4)
                        for t4 in range(4):
                            tt = half * 8 + q4 * 4 + t4
                            S.op("pe", lambda e: e.transpose(out=G.ps[b][:, t4 * 128:(t4 + 1) * 128], in_=vsf[:, tt * 128:(tt + 1) * 128],
                                                             identity=G.identf[:, :]), r=["vsf", "identf"], w=[f"ps{b}"])
                        S.op("act", lambda e: e.copy(out=VS[:, half * 8 + q4 * 4:half * 8 + q4 * 4 + 4, :].rearrange("p a b -> p (a b)"),
                                                     in_=G.ps[b][:, :]), r=[f"ps{b}"], w=["VS"])
                S.dma("sp", "y1", G.vtk.rearrange("(tt p) v -> p tt v", p=128)[:, :, pr * 128:(pr + 1) * 128], VS[:, :, :],
                      r=["VS"])
                for tc in range(4):
                    b = S.rr("st", 4)
                    S.op("pe", lambda e: e.matmul(G.ps[b][:, :], lhsT=w2b[:, pr * 128:(pr + 1) * 128],
                                                  rhs=tw[:, tc * 512:(tc + 1) * 512], start=True, stop=True),
                         r=["w2b", "tw"], w=[f"ps{b}"])
                    S.op("act", lambda e: e.activation(out=lw[:, tc * 512:(tc + 1) * 512], in_=G.ps[b][:, :],
                                                       func=AF.Sigmoid, bias=P(3)), r=[f"ps{b}", "pk"], w=["U2"])
                    b = S.rr("st", 4)
                    S.op("pe", lambda e: e.matmul(G.ps[b][:, :], lhsT=a2b[:, pr * 128:(pr + 1) * 128],
                                                  rhs=adb[:, tc * 512:(tc + 1) * 512], start=True, stop=True),
                         r=["a2b", "adb"], w=[f"ps{b}"])
                    S.op("act", lambda e: e.activation(out=asg[:, tc * 512:(tc + 1) * 512], in_=G.ps[b][:, :],
                                                       func=AF.Sigmoid, bias=P(4)), r=[f"ps{b}", "pk"], w=["U3"])
                S.op("pool", lambda e: e.tensor_scalar(out=lw[:], in0=lw[:], scalar1=-math.exp(-0.5), scalar2=0.0,
                                                       op0=ALU.mult, op1=ALU.add), r=["U2"], w=["U2"])
                S.op("dve", lambda e: e.tensor_scalar(out=kkn[:], in0=ks[:], scalar1=P(5), scalar2=None, op0=ALU.mult),
                     r=["U1", "pk"], w=["U4"])
                S.op("pool", lambda e: e.tensor_tensor(out=SQ[:], in0=kkn[:], in1=kkn[:], op=ALU.mult), r=["U4"], w=["SQ"])
                for tc in range(4):
                    b = S.rr("st", 4)
                    S.op("pe", lambda e: e.matmul(G.ps[b][:, :], lhsT=ones2[:], rhs=SQ[:, tc * 512:(tc + 1) * 512],
                                                  start=True, stop=True), r=["ones2", "SQ"], w=[f"ps{b}"])
                    S.op("act", lambda e: e.activation(out=U[5][:, tc * 512:(tc + 1) * 512], in_=G.ps[b][:, :],
                                                       func=AF.Sqrt), r=[f"ps{b}"], w=["U5"])
                S.op("dve", lambda e: e.tensor_scalar(out=U[5][:], in0=U[5][:], scalar1=1e-12, scalar2=None, op0=ALU.max),
                     r=["U5"], w=["U5"])
                S.op("dve", lambda e: e.reciprocal(out=U[5][:], in_=U[5][:]), r=["U5"], w=["U5"])
                S.op("pool", lambda e: e.tensor_tensor(out=kkn[:], in0=kkn[:], in1=U[5][:], op=ALU.mult),
                     r=["U4", "U5"], w=["U4"])
                S.op("dve", lambda e: e.tensor_scalar(out=U[6][:], in0=asg[:], scalar1=P(6), scalar2=omk[:, pr:pr + 1],
                                                      op0=ALU.mult, op1=ALU.add), r=["U3", "pk", "omk"], w=["U6"])
                S.op("pool", lambda e: e.tensor_tensor(out=ks[:], in0=ks[:], in1=U[6][:], op=ALU.mult),
                     r=["U1", "U6"], w=["U1"])
                bb = U[7]
                S.op("dve", lambda e: e.tensor_tensor(out=bb[:], in0=kkn[:], in1=asg[:], op=ALU.mult),
                     r=["U4", "U3"], w=["U7"])
                S.op("dve", lambda e: e.scalar_tensor_tensor(out=RK[:], in0=rs[:], scalar=P(7), in1=ks[:], op0=ALU.mult,
                                                             op1=ALU.mult), r=["U0", "U1", "pk"], w=["RK"])
                b = S.rr("st", 4)
                for tt in range(16):
                    S.op("pe", lambda e: e.matmul(G.ps[b][:, 2 * tt:2 * tt + 2], lhsT=RK[:, tt * 128:(tt + 1) * 128], rhs=bones[:, :],
                                                  start=True, stop=True), r=["RK", "bones"], w=[f"ps{b}"])
                S.op("act", lambda e: e.copy(out=bon[:, :, 2 * pr:2 * pr + 2], in_=G.ps[b][:, 0:32].rearrange("p (t h) -> p t h", h=2)),
                     r=[f"ps{b}"], w=["bon"])
                S.op("dve", lambda e: e.tensor_tensor_scan(out=cum[:, 0:S_], data0=rst[:], data1=lw[:], initial=0.0,
                                                           op0=ALU.mult, op1=ALU.add), r=["rst", "U2"], w=["T0"])
                cum3 = cum[:, 0:S_].rearrange("p (c t) -> p c t", t=64)
                S.op("dve", lambda e: e.tensor_copy(out=cC[:], in_=cum3[:, :, 63]), r=["T0"], w=["cC"])
                S.op("act", lambda e: e.activation(out=WC[:], in_=cC[:], func=AF.Exp), r=["cC"], w=["WC"])
                S.op("dve", lambda e: e.tensor_tensor(out=DR[:, :, 0:64],
                                                      in0=E2[:, :].unsqueeze(1).to_broadcast([128, NCH, 64]),
                                                      in1=WC[:].unsqueeze(2).to_broadcast([128, NCH, 64]), op=ALU.mult),
                     r=["E2", "WC"], w=["DR"])
                ex = T[1]
                ex2 = T[2]
                S.op("act", lambda e: e.activation(out=ex[:, 0:S_], in_=cum[:, 0:S_], func=AF.Exp), r=["T0"], w=["T1"])
                S.op("act", lambda e: e.activation(out=ex2[:, 0:S_], in_=cum[:, 0:S_], func=AF.Exp, scale=-1.0),
                     r=["T0", "vsf"], w=["T2"])
                S.op("pool", lambda e: e.tensor_tensor(out=rs[:], in0=rs[:], in1=ex[:, 0:S_], op=ALU.mult),
                     r=["U0", "T1"], w=["U0"])
                S.op("act", lambda e: e.copy(out=AR[:, :, 0:64], in_=rs[:].rearrange("p (c t) -> p c t", t=64)),
                     r=["U0"], w=["AR"])
                S.op("act", lambda e: e.copy(out=DR[:, :, 64:128], in_=rs[:].rearrange("p (c t) -> p c t", t=64)),
                     r=["U0"], w=["DR"])
                S.op("pool", lambda e: e.tensor_tensor(out=KT[:], in0=ks[:], in1=ex2[:, 0:S_], op=ALU.mult),
                     r=["U1", "T2"], w=["KT"])
                S.op("dve", lambda e: e.tensor_tensor(out=BT[:], in0=bb[:], in1=ex2[:, 0:S_], op=ALU.mult),
                     r=["U7", "T2"], w=["BT"])
                S.op("pool", lambda e: e.tensor_tensor(out=lw[:], in0=cum[:, 0:S_], in1=lw[:], op=ALU.subtract),
                     r=["T0", "U2"], w=["U2"])
                S.op("act", lambda e: e.activation(out=lw[:], in_=lw[:], func=AF.Exp), r=["U2"], w=["U2"])
                S.op("dve", lambda e: e.scalar_tensor_tensor(out=AT[:], in0=kkn[:], scalar=-1.0, in1=lw[:],
                                                             op0=ALU.mult, op1=ALU.mult), r=["U4", "U2"], w=["AT"])
                S.op("act", lambda e: e.copy(out=AR[:, :, 64:128], in_=AT[:].rearrange("p (c t) -> p c t", t=64)),
                     r=["AT"], w=["AR"])
                S.op("dve", lambda e: e.tensor_tensor(out=ex[:, 0:S_].rearrange("p (c t) -> p c t", t=64),
                                                      in0=cC[:].unsqueeze(2).to_broadcast([128, NCH, 64]), in1=cum3,
                                                      op=ALU.subtract), r=["cC", "T0", "T1"], w=["T1"])
                S.op("act", lambda e: e.activation(out=ex[:, 0:S_], in_=ex[:, 0:S_], func=AF.Exp), r=["T1"], w=["T1"])
                S.op("pool", lambda e: e.tensor_tensor(out=KBr[:], in0=ks[:], in1=ex[:, 0:S_], op=ALU.mult),
                     r=["U1", "T1"], w=["KBr"])
                S.op("dve", lambda e: e.tensor_tensor(out=BBr[:], in0=bb[:], in1=ex[:, 0:S_], op=ALU.mult),
                     r=["U7", "T1"], w=["BBr"])
                S.barrier()
          if True:
            base = 64 * (h % 2)
            hp = slice(base, base + 64)
            with ExitStack() as e2:
                NQ = NCH // 2
                Xp = sbt(e2, nc, "Xp", [128, NQ, 128], BF16)
                W1p = sbt(e2, nc, "W1p", [128, NQ, 128], BF16)
                W2p = sbt(e2, nc, "W2p", [128, NQ, 128], BF16)
                Vtp = sbt(e2, nc, "Vtp", [128, NQ, 64], BF16)
                AK = sbt(e2, nc, "AK", [128, NQ, 128], BF16)
                AN = [sbt(e2, nc, f"AN{i}", [128, NQ, 256], BF16) for i in range(2)]
                GRT = sbt(e2, nc, "GRT", [64, NCH, 128], BF16)
                HYb = sbt(e2, nc, "HYb", [128, NCH, 64], BF16)
                YD = sbt(e2, nc, "YD", [128, NCH, 64], F32)
                DRs = sbt(e2, nc, "DRs", [64, NCH, 128], BF16)
                STb = [sbt(e2, nc, f"STb{i}", [64, 64], BF16) for i in range(2)]
                idbb = G.identb[hp, hp]
                if base == 0:
                    DRv = DR[0:64, :, :]
                else:
                    for c4 in range(NCH // 4):
                        b = S.rr("all", 8)
                        S.op("pe", lambda e: e.matmul(G.ps[b][0:64, :], lhsT=G.identb[:, 64:128],
                                                      rhs=DR[:, c4 * 4:(c4 + 1) * 4, :].rearrange("p c x -> p (c x)"),
                                                      start=True, stop=True), r=["DR", "identb"], w=[f"ps{b}"])
                        o_ = DRs[:, c4 * 4:(c4 + 1) * 4, :].rearrange("p c x -> p (c x)")
                        if c4 % 2 == 0:
                            S.op("act", lambda e: e.copy(out=o_, in_=G.ps[b][0:64, :]), r=[f"ps{b}"], w=["DRs"])
                        else:
                            S.op("dve", lambda e: e.tensor_copy(out=o_, in_=G.ps[b][0:64, :]), r=[f"ps{b}"], w=["DRs"])
                    DRv = DRs[:, :, :]
                for (src, sname, dst3, dname, eng) in ((AT, "AT", Xp[:, :, 0:64], "XpA", "act"), (BBr, "BBr", W1p[:, :, 0:64], "W1A", "dve"),
                                                       (KBr, "KBr", W2p[:, :, 0:64], "W2A", "act"), (VB, "VB", Vtp[:, :, :], "Vtp", "dve")):
                    b = S.rr("all", 8)
                    pv = G.ps[b][:].bitcast(BF16)
                    for q in range(NQ):
                        S.op("pe", lambda e: e.transpose(out=pv[:, q * 64:(q + 1) * 64], in_=src[hp, q * 128:(q + 1) * 128],
                                                         identity=idbb), r=[sname, "identb"], w=[f"ps{b}"])
                    i_ = pv[:, 0:1024].rearrange("p (q k) -> p q k", k=64)
                    if eng == "act":
                        S.op("act", lambda e: e.copy(out=dst3, in_=i_), r=[f"ps{b}"], w=[dname])
                    else:
                        S.op("dve", lambda e: e.tensor_copy(out=dst3, in_=i_), r=[f"ps{b}"], w=[dname])

                def group_steps(gq):
                    q0 = gq * 4
                    g = f"_{gq}"
                    steps = []

                    def s0():
                        for (srcT, sn, Wp, wtok, dstbd, dtok) in ((BT, "BT", W1p, "W1" + g, AN[0], "AN0" + g),
                                                                    (KT, "KT", W2p, "W2" + g, AK, "AK" + g)):
                            for hb2 in range(2):
                                b = S.rr("all", 8)
                                for qi in range(2):
                                    q = q0 + hb2 * 2 + qi
                                    S.op("pe", lambda e: e.matmul(G.ps[b][:, qi * 256:(qi + 1) * 256], lhsT=srcT[hp, q * 128:(q + 1) * 128],
                                                                  rhs=AR[hp, 2 * q:2 * q + 2, :].rearrange("p c x -> p (c x)"),
                                                                  start=True, stop=True), r=[sn, "AR"], w=[f"ps{b}"])
                                qs = slice(q0 + hb2 * 2, q0 + hb2 * 2 + 2)
                                pq = G.ps[b][:, :].rearrange("p (q x) -> p q x", q=2)
                                for par in range(2):
                                    rows = slice(par * 64, par * 64 + 64)
                                    S.op("dve", lambda e: e.tensor_tensor(out=Wp[rows, qs, 64:128], in0=pq[rows, :, par * 128:par * 128 + 64],
                                                                          in1=mi2[rows, :].unsqueeze(1).to_broadcast([64, 2, 64]), op=ALU.mult),
                                         r=[f"ps{b}", "mi2"], w=[wtok])
                                p4 = G.ps[b][:, :].rearrange("p (q a x) -> p q a x", q=2, a=2)
                                if dstbd is AK:
                                    o4 = AK[:, qs, :].rearrange("p q (a x) -> p q a x", a=2)
                                else:
                                    o4 = AN[0][:, qs, 128:256].rearrange("p q (a x) -> p q a x", a=2)
                                S.op("dve", lambda e: e.tensor_tensor(out=o4, in0=p4[:, :, :, 64:128],
                                                                      in1=msbd[:, :].rearrange("p (a x) -> p a x", a=2).unsqueeze(1).to_broadcast([128, 2, 2, 64]),
                                                                      op=ALU.mult), r=[f"ps{b}", "msbd"], w=[dtok])
                        b = S.rr("all", 8)
                        for qi in range(4):
                            q = q0 + qi
                            S.op("pe", lambda e: e.matmul(G.ps[b][:, qi * 128:(qi + 1) * 128], lhsT=AT[hp, q * 128:(q + 1) * 128],
                                                          rhs=BT[hp, q * 128:(q + 1) * 128], start=True, stop=True), r=["AT", "BT"], w=[f"ps{b}"])
                        S.op("dve", lambda e: e.tensor_tensor(out=AN[0][:, q0:q0 + 4, 0:128],
                                                              in0=G.ps[b][:, :].rearrange("p (q x) -> p q x", q=4),
                                                              in1=msbdT[:, :].unsqueeze(1).to_broadcast([128, 4, 128]), op=ALU.mult),
                             r=[f"ps{b}", "msbdT"], w=["AN0" + g])
                    steps.append(s0)

                    def s1():
                        b = S.rr("all", 8)
                        for qi in range(4):
                            q = q0 + qi
                            S.op("pe", lambda e: e.matmul(G.ps[b][:, qi * 64:(qi + 1) * 64], lhsT=AK[:, q, :], rhs=Vtp[:, q, :],
                                                          start=True, stop=True), r=["AK" + g, "Vtp"], w=[f"ps{b}"])
                        S.op("act", lambda e: e.copy(out=Xp[:, q0:q0 + 4, 64:128],
                                                     in_=G.ps[b][:, 0:256].rearrange("p (q x) -> p q x", q=4)),
                             r=[f"ps{b}"], w=["Xp" + g])
                    steps.append(s1)

                    def mkx(j):
                        def sx():
                            an = AN[j % 2]
                            ann = f"AN{j % 2}" + g
                            b = S.rr("all", 8)
                            for qi in range(4):
                                q = q0 + qi
                                S.op("pe", lambda e: e.matmul(G.ps[b][:, qi * 128:(qi + 1) * 128], lhsT=an[:, q, 128:256], rhs=Xp[:, q, :],
                                                              start=True, stop=True), r=[ann, "Xp" + g, "XpA"], w=[f"ps{b}"])
                            if j < 5:
                                for hb2 in range(2):
                                    bq = S.rr("all", 8)
                                    for qi in range(2):
                                        q = q0 + hb2 * 2 + qi
                                        S.op("pe", lambda e: e.matmul(G.ps[bq][:, qi * 256:qi * 256 + 128], lhsT=an[:, q, 128:256],
                                                                      rhs=an[:, q, 0:128], start=True, stop=True), r=[ann], w=[f"ps{bq}"])
                                        S.op("pe", lambda e: e.matmul(G.ps[bq][:, qi * 256 + 128:(qi + 1) * 256], lhsT=an[:, q, 0:128],
                                                                      rhs=an[:, q, 128:256], start=True, stop=True), r=[ann], w=[f"ps{bq}"])
                                    S.op("act", lambda e: e.copy(out=AN[(j + 1) % 2][:, q0 + hb2 * 2:q0 + hb2 * 2 + 2, :],
                                                                 in_=G.ps[bq][:, :].rearrange("p (q x) -> p q x", q=2)),
                                         r=[f"ps{bq}"], w=[f"AN{(j + 1) % 2}" + g])
                            S.op("dve", lambda e: e.tensor_tensor(out=Xp[:, q0:q0 + 4, :], in0=Xp[:, q0:q0 + 4, :],
                                                                  in1=G.ps[b][:, :].rearrange("p (q x) -> p q x", q=4), op=ALU.add),
                                 r=[f"ps{b}", "Xp" + g, "XpA"], w=["Xp" + g, "XpA" + g])
                        return sx
                    for j in range(6):
                        steps.append(mkx(j))

                    def s8():
                        for par in range(2):
                            rows = slice(par * 64, par * 64 + 64)
                            b = S.rr("all", 8)
                            for qi in range(4):
                                q = q0 + qi
                                S.op("pe", lambda e: e.matmul(G.ps[b][0:64, qi * 128:(qi + 1) * 128], lhsT=Xp[rows, q, 0:64],
                                                              rhs=W1p[rows, q, 0:128], start=True, stop=True),
                                     r=["Xp" + g, "W1" + g, "W1A"], w=[f"ps{b}"])
                            cs = slice(2 * q0 + par, 2 * q0 + 8, 2)
                            S.op("dve", lambda e: e.tensor_tensor(out=GRT[:, cs, :], in0=G.ps[b][0:64, :].rearrange("p (c x) -> p c x", c=4),
                                                                  in1=DRv[:, cs, :], op=ALU.add), r=[f"ps{b}", "DRs", "DR"], w=["GRT" + g])
                        for par in range(2):
                            rows = slice(par * 64, par * 64 + 64)
                            b = S.rr("all", 8)
                            for qi in range(4):
                                q = q0 + qi
                                S.op("pe", lambda e: e.matmul(G.ps[b][:, qi * 64:(qi + 1) * 64], lhsT=W1p[rows, q, 0:128],
                                                              rhs=Xp[rows, q, 64:128], start=True, stop=False),
                                     r=["Xp" + g, "W1" + g, "W1A"], w=[f"ps{b}"])
                                S.op("pe", lambda e: e.matmul(G.ps[b][:, qi * 64:(qi + 1) * 64], lhsT=W2p[rows, q, 0:128],
                                                              rhs=Vtp[rows, q, :], start=False, stop=True),
                                     r=["Vtp", "W2" + g, "W2A"], w=[f"ps{b}"])
                            cs = slice(2 * q0 + par, 2 * q0 + 8, 2)
                            S.op("act", lambda e: e.copy(out=HYb[:, cs, :], in_=G.ps[b][:, 0:256].rearrange("p (c x) -> p c x", c=4)),
                                 r=[f"ps{b}"], w=["HYb" + g])
                    steps.append(s8)
                    return steps

                for batch in range(2):
                    lists = [group_steps(batch * 2 + gg) for gg in range(2)]
                    for si in range(len(lists[0])):
                        for gg in range(2):
                            lists[gg][si]()
                S.op("dve", lambda e: e.memset(STb[0][:], 0.0), w=["STb0"])
                for c in range(NCH):
                    si = c % 2
                    g = f"_{c // 8}"
                    b = S.rr("all", 8)
                    S.op("pe", lambda e: e.matmul(G.ps[b][:, 0:64], lhsT=GRT[:, c, :], rhs=STb[si][:], start=True, stop=False),
                         r=["GRT" + g, f"STb{si}"], w=[f"ps{b}"])
                    S.op("pe", lambda e: e.matmul(G.ps[b][:, 0:64], lhsT=G.identb[:, :], rhs=HYb[:, c, :], start=False, stop=True),
                         r=["HYb" + g, "identb"], w=[f"ps{b}"])
                    S.op("dve", lambda e: e.tensor_copy(out=STb[1 - si][:], in_=G.ps[b][0:64, 0:64]),
                         r=[f"ps{b}"], w=[f"STb{1 - si}"])
                    S.op("act", lambda e: e.copy(out=YD[64:128, c, :], in_=G.ps[b][64:128, 0:64]), r=[f"ps{b}"], w=["YD"])
                S.dma("sp", "y0", G.ydr.rearrange("(c t) v -> t c v", t=64)[:, :, h * 64:(h + 1) * 64], YD[64:128, :, :],
                      r=["YD"])
                S.barrier()

        with ExitStack() as e3:
            lgx = sbt(e3, nc, "lgx", [128, GW], F32)
            lbx = sbt(e3, nc, "lbx", [128, GW], F32)
            yt = [sbt(e3, nc, f"eyt{i}", [128, GW], F32) for i in range(2)]
            vt = [sbt(e3, nc, f"evt{i}", [128, GW], F32) for i in range(2)]
            sqs = [sbt(e3, nc, f"esq{i}", [128, GW], F32) for i in range(2)]
            yo = [sbt(e3, nc, f"eyo{i}", [128, GW], F32) for i in range(2)]
            stts = [sbt(e3, nc, f"est{i}", [128, 8, 8], F32) for i in range(2)]
            S.dma("sp", "c1", lgx[:], I["rwkv_lnx_g"][l:l + 1, :].partition_broadcast(128), w=["lgx"])
            S.dma("sp", "c1", lbx[:], I["rwkv_lnx_b"][l:l + 1, :].partition_broadcast(128), w=["lbx"])

            def ep_steps(tt, i):
                y, sq, stt = yt[i], sqs[i], stts[i]
                yn, sqn, stn = f"eyt{i}", f"esq{i}", f"est{i}"
                y3 = y[:].rearrange("p (h v) -> p h v", h=8)
                st = []
                st.append(lambda: S.dma("sp", f"x{i}", y[:], G.ydr[tt * 128:(tt + 1) * 128, :], w=[yn]))
                st.append(lambda: S.dma("sp", f"q{i}", vt[i][:], G.vtk[tt * 128:(tt + 1) * 128, :], w=[f"evt{i}"]))
                st.append(lambda: S.op("dve", lambda e: e.tensor_reduce(out=stt[:, 0, :], in_=y3, axis=AX.X, op=ALU.add), r=[yn], w=[stn]))
                st.append(lambda: S.op("act", lambda e: e.activation(out=sq[:], in_=y[:], func=AF.Square), r=[yn], w=[sqn]))
                st.append(lambda: S.op("dve", lambda e: e.tensor_reduce(out=stt[:, 1, :], in_=sq[:].rearrange("p (h v) -> p h v", h=8),
                                                                        axis=AX.X, op=ALU.add), r=[sqn], w=[stn]))
                st.append(lambda: S.op("dve", lambda e: e.tensor_scalar(out=stt[:, 2, :], in0=stt[:, 0, :], scalar1=1.0 / 64, scalar2=None,
                                                                        op0=ALU.mult), r=[stn], w=[stn]))
                st.append(lambda: S.op("dve", lambda e: e.tensor_tensor(out=stt[:, 3, :], in0=stt[:, 2, :], in1=stt[:, 2, :], op=ALU.mult),
                                       r=[stn], w=[stn]))
                st.append(lambda: S.op("dve", lambda e: e.scalar_tensor_tensor(out=stt[:, 4, :], in0=stt[:, 1, :], scalar=1.0 / 64,
                                                                               in1=stt[:, 3, :], op0=ALU.mult, op1=ALU.subtract),
                                       r=[stn], w=[stn]))
                st.append(lambda: S.op("dve", lambda e: e.tensor_scalar(out=stt[:, 4, :], in0=stt[:, 4, :], scalar1=64e-5, scalar2=None,
                                                                        op0=ALU.add), r=[stn], w=[stn]))
                st.append(lambda: S.op("act", lambda e: e.activation(out=stt[:, 5, :], in_=stt[:, 4, :], func=AF.Sqrt), r=[stn], w=[stn]))
                st.append(lambda: S.op("dve", lambda e: e.reciprocal(out=stt[:, 6, :], in_=stt[:, 5, :]), r=[stn], w=[stn]))
                st.append(lambda: S.op("dve", lambda e: e.tensor_tensor(out=y3, in0=y3, in1=stt[:, 2, :].unsqueeze(2).to_broadcast([128, 8, 64]),
                                                                        op=ALU.subtract), r=[yn, stn], w=[yn]))
                st.append(lambda: S.op("dve", lambda e: e.tensor_tensor(out=y3, in0=y3, in1=stt[:, 6, :].unsqueeze(2).to_broadcast([128, 8, 64]),
                                                                        op=ALU.mult), r=[yn, stn], w=[yn]))
                st.append(lambda: S.op("pool", lambda e: e.tensor_tensor(out=sq[:].rearrange("p (h v) -> p h v", h=8),
                                                                         in0=vt[i][:].rearrange("p (h v) -> p h v", h=8),
                                                                         in1=bon[:, tt, :].unsqueeze(2).to_broadcast([128, 8, 64]), op=ALU.mult),
                                       r=[f"evt{i}", "bon", sqn], w=[sqn]))
                st.append(lambda: S.op("pool", lambda e: e.tensor_tensor(out=y[:], in0=y[:], in1=lgx[:], op=ALU.mult), r=[yn, "lgx"], w=[yn]))
                st.append(lambda: S.op("pool", lambda e: e.tensor_tensor(out=y[:], in0=y[:], in1=lbx[:], op=ALU.add), r=[yn, "lbx"], w=[yn]))
                st.append(lambda: S.op("pool", lambda e: e.tensor_tensor(out=y[:], in0=y[:], in1=sq[:], op=ALU.add), r=[yn, sqn], w=[yn]))

                def gate():
                    b = 4 + S.rr("pp", 4)
                    for j in range(2):
                        S.op("pe", lambda e: e.matmul(G.ps[b][:, :], lhsT=sgb[:, j, tt * 128:(tt + 1) * 128], rhs=g2b[:, j, :],
                                                      start=(j == 0), stop=(j == 1)), r=["sgb", "g2b"], w=[f"ps{b}"])
                    S.op("dve", lambda e: e.tensor_tensor(out=yo[i][:], in0=G.ps[b][:, :], in1=y[:], op=ALU.mult),
                         r=[f"ps{b}", yn], w=[f"eyo{i}"])
                st.append(gate)
                st.append(lambda: S.dma("sp", f"y{i}", G.ycat[tt * 128:(tt + 1) * 128, 1536:2048], yo[i][:], r=[f"eyo{i}"]))
                return st

            for t0 in range(0, 16, 2):
                lockstep([ep_steps(t0 + i, i) for i in range(2)])
            S.barrier()


def prepare_inputs(inp):
    f = lambda a: np.ascontiguousarray(np.asarray(a, dtype=np.float32))
    shared = {}
    for k in ("norm_mix_g", "w_in", "pos_bias", "sgu_ln_g", "rwkv_w2", "rwkv_a2", "rwkv_g2", "rwkv_lnx_g", "rwkv_lnx_b",
              "branch_norm_g", "w_out", "norm_ffn_g", "w_gate", "w_up", "w_down"):
        shared[k] = f(inp[k])
    shared["norm_final_g"] = f(inp["norm_final_g"]).reshape(1, D_)
    shared["sgu_wT"] = f(np.transpose(np.asarray(inp["sgu_w"]), (0, 1, 3, 2)))
    shared["sgu_bT"] = f(np.transpose(np.asarray(inp["sgu_b"]), (0, 2, 1)))
    mu = np.asarray(inp["rwkv_mu"], dtype=np.float32)
    hk = lambda a: np.asarray(a, dtype=np.float32).reshape(DEPTH, 8, 64).transpose(0, 2, 1)
    pk = np.stack([hk(mu[:, 0:512]), hk(mu[:, 608:1120]), hk(mu[:, 1120:1632]), hk(inp["rwkv_w0"]), hk(inp["rwkv_a0"]),
                   hk(inp["rwkv_k_k"]), hk(inp["rwkv_k_a"]), hk(inp["rwkv_r_k"])], axis=-1)
    pk = pk.reshape(DEPTH, 64, 4, 2, 8).transpose(0, 3, 1, 2, 4).reshape(DEPTH, 128, 4, 8)
    shared["rwkv_pk"] = f(pk)
    mu2 = np.zeros((DEPTH, 128, 4), np.float32)
    mu2[:, 0:96, 0] = mu[:, 512:608]
    mu2[:, 0:96, 1] = mu[:, 1632:1728]
    mu2[:, :, 2] = mu[:, 1728:1856]
    mu2[:, :, 3] = mu[:, 1856:1984]
    shared["rwkv_mu2"] = mu2
    shared.update(host_constants())
    x = f(inp["x"])
    return [dict(shared, x=x[b]) for b in range(NCORES)]


def kernel(**inputs):
    if "nc" not in _PROG:
        _PROG["nc"] = build()
    in_maps = prepare_inputs(inputs)
    res = run_bass_kernel_spmd(_PROG["nc"], in_maps, core_ids=list(range(NCORES)))
    return np.stack([np.asarray(r["out"], dtype=np.float32) for r in res.results], axis=0)
```
